# Optimizing a Trainium2 kernel written in Bass

```python
import jax, jax.numpy as jnp
from jax import lax
import numpy as np

D_MODEL = 1024
BATCH = 16
SEQ = 4096
DEPTH = 1

GRID_W = 64
CTX_LEN = 256
MIX_WIDTH = D_MODEL
RWKV_WIDTH = MIX_WIDTH // 2
HEAD_SIZE = 64
RWKV_HEADS = RWKV_WIDTH // HEAD_SIZE
FNET_WIDTH = MIX_WIDTH - RWKV_WIDTH
FNET_GROUPS = 8
FNET_GROUP_W = FNET_WIDTH // FNET_GROUPS
DECAY_LORA = 64
ICLR_LORA = 64
GATE_LORA = 128
N_DIR = 2
SHIFT_WIDTH = 3 * RWKV_WIDTH + N_DIR * (DECAY_LORA + ICLR_LORA + GATE_LORA)
IN_WIDTH = SHIFT_WIDTH + FNET_WIDTH
N_EXPERTS = 32
TOP_K = 4
D_FF = D_MODEL
SWIGLU_LIMIT = 7.0
SWIGLU_ALPHA = 1.702
EXPERT_BLOCK = 128
DEEPNORM_ALPHA = (2.0 * DEPTH) ** 0.25
DEEPNORM_BETA = (8.0 * DEPTH) ** -0.25
LN_EPS = 1e-5
GN_EPS = 64e-5

kernel_name = "hybrid_rwkv7_fnet_moe_prefix_dit_layer"


def layer_norm(u, g, b):
    u32 = u.astype(jnp.float32)
    mean = jnp.mean(u32, -1, keepdims=True)
    var = jnp.mean(jnp.square(u32 - mean), -1, keepdims=True)
    return ((u32 - mean) * lax.rsqrt(var + LN_EPS) * g + b).astype(u.dtype)


def grid_shift(u):
    b_, t, ch = u.shape
    rows = t // GRID_W
    g = u.reshape(b_, rows, GRID_W, ch)
    p = jnp.pad(g, ((0, 0), (1, 1), (1, 1), (0, 0)))
    nb = (p[:, :-2, 1:-1] + p[:, 2:, 1:-1] + p[:, 1:-1, :-2] + p[:, 1:-1, 2:]) * 0.25
    return nb.reshape(b_, t, ch)


def seq_shift(u):
    p = jnp.pad(u, ((0, 0), (1, 1), (0, 0)))
    return (p[:, :-2] + p[:, 2:]) * 0.5


def heads(z):
    return z.reshape(z.shape[:-1] + (RWKV_HEADS, HEAD_SIZE))


def rwkv_prepare(m, w0, w2_decay, a0, a2_iclr, g2_gate, k_k, k_a):
    b_, t, _ = m.shape
    c = RWKV_WIDTH
    r = m[..., :c]
    k = m[..., c:2 * c]
    v = m[..., 2 * c:3 * c]
    o = 3 * c
    wd = m[..., o:o + N_DIR * DECAY_LORA].reshape(b_, t, N_DIR, DECAY_LORA)
    o += N_DIR * DECAY_LORA
    ad = m[..., o:o + N_DIR * ICLR_LORA].reshape(b_, t, N_DIR, ICLR_LORA)
    o += N_DIR * ICLR_LORA
    gd = m[..., o:o + N_DIR * GATE_LORA].reshape(b_, t, N_DIR, GATE_LORA)
    w_log = w0 + jnp.einsum('btdr,drc->btdc', jnp.tanh(wd), w2_decay)
    w = jnp.exp(-jnp.exp(-jax.nn.softplus(-w_log) - 0.5))
    a = jax.nn.sigmoid(a0 + jnp.einsum('btdr,drc->btdc', ad, a2_iclr))
    g = jnp.einsum('btdr,drc->btdc', jax.nn.sigmoid(gd), g2_gate)
    kk = heads(k * k_k)
    kk = kk / jnp.maximum(jnp.sqrt(jnp.sum(jnp.square(kk), -1, keepdims=True)), 1e-12)
    k_d = k[:, :, None] * (1.0 + (a - 1.0) * k_a)
    return heads(r), heads(k_d), heads(v), kk, heads(w), heads(a), g


def wkv_bidir(r, k_d, v, kk, w, a, state0):
    def both(u):
        return jnp.stack([u, jnp.flip(u, 1)], 0)

    def per(u):
        return jnp.stack([u[:, :, 0], jnp.flip(u[:, :, 1], 1)], 0)

    def tm(z):
        return jnp.moveaxis(z, 2, 0)

    xs = (tm(both(r)), tm(per(w)), tm(per(k_d)), tm(both(v)), tm(both(-kk)), tm(per(kk[:, :, None] * a)))

    def step(s, inp):
        r_t, w_t, k_t, v_t, aa_t, bb_t = inp
        sa = jnp.einsum('dbhij,dbhj->dbhi', s, aa_t)
        s = s * w_t[..., None, :] + sa[..., :, None] * bb_t[..., None, :] + v_t[..., :, None] * k_t[..., None, :]
        y_t = jnp.einsum('dbhij,dbhj->dbhi', s, r_t)
        return s, y_t

    state, ys = lax.scan(step, state0, xs)
    ys = jnp.moveaxis(ys, 0, 2)
    y = jnp.stack([ys[0], jnp.flip(ys[1], 1)], axis=2)
    return y, state


def rwkv_output(y, r, k_d, v, g, r_k, gn_g, gn_b):
    b_, t = y.shape[:2]
    mean = jnp.mean(y, -1, keepdims=True)
    var = jnp.mean(jnp.square(y - mean), -1, keepdims=True)
    yn = (y - mean) * lax.rsqrt(var + GN_EPS) * heads(gn_g) + heads(gn_b)
    bonus = jnp.sum(r[:, :, None] * k_d * r_k, -1, keepdims=True) * v[:, :, None]
    return jnp.sum((yn + bonus).reshape(b_, t, N_DIR, RWKV_WIDTH) * g, axis=2)


def fourier_mix(f, w_fno, b_fno):
    b_, t, _ = f.shape
    fg = f.reshape(b_, t, FNET_GROUPS, FNET_GROUP_W).astype(jnp.float32)
    spec = jnp.real(jnp.fft.fft2(fg, axes=(1, 3), norm='ortho'))
    y = jnp.einsum('btgc,gce->btge', spec, w_fno) + b_fno
    return y.reshape(b_, t, FNET_WIDTH)


def token_mixer(h_lat, h_ctx, w_in, mu_shift, w0, w2_decay, a0, a2_iclr, g2_gate, r_k, k_k, k_a,
                gn_g, gn_b, w_fno, b_fno, w_out, need_ctx):
    f32 = jnp.float32
    p_lat = jnp.einsum('btd,de->bte', h_lat, w_in).astype(f32)
    p_ctx = jnp.einsum('btd,de->bte', h_ctx, w_in).astype(f32)
    s_lat, f_lat = p_lat[..., :SHIFT_WIDTH], p_lat[..., SHIFT_WIDTH:]
    s_ctx, f_ctx = p_ctx[..., :SHIFT_WIDTH], p_ctx[..., SHIFT_WIDTH:]
    mu = mu_shift.astype(f32)
    m_lat = s_lat + mu * (grid_shift(s_lat) - s_lat)
    m_ctx = s_ctx + mu * (seq_shift(s_ctx) - s_ctx)
    dp = (w0, w2_decay, a0, a2_iclr, g2_gate, k_k, k_a)
    r_c, k_c, v_c, kk_c, w_c, a_c, g_c = rwkv_prepare(m_ctx, *dp)
    state0 = jnp.zeros((N_DIR, h_ctx.shape[0], RWKV_HEADS, HEAD_SIZE, HEAD_SIZE), f32)
    y_c, state_c = wkv_bidir(r_c, k_c, v_c, kk_c, w_c, a_c, state0)
    r_l, k_l, v_l, kk_l, w_l, a_l, g_l = rwkv_prepare(m_lat, *dp)
    y_l, _ = wkv_bidir(r_l, k_l, v_l, kk_l, w_l, a_l, state_c)
    rw_lat = rwkv_output(y_l, r_l, k_l, v_l, g_l, r_k, gn_g, gn_b)
    out_lat = jnp.concatenate([rw_lat, fourier_mix(f_lat, w_fno, b_fno)], -1) @ w_out.astype(f32)
    out_ctx = None
    if need_ctx:
        rw_ctx = rwkv_output(y_c, r_c, k_c, v_c, g_c, r_k, gn_g, gn_b)
        out_ctx = (jnp.concatenate([rw_ctx, fourier_mix(f_ctx, w_fno, b_fno)], -1) @ w_out.astype(f32)).astype(h_ctx.dtype)
    return out_lat.astype(h_lat.dtype), out_ctx


def moe_ffn(h, w_router, b_router, w1, b1, w2, b2):
    f32 = jnp.float32
    n_tok = h.shape[0]
    logits = h.astype(f32) @ w_router.astype(f32) + b_router.astype(f32)
    top_val, top_idx = lax.top_k(logits, TOP_K)
    gates = jax.nn.softmax(top_val, axis=-1)
    n_assign = n_tok * TOP_K
    e_flat = top_idx.reshape(-1).astype(jnp.int32)
    tok_flat = jnp.repeat(jnp.arange(n_tok, dtype=jnp.int32), TOP_K)
    g_flat = gates.reshape(-1)
    order = jnp.argsort(e_flat)
    e_s, tok_s, g_s = e_flat[order], tok_flat[order], g_flat[order]
    counts = jnp.bincount(e_flat, length=N_EXPERTS).astype(jnp.int32)
    starts = jnp.cumsum(counts) - counts
    padded = (counts + EXPERT_BLOCK - 1) // EXPERT_BLOCK * EXPERT_BLOCK
    pends = jnp.cumsum(padded)
    pstarts = pends - padded
    slot = pstarts[e_s] + jnp.arange(n_assign, dtype=jnp.int32) - starts[e_s]
    n_blocks = -(-n_assign // EXPERT_BLOCK) + N_EXPERTS
    n_slots = n_blocks * EXPERT_BLOCK
    slot_tok = jnp.zeros((n_slots,), jnp.int32).at[slot].set(tok_s)
    slot_gate = jnp.zeros((n_slots,), f32).at[slot].set(g_s)
    block_start = jnp.arange(n_blocks, dtype=jnp.int32) * EXPERT_BLOCK
    block_e = jnp.minimum(jnp.searchsorted(pends, block_start, side='right'), N_EXPERTS - 1)

    def expert_block(args):
        tok_b, e = args
        xb = h[tok_b]
        u = xb @ w1[e] + b1[e]
        glu = jnp.minimum(u[:, 0::2], SWIGLU_LIMIT)
        lin = jnp.clip(u[:, 1::2], -SWIGLU_LIMIT, SWIGLU_LIMIT)
        act = glu * jax.nn.sigmoid(SWIGLU_ALPHA * glu) * (lin + 1.0)
        return act @ w2[e] + b2[e]

    y = lax.map(expert_block, (slot_tok.reshape(n_blocks, EXPERT_BLOCK), block_e))
    out = jnp.zeros((n_tok, h.shape[1]), f32).at[slot_tok].add(slot_gate[:, None] * y.reshape(n_slots, -1))
    return out.astype(h.dtype)


def setup_inputs(seed: int = 0) -> dict:
    key = jax.random.key(seed)
    ks = iter(jax.random.split(key, 48))
    f32 = jnp.float32

    def nrm(shape, scale):
        return scale * jax.random.normal(next(ks), shape, f32)

    L, C, H, N = DEPTH, RWKV_WIDTH, RWKV_HEADS, HEAD_SIZE
    x = nrm((BATCH, SEQ, D_MODEL), 1.0)
    c = nrm((BATCH, D_MODEL), 1.0)
    ctx = nrm((BATCH, CTX_LEN, D_MODEL), 1.0)
    c_ctx = nrm((D_MODEL,), 1.0)
    ln0_g = 1.0 + nrm((D_MODEL,), 0.05)
    ln0_b = nrm((D_MODEL,), 0.02)
    w_ada = nrm((L, D_MODEL, 6 * D_MODEL), 0.5 * D_MODEL ** -0.5)
    b_ada = nrm((L, 6 * D_MODEL), 0.02)
    w_in = nrm((L, D_MODEL, IN_WIDTH), D_MODEL ** -0.5)
    w_in = w_in.at[:, :, 2 * C:3 * C].multiply(DEEPNORM_BETA)
    mu_shift = jax.random.uniform(next(ks), (L, SHIFT_WIDTH), f32, 0.2, 0.8)
    w0 = jax.random.uniform(next(ks), (L, N_DIR, C), f32, -6.0, 1.0)
    w2_decay = nrm((L, N_DIR, DECAY_LORA, C), 0.5 * DECAY_LORA ** -0.5)
    a0 = nrm((L, N_DIR, C), 0.5)
    a2_iclr = nrm((L, N_DIR, ICLR_LORA, C), 0.5 * ICLR_LORA ** -0.5)
    g2_gate = nrm((L, N_DIR, GATE_LORA, C), GATE_LORA ** -0.5)
    r_k = nrm((L, N_DIR, H, N), 0.1)
    k_k = 0.85 + nrm((L, C), 0.05)
    k_a = 1.0 + nrm((L, C), 0.05)
    gn_g = 1.0 + nrm((L, C), 0.05)
    gn_b = nrm((L, C), 0.02)
    w_fno = nrm((L, FNET_GROUPS, FNET_GROUP_W, FNET_GROUP_W), FNET_GROUP_W ** -0.5)
    b_fno = nrm((L, FNET_GROUPS, FNET_GROUP_W), 0.02)
    w_out = nrm((L, MIX_WIDTH, D_MODEL), DEEPNORM_BETA * MIX_WIDTH ** -0.5)
    ln1_g = 1.0 + nrm((L, D_MODEL), 0.05)
    ln1_b = nrm((L, D_MODEL), 0.02)
    w_router = nrm((L, D_MODEL, N_EXPERTS), D_MODEL ** -0.5)
    b_router = nrm((L, N_EXPERTS), 0.01)
    w1 = nrm((L, N_EXPERTS, D_MODEL, 2 * D_FF), D_MODEL ** -0.5)
    b1 = nrm((L, N_EXPERTS, 2 * D_FF), 0.02)
    w2 = nrm((L, N_EXPERTS, D_FF, D_MODEL), DEEPNORM_BETA * D_FF ** -0.5)
    b2 = nrm((L, N_EXPERTS, D_MODEL), 0.02)
    ln2_g = 1.0 + nrm((L, D_MODEL), 0.05)
    ln2_b = nrm((L, D_MODEL), 0.02)
    return {"x": x, "c": c, "ctx": ctx, "c_ctx": c_ctx, "ln0_g": ln0_g, "ln0_b": ln0_b,
            "w_ada": w_ada, "b_ada": b_ada, "w_in": w_in, "mu_shift": mu_shift, "w0": w0,
            "w2_decay": w2_decay, "a0": a0, "a2_iclr": a2_iclr, "g2_gate": g2_gate, "r_k": r_k,
            "k_k": k_k, "k_a": k_a, "gn_g": gn_g, "gn_b": gn_b, "w_fno": w_fno, "b_fno": b_fno,
            "w_out": w_out, "ln1_g": ln1_g, "ln1_b": ln1_b, "w_router": w_router, "b_router": b_router,
            "w1": w1, "b1": b1, "w2": w2, "b2": b2, "ln2_g": ln2_g, "ln2_b": ln2_b}


def reference(x, c, ctx, c_ctx, ln0_g, ln0_b, w_ada, b_ada, w_in, mu_shift, w0, w2_decay, a0, a2_iclr,
              g2_gate, r_k, k_k, k_a, gn_g, gn_b, w_fno, b_fno, w_out, ln1_g, ln1_b, w_router, b_router,
              w1, b1, w2, b2, ln2_g, ln2_b):
    x = layer_norm(x, ln0_g, ln0_b)
    ctx = layer_norm(ctx, ln0_g, ln0_b)
    for l in range(DEPTH):
        need_ctx = l < DEPTH - 1
        mod = (jax.nn.silu(c) @ w_ada[l] + b_ada[l])[:, None, :]
        sh1, sc1, gt1, sh2, sc2, gt2 = jnp.split(mod, 6, axis=-1)
        mod_c = jax.nn.silu(c_ctx) @ w_ada[l] + b_ada[l]
        sh1c, sc1c, gt1c, sh2c, sc2c, gt2c = jnp.split(mod_c, 6, axis=-1)
        h = x * (1.0 + sc1) + sh1
        hc = ctx * (1.0 + sc1c) + sh1c
        mo, mo_c = token_mixer(h, hc, w_in[l], mu_shift[l], w0[l], w2_decay[l], a0[l], a2_iclr[l], g2_gate[l],
                               r_k[l], k_k[l], k_a[l], gn_g[l], gn_b[l], w_fno[l], b_fno[l], w_out[l], need_ctx)
        x = layer_norm(DEEPNORM_ALPHA * x + gt1 * mo, ln1_g[l], ln1_b[l])
        h = x * (1.0 + sc2) + sh2
        f = moe_ffn(h.reshape(-1, D_MODEL), w_router[l], b_router[l], w1[l], b1[l], w2[l], b2[l]).reshape(x.shape)
        x = layer_norm(DEEPNORM_ALPHA * x + gt2 * f, ln2_g[l], ln2_b[l])
        if need_ctx:
            ctx = layer_norm(DEEPNORM_ALPHA * ctx + gt1c * mo_c, ln1_g[l], ln1_b[l])
            hc = ctx * (1.0 + sc2c) + sh2c
            fc = moe_ffn(hc.reshape(-1, D_MODEL), w_router[l], b_router[l], w1[l], b1[l], w2[l], b2[l]).reshape(ctx.shape)
            ctx = layer_norm(DEEPNORM_ALPHA * ctx + gt2c * fc, ln2_g[l], ln2_b[l])
    return x
```

```python
import numpy as np
import ml_dtypes
BF_NP = ml_dtypes.bfloat16
from contextlib import ExitStack, contextmanager
import concourse.bass as bass
import concourse.mybir as mybir
from concourse.bass_utils import run_bass_kernel_spmd

F32 = mybir.dt.float32
BF16 = mybir.dt.bfloat16
I32 = mybir.dt.int32
U32 = mybir.dt.uint32
ALU = mybir.AluOpType
AF = mybir.ActivationFunctionType
AX = mybir.AxisListType

NCORES = 8
BPC = 2
D = 1024
SEQ = 4096
CTX = 256
NV = CTX + SEQ
INW = 2560
SHW = 2048
C = 512
GRID = 64
NE = 32
ALPHA = 2.0 ** 0.25
LN_EPS = 1e-5
GN_EPS = 64e-5
NT6 = SEQ // 128
STOP6 = 99
NT8 = SEQ // 128
NE7 = 32
NB7 = 16
NBLK7 = 96
NTT = BPC * SEQ // 128
NBLK = 96
NSLOT = NBLK * 512
SPARSE = True

ENG = ("pe", "dve", "act", "pool", "sp")
NRING = 8


class Ctx:
    def __init__(self, nc, es):
        self.nc = nc
        self.es = es
        self.q = {e: [] for e in ENG}
        self.cnt = {e: 0 for e in ENG}
        self.sem = {e: es.enter_context(nc.semaphore("s_" + e)) for e in ENG}
        self.seen = {e: {} for e in ENG}
        self.W = {}
        self.R = {}
        self.ring = {}
        self.dn = {}
        for qn in ("sp", "act", "pool"):
            self.ring[qn] = [es.enter_context(nc.semaphore("d_%s%d" % (qn, i))) for i in range(NRING)]
            self.dn[qn] = 0
        self.uid = 0
        self.cur = es

    def sb(self, name, shape, dtype):
        return self.cur.enter_context(self.nc.sbuf_tensor("t_" + name, list(shape), dtype))

    def ps(self, name, shape, dtype=F32):
        return self.cur.enter_context(self.nc.psum_tensor("p_" + name, list(shape), dtype))

    @contextmanager
    def phase(self):
        st = ExitStack()
        prev = self.cur
        self.cur = st
        try:
            yield
        finally:
            self.barrier()
            self.flush()
            st.close()
            self.cur = prev

    def barrier(self):
        toks = []
        for e in ENG:
            if self.cnt[e] > 0:
                toks.append(("e_" + e, self.sem[e], self.cnt[e], e))
        for qn in self.ring:
            n = self.dn[qn]
            for slot in range(NRING):
                if n > slot:
                    rounds = (n - 1 - slot) // NRING + 1
                    toks.append(("d_%s%d" % (qn, slot), self.ring[qn][slot], 16 * rounds, "dma"))
        for eng in ENG:
            waits = []
            for (semkey, sem, val, src) in toks:
                if src == eng:
                    continue
                if self.seen[eng].get(semkey, 0) >= val:
                    continue
                self.seen[eng][semkey] = val
                waits.append((sem, val))

            def emit(e, waits=waits):
                for (s, v) in waits:
                    e.wait_ge(s, v)

            self.q[eng].append(emit)

    def flush(self):
        qs = self.q
        self.q = {e: [] for e in ENG}
        with self.nc.Block() as block:
            @block.tensor
            def _(e):
                for f in qs["pe"]:
                    f(e)

            @block.vector
            def _(e):
                for f in qs["dve"]:
                    f(e)

            @block.scalar
            def _(e):
                for f in qs["act"]:
                    f(e)

            @block.gpsimd
            def _(e):
                for f in qs["pool"]:
                    f(e)

            @block.sync
            def _(e):
                for f in qs["sp"]:
                    f(e)

    def _deps(self, eng, reads, writes):
        toks = []
        for k in reads:
            if k in self.W:
                toks.append(self.W[k])
        for k in writes:
            if k in self.W:
                toks.append(self.W[k])
            toks.extend(self.R.get(k, ()))
        waits = {}
        for (semkey, sem, val, src) in toks:
            if src == eng and eng == "pe":
                continue
            if self.seen[eng].get(semkey, 0) >= val:
                continue
            if waits.get(semkey, (None, 0))[1] < val:
                waits[semkey] = (sem, val)
        for semkey, (sem, val) in waits.items():
            self.seen[eng][semkey] = val
        return list(waits.values())

    def _commit(self, tok, reads, writes):
        for k in reads:
            self.R.setdefault(k, []).append(tok)
        for k in writes:
            self.W[k] = tok
            self.R[k] = []

    def op(self, eng, fn, reads=(), writes=()):
        waits = self._deps(eng, reads, writes)
        self.cnt[eng] += 1
        idx = self.cnt[eng]
        mysem = self.sem[eng]
        tok = ("e_" + eng, mysem, idx, eng)
        self._commit(tok, reads, writes)

        def emit(e, waits=waits, fn=fn, mysem=mysem):
            for (s, v) in waits:
                e.wait_ge(s, v)
            fn(e).then_inc(mysem, 1)

        self.q[eng].append(emit)

    def dma(self, qn, fn, reads=(), writes=()):
        waits = self._deps(qn, reads, writes)
        n = self.dn[qn]
        self.dn[qn] += 1
        slot, rnd = n % NRING, n // NRING
        sem = self.ring[qn][slot]
        semkey = "d_%s%d" % (qn, slot)
        if rnd > 0 and self.seen[qn].get(semkey, 0) < 16 * rnd:
            waits.append((sem, 16 * rnd))
            self.seen[qn][semkey] = 16 * rnd
        tok = (semkey, sem, 16 * (rnd + 1), "dma")
        self._commit(tok, reads, writes)

        def emit(e, waits=waits, fn=fn, sem=sem):
            for (s, v) in waits:
                e.wait_ge(s, v)
            fn(e).then_inc(sem, 16)

        self.q[qn].append(emit)

    def finish(self, final_keys):
        self.barrier()
        self.flush()


def build_program(dbg=None):
    _BREG.clear()
    nc = bass.Bass("TRN2", target_bir_lowering=False)
    es = ExitStack()
    with es:
        k = Ctx(nc, es)
        _build(nc, k, dbg)
    return nc


def dram_in(nc, name, shape, dtype=F32):
    return nc.dram_tensor(name, list(shape), dtype, kind="ExternalInput")


def _build(nc, k, dbg):
    x = dram_in(nc, "x", [BPC, SEQ, D])
    ctx_in = dram_in(nc, "ctx", [BPC, CTX, D])
    cvec = dram_in(nc, "cvec", [3, D])
    ln0 = dram_in(nc, "ln0", [2, D])
    w_ada = dram_in(nc, "w_ada", [D, 6 * D])
    b_ada = dram_in(nc, "b_ada", [1, 6 * D])
    w_in = dram_in(nc, "w_in", [D, INW])
    mu = dram_in(nc, "mu", [SHW // 128, 128])
    ident_d = dram_in(nc, "ident", [128, 128])
    out = nc.dram_tensor("out", [BPC, SEQ, D], F32, kind="ExternalOutput")

    vecs = dram_in(nc, "vecs", [44, 128])
    w2d_d = dram_in(nc, "w2d", [128, C])
    a2_d = dram_in(nc, "a2", [128, C])
    g2_d = dram_in(nc, "g2", [2, 128, C])
    bones_d = dram_in(nc, "bones", [128, 128])
    SC = nc.dram_tensor("SC", [BPC, 9, 4, 128, NV], F32, kind="Internal")
    GT = nc.dram_tensor("GT", [BPC, 2, 4, 128, SEQ], F32, kind="Internal")
    bonesb_d = dram_in(nc, "bonesb", [128, 128], BF16)
    i2rep_d = dram_in(nc, "i2rep", [128, 16, 64], BF16)
    hsel_d = dram_in(nc, "hsel", [128, 2], BF16)
    oh_d = dram_in(nc, "oh", [128, TC, 40], BF16)
    mask_d = dram_in(nc, "smask", [104, 512])
    YSr = nc.dram_tensor("YSr", [2, SEQ, 1024], BF16, kind="Internal")
    jmat_d = dram_in(nc, "jmat", [128, 128])
    ccbd_d = dram_in(nc, "ccbd", [128, 128])
    scbd_d = dram_in(nc, "scbd", [128, 128])
    cosm_d = dram_in(nc, "cosm", [8, 128, 32 * 512], BF16)
    nsin_d = dram_in(nc, "nsin", [8, 128, 32 * 512], BF16)
    wfno_d = dram_in(nc, "wfno", [8, 64, 64])
    w_out = dram_in(nc, "w_out", [D, D])
    ln1 = dram_in(nc, "ln1", [2, D])
    ln2 = dram_in(nc, "ln2", [2, D])
    w_router = dram_in(nc, "w_router", [D, NE])
    b_router = dram_in(nc, "b_router", [1, NE])
    w1 = dram_in(nc, "w1", [NE, D, 2 * D])
    b1 = dram_in(nc, "b1", [NE, 2 * D])
    w2 = dram_in(nc, "w2", [NE, D, D])
    b2 = dram_in(nc, "b2", [NE, D])
    MIXT = nc.dram_tensor("MIXT", [BPC, 8, 128, SEQ], BF16, kind="Internal")
    X1 = nc.dram_tensor("X1", [BPC, SEQ, D], F32, kind="Internal")
    H2T = nc.dram_tensor("H2T", [BPC, 8, 128, SEQ], BF16, kind="Internal")
    GTd = nc.dram_tensor("GTd", [NE, BPC * SEQ], F32, kind="Internal")
    FD = nc.dram_tensor("FD", [BPC * SEQ, D], F32, kind="Internal")
    GD = nc.dram_tensor("GD", [BPC * SEQ, NE], F32, kind="Internal")
    ustr_d = dram_in(nc, "ustr", [128, 128])
    thr_d = dram_in(nc, "thr", [1, NE, 16])
    jg_d = dram_in(nc, "jg", [1, NBLK, NE])
    kcp_d = dram_in(nc, "kcp", [128, 8])
    MOD = nc.dram_tensor("MOD", [3, 6 * D], F32, kind="Internal")
    PFM = nc.dram_tensor("PFM", [BPC, INW, NV], F32, kind="Internal")
    dbg_t = None
    if dbg is not None:
        dbg_t = nc.dram_tensor("dbg", list(dbg[1]), F32, kind="ExternalOutput")

    ident = k.sb("ident", [128, 128], F32)
    k.dma("sp", lambda e: e.dma_start(out=ident[:], in_=ident_d.ap()), writes=["ident"])
    identb = k.sb("identb", [128, 128], BF16)
    k.dma("pool", lambda e: e.dma_start(out=identb[:], in_=ident_d.ap()), writes=["identb"])

    with k.phase():
        phase0_mod(nc, k, cvec, w_ada, b_ada, MOD)
    if dbg is not None and dbg[0] == "mod":
        t = k.sb("dbgt", [3, 6 * D], F32)
        k.dma("sp", lambda e: e.dma_start(out=t[:], in_=MOD.ap()), reads=["MOD"], writes=["dbgt"])
        k.dma("sp", lambda e: e.dma_start(out=dbg_t.ap(), in_=t[:]), reads=["dbgt"], writes=["dbg"])
        k.finish(["dbg"])
        return
    with k.phase():
        phase1_proj(nc, k, x, ctx_in, ln0, MOD, w_in, mu, PFM, ident)
    if dbg is not None and dbg[0] == "sc":
        with k.phase():
            phase2_prepare(nc, k, PFM, vecs, w2d_d, a2_d, g2_d, bones_d, SC, GT)
        b_, q_, hp_, n0 = dbg[2]
        t = k.sb("dbgt", [128, 512], F32)
        srcap = SC[b_, q_, hp_, :, n0:n0 + 512] if q_ < 9 else GT[b_, q_ - 9, hp_, :, n0:n0 + 512]
        k.dma("sp", lambda e: e.dma_start(out=t[:], in_=srcap), reads=["SC", "GT"], writes=["dbgt"])
        k.dma("sp", lambda e: e.dma_start(out=dbg_t.ap(), in_=t[:]), reads=["dbgt"], writes=["dbg"])
        k.finish(["dbg"])
        return
    if dbg is not None and dbg[0] == "scan":
        with k.phase():
            phase2_prepare(nc, k, PFM, vecs, w2d_d, a2_d, g2_d, bones_d, SC, GT)
        with k.phase():
            phase3_scan(nc, k, SC, YSr, ident, identb, oh_d, mask_d)
        hh_, s0 = dbg[2]
        t = k.sb("dbgt", [128, 1024], F32)
        k.dma("pool", lambda e: e.dma_start(out=t[:], in_=YSr[hh_, s0:s0 + 128, :]), reads=["YSr"], writes=["dbgt"])
        k.dma("sp", lambda e: e.dma_start(out=dbg_t.ap(), in_=t[:]), reads=["dbgt"], writes=["dbg"])
        k.finish(["dbg"])
        return
    if dbg is not None and dbg[0] == "pfm":
        b_, r0, n0 = dbg[2]
        t = k.sb("dbgt", [128, 512], F32)
        k.dma("sp", lambda e: e.dma_start(out=t[:], in_=PFM[b_, r0:r0 + 128, n0:n0 + 512]), reads=["PFM"], writes=["dbgt"])
        k.dma("sp", lambda e: e.dma_start(out=dbg_t.ap(), in_=t[:]), reads=["dbgt"], writes=["dbg"])
        k.finish(["dbg"])
        return
    with k.phase():
        phase2_prepare(nc, k, PFM, vecs, w2d_d, a2_d, g2_d, bones_d, SC, GT)
    with k.phase():
        phase3_scan(nc, k, SC, YSr, ident, identb, oh_d, mask_d)
    with k.phase():
        phase4_rwkv_out(nc, k, SC, GT, YSr, vecs, bones_d, ident, jmat_d, MIXT)
    if not (dbg is not None and len(dbg) > 3 and dbg[3] == "skip5"):
        with k.phase():
            phase5_fft(nc, k, PFM, wfno_d, vecs, ccbd_d, scbd_d, cosm_d, nsin_d, MIXT)
    if dbg is not None and dbg[0] == "mixt":
        b_, c_, n0 = dbg[2]
        t = k.sb("dbgtb", [128, 512], BF16)
        t2 = k.sb("dbgt", [128, 512], F32)
        k.dma("sp", lambda e: e.dma_start(out=t[:], in_=MIXT[b_, c_, :, n0:n0 + 512]), reads=["MIXT"], writes=["dbgtb"])
        k.op("dve", lambda e: e.tensor_copy(out=t2[:], in_=t[:]), reads=["dbgtb"], writes=["dbgt"])
        k.dma("sp", lambda e: e.dma_start(out=dbg_t.ap(), in_=t2[:]), reads=["dbgt"], writes=["dbg"])
        k.finish(["dbg"])
        return
    sp = make_sp(nc, k, ustr_d, thr_d, jg_d, kcp_d)
    sp["GD"] = GD
    with k.phase():
        phase6_outproj(nc, k, x, ln0, ln1, MOD, w_out, w_router, b_router, MIXT, ident, X1, H2T, GTd, GD,
                       sp if SPARSE else None)
    if dbg is not None and dbg[0] == "x1":
        b_, n0 = dbg[2]
        t2 = k.sb("dbgt", [128, 1024], F32)
        k.dma("sp", lambda e: e.dma_start(out=t2[:], in_=X1[b_, n0:n0 + 128, :]), reads=["X1"], writes=["dbgt"])
        k.dma("sp", lambda e: e.dma_start(out=dbg_t.ap(), in_=t2[:]), reads=["dbgt"], writes=["dbg"])
        k.finish(["dbg"])
        return
    if SPARSE:
        with k.phase():
            phase6b_blocks(nc, k, sp)
        with k.phase():
            phase6c_dispatch(nc, k, sp)
        with k.phase():
            phase7_sparse(nc, k, w1, b1, w2, sp, identb)
        with k.phase():
            phase8_final(nc, k, X1, FD, GTd, b2, ln2, MOD, out, sp)
    else:
        with k.phase():
            phase7_moe(nc, k, w1, b1, w2, H2T, GD, FD)
        with k.phase():
            phase8_final(nc, k, X1, FD, GTd, b2, ln2, MOD, out)
    k.finish(["out"])


def phase0_mod(nc, k, cvec, w_ada, b_ada, MOD):
    cT = k.sb("cT", [128, 8, 3], F32)
    for r in range(3):
        k.dma("sp", lambda e, r=r: e.dma_start(
            out=cT[:, :, r], in_=cvec[r].rearrange("(kc p) -> p kc", p=128),
            allow_slow_non_contiguous=True), writes=["cT"])
    sT = k.sb("sT", [128, 8, 3], F32)
    k.op("act", lambda e: e.activation(out=sT[:], in_=cT[:], func=AF.Silu), reads=["cT"], writes=["sT"])
    bada = k.sb("bada", [3, 6 * D], F32)
    for r in range(3):
        k.dma("sp", lambda e, r=r: e.dma_start(out=bada[r:r + 1, :], in_=b_ada.ap()), writes=["bada"])
    modsb = k.sb("modsb", [3, 6 * D], F32)
    wblk = [k.sb("wadab%d" % i, [128, 8, 512], F32) for i in range(2)]
    pm = k.ps("pmod", [3, 512])
    for cb in range(12):
        wb = wblk[cb % 2]
        wn = "wadab%d" % (cb % 2)
        k.dma("sp", lambda e, cb=cb, wb=wb: e.dma_start(
            out=wb[:], in_=w_ada[:, cb * 512:(cb + 1) * 512].rearrange("(kc p) n -> p kc n", p=128)),
            writes=[wn])
        for kc in range(8):
            k.op("pe", lambda e, kc=kc, wb=wb: e.matmul(pm[:], lhsT=sT[:, kc, :], rhs=wb[:, kc, :],
                                                       start=(kc == 0), stop=(kc == 7)),
                 reads=["sT", wn], writes=["pmod"])
        k.op("dve", lambda e, cb=cb: e.tensor_tensor(out=modsb[:, cb * 512:(cb + 1) * 512], in0=pm[:],
                                                    in1=bada[:, cb * 512:(cb + 1) * 512], op=ALU.add),
             reads=["pmod", "bada"], writes=["modsb"])
    k.dma("sp", lambda e: e.dma_start(out=MOD.ap(), in_=modsb[:]), reads=["modsb"], writes=["MOD"])


def load_fm_vec(k, name, src_ap_1d, ncols, q="sp"):
    t = k.sb(name, [128, ncols], F32)
    k.dma(q, lambda e: e.dma_start(out=t[:], in_=src_ap_1d.rearrange("(j p) -> p j", p=128),
                                   allow_slow_non_contiguous=True), reads=["MOD"], writes=[name])
    return t


def phase1_proj(nc, k, x, ctx_in, ln0, MOD, w_in, mu, PFM, ident):
    g0F = load_fm_vec(k, "g0F", ln0[0], 8)
    b0F = load_fm_vec(k, "b0F", ln0[1], 8)
    modF = [load_fm_vec(k, "modF%d" % r, MOD[r], 48) for r in range(3)]
    S1 = k.sb("S1", [128, 3, 8], F32)
    B1 = k.sb("B1", [128, 3, 8], F32)
    for r in range(3):
        k.op("dve", lambda e, r=r: e.scalar_tensor_tensor(out=S1[:, r, :], in0=modF[r][:, 8:16], scalar=1.0,
                                                         in1=g0F[:], op0=ALU.add, op1=ALU.mult),
             reads=["modF%d" % r, "g0F"], writes=["S1"])
        k.op("dve", lambda e, r=r: e.scalar_tensor_tensor(out=B1[:, r, :], in0=modF[r][:, 8:16], scalar=1.0,
                                                         in1=b0F[:], op0=ALU.add, op1=ALU.mult),
             reads=["modF%d" % r, "b0F"], writes=["B1"])
        k.op("dve", lambda e, r=r: e.tensor_tensor(out=B1[:, r, :], in0=B1[:, r, :], in1=modF[r][:, 0:8],
                                                  op=ALU.add),
             reads=["modF%d" % r, "B1"], writes=["B1"])
    muF = k.sb("muF", [128, 16], F32)
    k.dma("sp", lambda e: e.dma_start(out=muF[:], in_=mu.ap().rearrange("o p -> p o"),
                                      allow_slow_non_contiguous=True), writes=["muF"])
    omm = k.sb("omm", [128, 16], F32)
    mu025 = k.sb("mu025", [128, 16], F32)
    mu05 = k.sb("mu05", [128, 16], F32)
    k.op("dve", lambda e: e.tensor_scalar(out=omm[:], in0=muF[:], scalar1=-1.0, scalar2=1.0,
                                          op0=ALU.mult, op1=ALU.add), reads=["muF"], writes=["omm"])
    k.op("dve", lambda e: e.tensor_scalar(out=mu025[:], in0=muF[:], scalar1=0.25, scalar2=None,
                                          op0=ALU.mult), reads=["muF"], writes=["mu025"])
    k.op("dve", lambda e: e.tensor_scalar(out=mu05[:], in0=muF[:], scalar1=0.5, scalar2=None,
                                          op0=ALU.mult), reads=["muF"], writes=["mu05"])
    wbf = k.sb("winbf", [128, 8, INW], BF16)
    for kc in range(8):
        for hf in range(2):
            k.dma("pool", lambda e, kc=kc, hf=hf: e.dma_start(
                out=wbf[:, kc, hf * 1280:(hf + 1) * 1280],
                in_=w_in[kc * 128:(kc + 1) * 128, hf * 1280:(hf + 1) * 1280]), writes=["winbf"])
    hT = k.sb("hT", [128, 8, SEQ], BF16)
    xt = [k.sb("xt%d" % i, [128, D], F32) for i in range(2)]
    xn = [k.sb("xn%d" % i, [128, D], F32) for i in range(2)]
    st = k.sb("bnst", [128, 2, 6], F32)
    mv = k.sb("bnmv", [128, 2], F32)
    rstd = k.sb("rstd", [128, 1], F32)
    pT = [k.ps("pT%d" % i, [128, 8, 128]) for i in range(2)]
    pmm = [k.ps("pmm%d" % i, [128, 512]) for i in range(2)]
    pch = [k.sb("pch%d" % i, [128, SEQ], F32) for i in range(2)]
    acc = k.sb("shacc", [128, SEQ], F32)
    mch = k.sb("mch", [128, SEQ], F32)
    ti = 0
    ci = 0
    mi = 0
    for b in range(BPC):
        for seg in range(2):
            ntok = CTX if seg == 0 else SEQ
            r = 2 if seg == 0 else b
            off = 0 if seg == 0 else CTX
            src = ctx_in[b] if seg == 0 else x[b]
            for t in range(ntok // 128):
                X, XN, PT = xt[ti % 2], xn[ti % 2], pT[ti % 2]
                xk, xnk, ptk = "xt%d" % (ti % 2), "xn%d" % (ti % 2), "pT%d" % (ti % 2)
                ti += 1
                k.dma("sp", lambda e, X=X, t=t, src=src: e.dma_start(out=X[:], in_=src[t * 128:(t + 1) * 128, :]),
                      writes=[xk])
                for hf in range(2):
                    k.op("dve", lambda e, X=X, hf=hf: e.bn_stats(out=st[:, hf, :], in_=X[:, hf * 512:(hf + 1) * 512]),
                         reads=[xk], writes=["bnst"])
                k.op("dve", lambda e: e.bn_aggr(out=mv[:], in_=st[:].rearrange("p a b -> p (a b)")),
                     reads=["bnst"], writes=["bnmv"])
                k.op("act", lambda e: e.activation(out=rstd[:], in_=mv[:, 1:2], func=AF.Sqrt, bias=LN_EPS),
                     reads=["bnmv"], writes=["rstd"])
                k.op("dve", lambda e: e.reciprocal(out=rstd[:], in_=rstd[:]), reads=["rstd"], writes=["rstd"])
                k.op("dve", lambda e, X=X, XN=XN: e.tensor_scalar(out=XN[:], in0=X[:], scalar1=mv[:, 0:1],
                                                                 scalar2=rstd[:, 0:1], op0=ALU.subtract,
                                                                 op1=ALU.mult),
                     reads=[xk, "bnmv", "rstd"], writes=[xnk])
                for kc in range(8):
                    k.op("pe", lambda e, XN=XN, PT=PT, kc=kc: e.transpose(out=PT[:, kc, :],
                                                                         in_=XN[:, kc * 128:(kc + 1) * 128],
                                                                         identity=ident[:]),
                         reads=[xnk, "ident"], writes=[ptk])
                for kc in range(8):
                    if kc % 2 == 0:
                        k.op("act", lambda e, PT=PT, kc=kc, t=t, r=r: e.activation(
                            out=hT[:, kc, t * 128:(t + 1) * 128], in_=PT[:, kc, :], func=AF.Identity,
                            scale=S1[:, r, kc:kc + 1], bias=B1[:, r, kc:kc + 1]),
                            reads=[ptk, "S1", "B1"], writes=["hT"])
                    else:
                        k.op("dve", lambda e, PT=PT, kc=kc, t=t, r=r: e.tensor_scalar(
                            out=hT[:, kc, t * 128:(t + 1) * 128], in0=PT[:, kc, :],
                            scalar1=S1[:, r, kc:kc + 1], scalar2=B1[:, r, kc:kc + 1],
                            op0=ALU.mult, op1=ALU.add),
                            reads=[ptk, "S1", "B1"], writes=["hT"])
            noc = 16 if seg == 0 else 20
            tbw = min(512, ntok)
            for oc in range(noc):
                PCH = pch[ci % 2]
                pk = "pch%d" % (ci % 2)
                ci += 1
                for tb in range(ntok // tbw):
                    PM = pmm[mi % 2]
                    pmk = "pmm%d" % (mi % 2)
                    mi += 1
                    for kc in range(8):
                        k.op("pe", lambda e, PM=PM, kc=kc, oc=oc, tb=tb, tbw=tbw: e.matmul(
                            PM[:, 0:tbw], lhsT=wbf[:, kc, oc * 128:(oc + 1) * 128],
                            rhs=hT[:, kc, tb * tbw:(tb + 1) * tbw], start=(kc == 0), stop=(kc == 7)),
                            reads=["winbf", "hT"], writes=[pmk])
                    k.op("act", lambda e, PM=PM, PCH=PCH, tb=tb, tbw=tbw: e.activation(
                        out=PCH[:, tb * tbw:(tb + 1) * tbw], in_=PM[:, 0:tbw], func=AF.Copy),
                        reads=[pmk], writes=[pk])
                dst = PFM[b, oc * 128:(oc + 1) * 128, off:off + ntok]
                if oc >= 16:
                    k.dma("sp", lambda e, PCH=PCH, dst=dst, ntok=ntok: e.dma_start(out=dst, in_=PCH[:, 0:ntok]),
                          reads=[pk], writes=["PFM"])
                    continue
                if seg == 1:
                    s3 = PCH[:].rearrange("p (r c) -> p r c", c=GRID)
                    a3 = acc[:].rearrange("p (r c) -> p r c", c=GRID)
                    k.op("pool", lambda e, a3=a3: e.memset(a3[:, 0:1, :], 0.0), writes=["shacc"])
                    k.op("pool", lambda e, a3=a3, s3=s3: e.tensor_copy(out=a3[:, 1:, :], in_=s3[:, 0:GRID - 1, :]),
                         reads=[pk], writes=["shacc"])
                    k.op("pool", lambda e, a3=a3, s3=s3: e.tensor_tensor(out=a3[:, 0:GRID - 1, :],
                                                                        in0=a3[:, 0:GRID - 1, :],
                                                                        in1=s3[:, 1:, :], op=ALU.add),
                         reads=[pk, "shacc"], writes=["shacc"])
                    k.op("dve", lambda e, a3=a3, s3=s3: e.tensor_tensor(out=a3[:, :, 1:], in0=a3[:, :, 1:],
                                                                       in1=s3[:, :, 0:GRID - 1], op=ALU.add),
                         reads=[pk, "shacc"], writes=["shacc"])
                    k.op("dve", lambda e, a3=a3, s3=s3: e.tensor_tensor(out=a3[:, :, 0:GRID - 1],
                                                                       in0=a3[:, :, 0:GRID - 1],
                                                                       in1=s3[:, :, 1:], op=ALU.add),
                         reads=[pk, "shacc"], writes=["shacc"])
                    msc = mu025
                    mk = "mu025"
                else:
                    k.op("pool", lambda e: e.memset(acc[:, 0:1], 0.0), writes=["shacc"])
                    k.op("pool", lambda e, PCH=PCH: e.tensor_copy(out=acc[:, 1:CTX], in_=PCH[:, 0:CTX - 1]),
                         reads=[pk], writes=["shacc"])
                    k.op("dve", lambda e, PCH=PCH: e.tensor_tensor(out=acc[:, 0:CTX - 1], in0=acc[:, 0:CTX - 1],
                                                                  in1=PCH[:, 1:CTX], op=ALU.add),
                         reads=[pk, "shacc"], writes=["shacc"])
                    msc = mu05
                    mk = "mu05"
                k.op("dve", lambda e, msc=msc, oc=oc, ntok=ntok: e.tensor_scalar(
                    out=mch[:, 0:ntok], in0=acc[:, 0:ntok], scalar1=msc[:, oc:oc + 1], scalar2=None, op0=ALU.mult),
                    reads=["shacc", mk], writes=["mch"])
                k.op("dve", lambda e, PCH=PCH, oc=oc, ntok=ntok: e.scalar_tensor_tensor(
                    out=mch[:, 0:ntok], in0=PCH[:, 0:ntok], scalar=omm[:, oc:oc + 1], in1=mch[:, 0:ntok],
                    op0=ALU.mult, op1=ALU.add),
                    reads=[pk, "omm", "mch"], writes=["mch"])
                k.dma("sp", lambda e, dst=dst, ntok=ntok: e.dma_start(out=dst, in_=mch[:, 0:ntok]),
                      reads=["mch"], writes=["PFM"])


Q_KKNEG, Q_R, Q_V = 0, 1, 2
Q_W, Q_BB, Q_KD = 3, 4, 5


def phase2_prepare(nc, k, PFM, vecs, w2d_d, a2_d, g2_d, bones_d, SC, GT):
    vF = load_fm_vec(k, "vF", vecs.ap().rearrange("a n -> (a n)"), 44)
    W2D = k.sb("W2D", [128, C], F32)
    A2 = k.sb("A2", [128, C], F32)
    G2 = [k.sb("G2_%d" % d, [128, C], F32) for d in range(2)]
    bones = k.sb("bones", [128, 128], F32)
    k.dma("sp", lambda e: e.dma_start(out=W2D[:], in_=w2d_d.ap()), writes=["W2D"])
    k.dma("sp", lambda e: e.dma_start(out=A2[:], in_=a2_d.ap()), writes=["A2"])
    for d in range(2):
        k.dma("sp", lambda e, d=d: e.dma_start(out=G2[d][:], in_=g2_d[d]), writes=["G2_%d" % d])
    k.dma("sp", lambda e: e.dma_start(out=bones[:], in_=bones_d.ap()), writes=["bones"])
    omka = k.sb("omka", [128, 4], F32)
    k.op("dve", lambda e: e.tensor_scalar(out=omka[:], in0=vF[:, 20:24], scalar1=-1.0, scalar2=1.0,
                                          op0=ALU.mult, op1=ALU.add), reads=["vF"], writes=["omka"])
    TB = 512
    mt = [k.sb("mt%d" % i, [128, 16, TB], F32) for i in range(2)]
    ot = [k.sb("ot%d" % i, [128, 11, TB], F32) for i in range(2)]
    twd = k.sb("twd", [128, TB], F32)
    sgd = k.sb("sgd", [128, 2, TB], F32)
    tmp = k.sb("p2tmp", [128, TB], F32)
    tmp2 = k.sb("p2tmp2", [128, TB], F32)
    aic = k.sb("aic", [128, TB], F32)
    pp = [k.ps("pp%d" % i, [128, TB]) for i in range(4)]
    ppi = 0
    bi = 0
    oi = 0
    for b in range(BPC):
        blocks = [(0, CTX)] + [(CTX + i * TB, TB) for i in range(SEQ // TB)]
        for (n0, tb) in blocks:
            M = mt[bi % 2]
            mk = "mt%d" % (bi % 2)
            bi += 1
            for half in range(2):
                k.dma("sp", lambda e, M=M, half=half, n0=n0, tb=tb, b=b: e.dma_start(
                    out=M[:, half * 8:(half + 1) * 8, 0:tb],
                    in_=PFM[b, half * 1024:(half + 1) * 1024, n0:n0 + tb].rearrange("(c p) n -> p c n", p=128)),
                    reads=["PFM"], writes=[mk])
            k.op("act", lambda e, M=M, tb=tb: e.activation(out=twd[:, 0:tb], in_=M[:, 12, 0:tb], func=AF.Tanh),
                 reads=[mk], writes=["twd"])
            k.op("act", lambda e, M=M, tb=tb: e.activation(out=sgd[:, :, 0:tb], in_=M[:, 14:16, 0:tb],
                                                          func=AF.Sigmoid), reads=[mk], writes=["sgd"])
            for hp in range(4):
                O = ot[oi % 2]
                ok = "ot%d" % (oi % 2)
                oi += 1
                kt = M[:, 4 + hp, 0:tb]
                k.op("dve", lambda e, O=O, kt=kt, hp=hp, tb=tb: e.tensor_scalar(
                    out=O[:, 9, 0:tb], in0=kt, scalar1=vF[:, 16 + hp:17 + hp], scalar2=None, op0=ALU.mult),
                    reads=[mk, "vF"], writes=[ok])
                k.op("dve", lambda e, O=O, tb=tb: e.tensor_tensor(out=tmp[:, 0:tb], in0=O[:, 9, 0:tb],
                                                                 in1=O[:, 9, 0:tb], op=ALU.mult),
                     reads=[ok], writes=["p2tmp"])
                P = pp[ppi % 4]
                pk_ = "pp%d" % (ppi % 4)
                ppi += 1
                k.op("pe", lambda e, P=P, tb=tb: e.matmul(P[:, 0:tb], lhsT=bones[:], rhs=tmp[:, 0:tb],
                                                         start=True, stop=True),
                     reads=["bones", "p2tmp"], writes=[pk_])
                k.op("act", lambda e, P=P, tb=tb: e.activation(out=tmp2[:, 0:tb], in_=P[:, 0:tb], func=AF.Sqrt),
                     reads=[pk_], writes=["p2tmp2"])
                k.op("dve", lambda e, tb=tb: e.tensor_scalar(out=tmp2[:, 0:tb], in0=tmp2[:, 0:tb], scalar1=1e-12,
                                                            scalar2=None, op0=ALU.max),
                     reads=["p2tmp2"], writes=["p2tmp2"])
                k.op("dve", lambda e, tb=tb: e.reciprocal(out=tmp2[:, 0:tb], in_=tmp2[:, 0:tb]),
                     reads=["p2tmp2"], writes=["p2tmp2"])
                k.op("dve", lambda e, O=O, tb=tb: e.tensor_tensor(out=O[:, 9, 0:tb], in0=O[:, 9, 0:tb],
                                                                 in1=tmp2[:, 0:tb], op=ALU.mult),
                     reads=[ok, "p2tmp2"], writes=[ok])
                k.op("pool", lambda e, O=O, tb=tb: e.tensor_scalar(out=O[:, Q_KKNEG, 0:tb], in0=O[:, 9, 0:tb],
                                                                  scalar1=-1.0, scalar2=None, op0=ALU.mult),
                     reads=[ok], writes=[ok])
                for d in range(2):
                    P = pp[ppi % 4]
                    pk_ = "pp%d" % (ppi % 4)
                    ppi += 1
                    k.op("pe", lambda e, P=P, d=d, hp=hp, tb=tb: e.matmul(
                        P[:, 0:tb], lhsT=W2D[d * 64:(d + 1) * 64, hp * 128:(hp + 1) * 128],
                        rhs=twd[d * 64:(d + 1) * 64, 0:tb], start=True, stop=True),
                        reads=["W2D", "twd"], writes=[pk_])
                    k.op("act", lambda e, P=P, d=d, hp=hp, tb=tb: e.activation(
                        out=tmp[:, 0:tb], in_=P[:, 0:tb], func=AF.Sigmoid,
                        bias=vF[:, d * 4 + hp:d * 4 + hp + 1]), reads=[pk_, "vF"], writes=["p2tmp"])
                    k.op("act", lambda e, O=O, d=d, tb=tb: e.activation(
                        out=O[:, Q_W + 3 * d, 0:tb], in_=tmp[:, 0:tb], func=AF.Exp, scale=-0.6065306597126334),
                        reads=["p2tmp"], writes=[ok])
                    P = pp[ppi % 4]
                    pk_ = "pp%d" % (ppi % 4)
                    ppi += 1
                    k.op("pe", lambda e, P=P, d=d, hp=hp, tb=tb, M=M: e.matmul(
                        P[:, 0:tb], lhsT=A2[d * 64:(d + 1) * 64, hp * 128:(hp + 1) * 128],
                        rhs=M[d * 64:(d + 1) * 64, 13, 0:tb], start=True, stop=True),
                        reads=["A2", mk], writes=[pk_])
                    k.op("act", lambda e, P=P, d=d, hp=hp, tb=tb: e.activation(
                        out=aic[:, 0:tb], in_=P[:, 0:tb], func=AF.Sigmoid,
                        bias=vF[:, 8 + d * 4 + hp:8 + d * 4 + hp + 1]), reads=[pk_, "vF"], writes=["aic"])
                    k.op("dve", lambda e, O=O, d=d, tb=tb: e.tensor_tensor(
                        out=O[:, Q_BB + 3 * d, 0:tb], in0=O[:, 9, 0:tb], in1=aic[:, 0:tb], op=ALU.mult),
                        reads=[ok, "aic"], writes=[ok])
                    k.op("dve", lambda e, hp=hp, tb=tb: e.tensor_scalar(
                        out=tmp2[:, 0:tb], in0=aic[:, 0:tb], scalar1=vF[:, 20 + hp:21 + hp],
                        scalar2=omka[:, hp:hp + 1], op0=ALU.mult, op1=ALU.add),
                        reads=["aic", "vF", "omka"], writes=["p2tmp2"])
                    k.op("dve", lambda e, O=O, d=d, tb=tb, kt=kt: e.tensor_tensor(
                        out=O[:, Q_KD + 3 * d, 0:tb], in0=kt, in1=tmp2[:, 0:tb], op=ALU.mult),
                        reads=[mk, "p2tmp2"], writes=[ok])
                    if n0 >= CTX:
                        P = pp[ppi % 4]
                        pk_ = "pp%d" % (ppi % 4)
                        ppi += 1
                        k.op("pe", lambda e, P=P, d=d, hp=hp, tb=tb: e.matmul(
                            P[:, 0:tb], lhsT=G2[d][:, hp * 128:(hp + 1) * 128], rhs=sgd[:, d, 0:tb],
                            start=True, stop=True), reads=["G2_%d" % d, "sgd"], writes=[pk_])
                        k.op("act", lambda e, P=P, O=O, d=d, tb=tb: e.activation(
                            out=O[:, 9 + d, 0:tb] if False else O[:, 10, 0:tb], in_=P[:, 0:tb], func=AF.Copy),
                            reads=[pk_], writes=[ok])
                        k.dma("act", lambda e, O=O, b=b, d=d, hp=hp, n0=n0, tb=tb: e.dma_start(
                            out=GT[b, d, hp, :, n0 - CTX:n0 - CTX + tb], in_=O[:, 10, 0:tb]),
                            reads=[ok], writes=["GT"])
                k.dma("sp", lambda e, M=M, b=b, hp=hp, n0=n0, tb=tb: e.dma_start(
                    out=SC[b, Q_R, hp, :, n0:n0 + tb], in_=M[:, hp, 0:tb]), reads=[mk], writes=["SC"])
                k.dma("sp", lambda e, M=M, b=b, hp=hp, n0=n0, tb=tb: e.dma_start(
                    out=SC[b, Q_V, hp, :, n0:n0 + tb], in_=M[:, 8 + hp, 0:tb]), reads=[mk], writes=["SC"])
                for q in (Q_KKNEG, 3, 4, 5, 6, 7, 8):
                    k.dma("sp", lambda e, O=O, b=b, hp=hp, n0=n0, tb=tb, q=q: e.dma_start(
                        out=SC[b, q, hp, :, n0:n0 + tb], in_=O[:, q, 0:tb]), reads=[ok], writes=["SC"])


TC = 64
NSTEP = NV
NSL = 16


def phase3_scan(nc, k, SC, YSr, ident, identb, oh_d, mask_d):
    OH = k.sb("s_oh", [128, TC, 40], BF16)
    MK = k.sb("s_mask", [104, 512], F32)
    k.dma("sp", lambda e: e.dma_start(out=OH[:], in_=oh_d.ap()), writes=["s_oh"])
    k.dma("sp", lambda e: e.dma_start(out=MK[:], in_=mask_d.ap()), writes=["s_mask"])
    zer = k.sb("s_zero", [128, 512], BF16)
    k.op("pool", lambda e: e.memset(zer[:], 0.0), writes=["s_zero"])
    CO = [k.sb("CO%d" % i, [128, 6, 16, TC], F32) for i in range(2)]
    LA = [k.sb("LA%d" % i, [128, TC, 2, 32], BF16) for i in range(2)]
    CS = [k.sb("CSt%d" % i, [128, TC, 2, 104], BF16) for i in range(2)]
    VT = [k.sb("VT%d" % i, [TC, 2, 8, 2, 64], BF16) for i in range(2)]
    VT2 = [k.sb("VT2_%d" % i, [128, 2, 8, 64], BF16) for i in range(2)]
    for i in range(2):
        k.op("pool", lambda e, i=i: e.memset(LA[i][:], 0.0), writes=["LA%d" % i])
        k.op("pool", lambda e, i=i: e.memset(CS[i][:], 0.0), writes=["CSt%d" % i])
    Tbf = [k.sb("Tbf%d" % h, [128, 512], BF16) for h in range(2)]
    CT = [k.sb("CT%d" % h, [104, 128], BF16) for h in range(2)]
    RR = [[k.sb("RR%d_%d" % (h, p), [104, NSL, 512], BF16) for p in range(2)] for h in range(2)]
    PT = [k.ps("PT%d" % h, [128, 512]) for h in range(2)]
    PA = [k.ps("PA%d" % h, [128, 512]) for h in range(2)]
    PC = [k.ps("PC%d" % h, [104, 128], BF16) for h in range(2)]
    PVT = k.ps("PVT", [TC, 4, 128])
    for h in range(2):
        k.op("pe", lambda e, h=h: e.matmul(PT[h][:], lhsT=zer[:, 0:128], rhs=zer[:], start=True, stop=True),
             reads=["s_zero"], writes=["PT%d" % h])
        k.op("pe", lambda e, h=h: e.matmul(PA[h][:], lhsT=zer[:, 0:128], rhs=zer[:], start=True, stop=True),
             reads=["s_zero"], writes=["PA%d" % h])

    def nat_lo(c, d):
        if d == 0:
            return c * TC
        if c < CTX // TC:
            return CTX - TC * (c + 1)
        return CTX + SEQ + CTX - TC * (c + 1)

    def prep_chunk(c, nsteps=TC):
        COt, LAt, CSt, VTt = CO[c % 2], LA[c % 2], CS[c % 2], VT[c % 2]
        ck, lk, sk, vk = "CO%d" % (c % 2), "LA%d" % (c % 2), "CSt%d" % (c % 2), "VT%d" % (c % 2)
        last = (nsteps == 1)
        for q in range(6):
            if last and q != 1:
                continue
            for d in range(2):
                nlo = nat_lo(c, d) if not last else (NV if d == 0 else CTX - 1)
                if last:
                    lo, hi, dst0 = (NV - 1, NV, 0) if d == 0 else (CTX, CTX + 1, 0)
                elif q == 1:
                    if d == 0:
                        lo, hi, dst0 = max(nlo - 1, 0), nlo + TC - 1, (1 if nlo == 0 else 0)
                    else:
                        lo, hi, dst0 = nlo + 1, min(nlo + TC + 1, NV), 0
                else:
                    lo, hi, dst0 = nlo, nlo + TC, 0
                for b in range(BPC):
                    srcq = q if q < 3 else q + 3 * d
                    k.dma("sp", lambda e, COt=COt, q=q, d=d, b=b, srcq=srcq, lo=lo, hi=hi, dst0=dst0: e.dma_start(
                        out=COt[:, q, d * 8 + b * 4:d * 8 + b * 4 + 4, dst0:dst0 + hi - lo],
                        in_=SC[b, srcq, :, :, lo:hi].rearrange("g p t -> p g t"),
                        allow_slow_non_contiguous=True),
                        reads=["SC"], writes=[ck])
        for h in range(2):
            for hh in range(2):
                ps = slice(hh * 64, (hh + 1) * 64)
                if not last:
                    k.op("pool", lambda e, h=h, hh=hh, ps=ps, COt=COt, LAt=LAt: e.tensor_copy(
                        out=LAt[ps, :, h, hh:16:2], in_=COt[ps, 0, h * 8:(h + 1) * 8, :].rearrange("p g t -> p t g")),
                        reads=[ck], writes=[lk])
                k.op("pool", lambda e, h=h, hh=hh, ps=ps, COt=COt, LAt=LAt: e.tensor_copy(
                    out=LAt[ps, :, h, 16 + hh:32:2], in_=COt[ps, 1, h * 8:(h + 1) * 8, :].rearrange("p g t -> p t g")),
                    reads=[ck], writes=[lk])
                if last:
                    continue
                k.op("pool", lambda e, h=h, hh=hh, ps=ps, COt=COt, CSt=CSt: e.tensor_copy(
                    out=CSt[ps, :, h, hh:16:2], in_=COt[ps, 4, h * 8:(h + 1) * 8, :].rearrange("p g t -> p t g")),
                    reads=[ck], writes=[sk])
                k.op("pool", lambda e, h=h, hh=hh, ps=ps, COt=COt, CSt=CSt: e.tensor_copy(
                    out=CSt[ps, :, h, 64 + 32 * hh:72 + 32 * hh],
                    in_=COt[ps, 5, h * 8:(h + 1) * 8, :].rearrange("p g t -> p t g")),
                    reads=[ck], writes=[sk])
        if last:
            return
        for h in range(2):
            for half in range(2):
                for gg in range(4):
                    g = h * 8 + half * 4 + gg
                    k.op("pe", lambda e, COt=COt, g=g, gg=gg: e.transpose(out=PVT[:, gg, :], in_=COt[:, 2, g, :],
                                                                          identity=ident[:]),
                         reads=[ck, "ident"], writes=["PVT"])
                k.op("act", lambda e, VTt=VTt, h=h, half=half: e.activation(
                    out=VTt[:, h, half * 4:(half + 1) * 4, :, :].rearrange("t g a i -> t g (a i)"), in_=PVT[:],
                    func=AF.Copy), reads=["PVT"], writes=[vk])
        V2t = VT2[c % 2]
        for hh in range(2):
            k.dma("act", lambda e, VTt=VTt, V2t=V2t, hh=hh: e.dma_start(
                out=V2t[hh * 64:(hh + 1) * 64, :, :, :], in_=VTt[:, :, :, hh, :]),
                reads=[vk], writes=["VT2_%d" % (c % 2)])

    def w_ap(COt, h, tau):
        base = COt[:]
        pstep = base.ap[0][0]
        off = base.offset + 3 * 16 * TC + h * 8 * TC + tau
        return bass.AP(base.tensor, off, [[pstep, 128], [TC, 8], [0, 64]])

    nchunk = NSTEP // TC
    prep_chunk(0)
    for c in range(nchunk + 1):
        last = (c == nchunk)
        if c + 1 <= nchunk:
            prep_chunk(c + 1, TC if c + 1 < nchunk else 1)
        COt, LAt, CSt, VTt = CO[c % 2], LA[c % 2], CS[c % 2], VT[c % 2]
        ck, lk, sk, vk = "CO%d" % (c % 2), "LA%d" % (c % 2), "CSt%d" % (c % 2), "VT%d" % (c % 2)
        V2t, v2k = VT2[c % 2], "VT2_%d" % (c % 2)
        for j in range(1 if last else TC):
            s_ = c * TC + j
            ls = s_ - 1 - CTX
            par = (ls // NSL) % 2 if ls >= 0 else 0
            slot = ls % NSL if ls >= 0 else 0
            taus = [0, 0] if last else [j, TC - 1 - j]
            Rs = [RR[h][par] for h in range(2)]
            rks = ["RR%d_%d" % (h, par) for h in range(2)]
            for h in range(2):
                k.op("act", lambda e, h=h: e.activation(out=Tbf[h][:], in_=PT[h][:], func=AF.Copy),
                     reads=["PT%d" % h], writes=["Tbf%d" % h])
            for h in range(2):
                tau = taus[h]
                if not last:
                    k.op("pe", lambda e, h=h, tau=tau, V2t=V2t: e.matmul(
                        PA[h][64:104, :], lhsT=OH[:, tau, :], rhs=V2t[:, h, :, :], start=True, stop=True),
                        reads=["s_oh", v2k], writes=["PA%d" % h])
                    k.op("pe", lambda e, h=h, tau=tau, CSt=CSt: e.transpose(out=PC[h][:], in_=CSt[:, tau, h, :],
                                                                           identity=identb[:]),
                         reads=[sk, "identb"], writes=["PC%d" % h])
                k.op("pe", lambda e, h=h, tau=tau, LAt=LAt: e.matmul(PA[h][0:32, :], lhsT=LAt[:, tau, h, :],
                                                                    rhs=Tbf[h][:], start=True, stop=True),
                     reads=[lk, "Tbf%d" % h], writes=["PA%d" % h])
            if not last:
                for h in range(2):
                    k.op("act", lambda e, h=h: e.activation(out=CT[h][:], in_=PC[h][:], func=AF.Copy),
                         reads=["PC%d" % h], writes=["CT%d" % h])
                for h in range(2):
                    tau = taus[h]
                    k.op("dve", lambda e, h=h, tau=tau, COt=COt: e.tensor_tensor(
                        out=PT[h][:].rearrange("p (g i) -> p g i", g=8), in0=PT[h][:].rearrange("p (g i) -> p g i", g=8),
                        in1=w_ap(COt, h, tau), op=ALU.mult), reads=["PT%d" % h, ck], writes=["PT%d" % h])
            for h in range(2):
                k.op("dve", lambda e, h=h, R=Rs[h], slot=slot: e.tensor_tensor(out=R[:, slot, :], in0=PA[h][0:104, :],
                                                                              in1=MK[:], op=ALU.mult),
                     reads=["PA%d" % h, "s_mask"], writes=[rks[h]])
            if not last:
                for h in range(2):
                    k.op("pe", lambda e, h=h, R=Rs[h], slot=slot: e.matmul(PT[h][:], lhsT=CT[h][:], rhs=R[:, slot, :],
                                                                          start=False, stop=True),
                         reads=["CT%d" % h, rks[h], "PT%d" % h], writes=["PT%d" % h])
            if ls >= 0 and slot == NSL - 1:
                for h in range(2):
                    for gl in range(8):
                        g = h * 8 + gl
                        k.dma("sp", lambda e, R=Rs[h], gl=gl, g=g, ls=ls: e.dma_start(
                            out=YSr[:, ls - NSL + 1:ls + 1, g * 64:(g + 1) * 64],
                            in_=R[16 + gl * 2:18 + gl * 2, :, gl * 64:(gl + 1) * 64]),
                            reads=[rks[h]], writes=["YSr"])


def phase4_rwkv_out(nc, k, SC, GT, YSr, vecs, bones_d, ident, jmat_d, MIXT):
    vF = load_fm_vec(k, "vF4", vecs.ap().rearrange("a n -> (a n)"), 44)
    bones = k.sb("bones4", [128, 128], F32)
    jmat = k.sb("jmat", [128, 128], F32)
    k.dma("sp", lambda e: e.dma_start(out=bones[:], in_=bones_d.ap()), writes=["bones4"])
    k.dma("sp", lambda e: e.dma_start(out=jmat[:], in_=jmat_d.ap()), writes=["jmat"])
    TB = 512
    Yt = [k.sb("Yt%d" % i, [128, 2, 64], BF16) for i in range(4)]
    identb4 = k.sb("identb4", [128, 128], BF16)
    jmatb = k.sb("jmatb", [128, 128], BF16)
    k.op("pool", lambda e: e.tensor_copy(out=identb4[:], in_=ident[:]), reads=["ident"], writes=["identb4"])
    k.op("pool", lambda e: e.tensor_copy(out=jmatb[:], in_=jmat[:]), reads=["jmat"], writes=["jmatb"])
    rt = k.sb("r4", [128, TB], F32)
    vt = k.sb("v4", [128, TB], F32)
    kdt = [k.sb("kd4_%d" % i, [128, TB], F32) for i in range(2)]
    gt = [k.sb("g4_%d" % i, [128, TB], F32) for i in range(2)]
    yF = k.sb("yF", [128, TB], F32)
    ym = k.sb("ym", [128, TB], F32)
    sq = k.sb("sq4", [128, TB], F32)
    rs = k.sb("rs4", [128, TB], F32)
    t1 = k.sb("t14", [128, TB], F32)
    acc = k.sb("acc4", [128, TB], F32)
    rwbf = [k.sb("rwbf%d" % i, [128, TB], BF16) for i in range(2)]
    PSY = k.ps("PSY", [128, TB])
    PM = k.ps("PM4", [128, TB])
    PV_ = k.ps("PV4", [128, TB])
    PB = k.ps("PB4", [128, TB])
    yi = 0
    oi = 0
    for b in range(BPC):
        for hp in range(4):
            for blk in range(SEQ // TB):
                n0 = blk * TB
                k.dma("sp", lambda e, b=b, hp=hp, n0=n0: e.dma_start(
                    out=rt[:], in_=SC[b, Q_R, hp, :, CTX + n0:CTX + n0 + TB]), reads=["SC"], writes=["r4"])
                k.dma("sp", lambda e, b=b, hp=hp, n0=n0: e.dma_start(
                    out=vt[:], in_=SC[b, Q_V, hp, :, CTX + n0:CTX + n0 + TB]), reads=["SC"], writes=["v4"])
                for d in range(2):
                    g = d * 8 + b * 4 + hp
                    KD, GTt = kdt[d], gt[d]
                    k.dma("sp", lambda e, b=b, hp=hp, n0=n0, d=d, KD=KD: e.dma_start(
                        out=KD[:], in_=SC[b, Q_KD + 3 * d, hp, :, CTX + n0:CTX + n0 + TB]),
                        reads=["SC"], writes=["kd4_%d" % d])
                    k.dma("sp", lambda e, b=b, hp=hp, n0=n0, d=d, GTt=GTt: e.dma_start(
                        out=GTt[:], in_=GT[b, d, hp, :, n0:n0 + TB]), reads=["GT"], writes=["g4_%d" % d])
                    for sub in range(4):
                        nn = n0 + sub * 128
                        ls0 = nn if d == 0 else SEQ - 128 - nn
                        Y = Yt[yi % 4]
                        yk = "Yt%d" % (yi % 4)
                        yi += 1
                        for hh in range(2):
                            k.dma("act", lambda e, Y=Y, hh=hh, ls0=ls0, g=g: e.dma_start(
                                out=Y[:, hh, :], in_=YSr[hh, ls0:ls0 + 128, g * 64:(g + 1) * 64]),
                                reads=["YSr"], writes=[yk])
                        rm, rmk = (identb4, "identb4") if d == 0 else (jmatb, "jmatb")
                        k.op("pe", lambda e, Y=Y, sub=sub, rm=rm: e.matmul(
                            PSY[:, sub * 128:(sub + 1) * 128], lhsT=Y[:].rearrange("p h i -> p (h i)"),
                            rhs=rm[:], start=True, stop=True), reads=[yk, rmk], writes=["PSY"])
                    k.op("act", lambda e: e.activation(out=yF[:], in_=PSY[:], func=AF.Copy),
                         reads=["PSY"], writes=["yF"])
                    k.op("pe", lambda e: e.matmul(PM[:], lhsT=bones[:], rhs=yF[:], start=True, stop=True),
                         reads=["bones4", "yF"], writes=["PM4"])
                    k.op("dve", lambda e: e.scalar_tensor_tensor(out=ym[:], in0=PM[:], scalar=-1.0 / 64, in1=yF[:],
                                                                 op0=ALU.mult, op1=ALU.add),
                         reads=["PM4", "yF"], writes=["ym"])
                    k.op("pool", lambda e: e.tensor_tensor(out=sq[:], in0=ym[:], in1=ym[:], op=ALU.mult),
                         reads=["ym"], writes=["sq4"])
                    k.op("pe", lambda e: e.matmul(PV_[:], lhsT=bones[:], rhs=sq[:], start=True, stop=True),
                         reads=["bones4", "sq4"], writes=["PV4"])
                    k.op("act", lambda e: e.activation(out=rs[:], in_=PV_[:], func=AF.Sqrt, scale=1.0 / 64,
                                                       bias=GN_EPS), reads=["PV4"], writes=["rs4"])
                    k.op("dve", lambda e: e.reciprocal(out=rs[:], in_=rs[:]), reads=["rs4"], writes=["rs4"])
                    k.op("dve", lambda e: e.tensor_tensor(out=ym[:], in0=ym[:], in1=rs[:], op=ALU.mult),
                         reads=["ym", "rs4"], writes=["ym"])
                    k.op("dve", lambda e, hp=hp: e.tensor_scalar(out=ym[:], in0=ym[:], scalar1=vF[:, 24 + hp:25 + hp],
                                                                scalar2=vF[:, 28 + hp:29 + hp], op0=ALU.mult,
                                                                op1=ALU.add), reads=["ym", "vF4"], writes=["ym"])
                    k.op("pool", lambda e, KD=KD: e.tensor_tensor(out=t1[:], in0=rt[:], in1=KD[:], op=ALU.mult),
                         reads=["r4", "kd4_%d" % d], writes=["t14"])
                    k.op("pool", lambda e, d=d, hp=hp: e.tensor_scalar(
                        out=t1[:], in0=t1[:], scalar1=vF[:, 32 + d * 4 + hp:33 + d * 4 + hp], scalar2=None,
                        op0=ALU.mult), reads=["t14", "vF4"], writes=["t14"])
                    k.op("pe", lambda e: e.matmul(PB[:], lhsT=bones[:], rhs=t1[:], start=True, stop=True),
                         reads=["bones4", "t14"], writes=["PB4"])
                    k.op("dve", lambda e: e.tensor_tensor(out=t1[:], in0=PB[:], in1=vt[:], op=ALU.mult),
                         reads=["PB4", "v4", "t14"], writes=["t14"])
                    k.op("dve", lambda e: e.tensor_tensor(out=ym[:], in0=ym[:], in1=t1[:], op=ALU.add),
                         reads=["ym", "t14"], writes=["ym"])
                    if d == 0:
                        k.op("dve", lambda e, GTt=GTt: e.tensor_tensor(out=acc[:], in0=ym[:], in1=GTt[:], op=ALU.mult),
                             reads=["ym", "g4_0"], writes=["acc4"])
                    else:
                        k.op("dve", lambda e, GTt=GTt: e.tensor_tensor(out=ym[:], in0=ym[:], in1=GTt[:], op=ALU.mult),
                             reads=["ym", "g4_1"], writes=["ym"])
                        RW = rwbf[oi % 2]
                        rk_ = "rwbf%d" % (oi % 2)
                        oi += 1
                        k.op("dve", lambda e, RW=RW: e.tensor_tensor(out=RW[:], in0=acc[:], in1=ym[:], op=ALU.add),
                             reads=["acc4", "ym"], writes=[rk_])
                        k.dma("sp", lambda e, RW=RW, b=b, hp=hp, n0=n0: e.dma_start(
                            out=MIXT[b, hp, :, n0:n0 + TB], in_=RW[:]), reads=[rk_], writes=["MIXT"])


def phase5_fft(nc, k, PFM, wfno_d, vecs, ccbd_d, scbd_d, cosm_d, nsin_d, MIXT):
    vF = load_fm_vec(k, "vF5", vecs.ap().rearrange("a n -> (a n)"), 44)
    ccbd = k.sb("ccbd", [128, 128], F32)
    scbd = k.sb("scbd", [128, 128], F32)
    k.dma("sp", lambda e: e.dma_start(out=ccbd[:], in_=ccbd_d.ap()), writes=["ccbd"])
    k.dma("sp", lambda e: e.dma_start(out=scbd[:], in_=scbd_d.ap()), writes=["scbd"])
    wst = k.sb("wst", [128, 4, 128], F32)
    k.op("pool", lambda e: e.memset(wst[:], 0.0), writes=["wst"])
    for fc in range(4):
        for g2 in range(2):
            k.dma("sp", lambda e, fc=fc, g2=g2: e.dma_start(
                out=wst[g2 * 64:(g2 + 1) * 64, fc, g2 * 64:(g2 + 1) * 64], in_=wfno_d[fc * 2 + g2]),
                writes=["wst"])
    ABD = k.sb("ABD", [128, 4, 2, 128], BF16)
    PA = k.ps("PA5", [128, 2, 128])
    for fc in range(4):
        k.op("pe", lambda e, fc=fc: e.matmul(PA[:, 0, :], lhsT=ccbd[:], rhs=wst[:, fc, :], start=True, stop=True),
             reads=["ccbd", "wst"], writes=["PA5"])
        k.op("pe", lambda e, fc=fc: e.matmul(PA[:, 1, :], lhsT=scbd[:], rhs=wst[:, fc, :], start=True, stop=True),
             reads=["scbd", "wst"], writes=["PA5"])
        k.op("act", lambda e, fc=fc: e.activation(out=ABD[:, fc, :, :], in_=PA[:], func=AF.Copy),
             reads=["PA5"], writes=["ABD"])
    U = k.sb("U5", [128, 32, 2, 512], BF16)
    fbf = [k.sb("fbf%d" % i, [128, SEQ], BF16) for i in range(2)]
    CS = k.sb("CS5", [128, 2, 32, 512], BF16)
    PU = [k.ps("PU5_%d" % i, [128, 2, 128]) for i in range(2)]
    PO = [k.ps("PO5_%d" % i, [128, 512]) for i in range(2)]
    ob = [k.sb("ob5_%d" % i, [128, 512], BF16) for i in range(2)]
    pui = 0
    poi = 0
    for b in range(BPC):
        for fc in range(4):
            FB = fbf[fc % 2]
            fk = "fbf%d" % (fc % 2)
            for hf in range(2):
                k.dma("pool", lambda e, FB=FB, b=b, fc=fc, hf=hf: e.dma_start(
                    out=FB[:, hf * 2048:(hf + 1) * 2048],
                    in_=PFM[b, SHW + fc * 128:SHW + (fc + 1) * 128, CTX + hf * 2048:CTX + (hf + 1) * 2048]),
                    reads=["PFM"], writes=[fk])
            for tt in range(32):
                P = PU[pui % 2]
                pk_ = "PU5_%d" % (pui % 2)
                pui += 1
                for ab in range(2):
                    k.op("pe", lambda e, P=P, FB=FB, tt=tt, fc=fc, ab=ab: e.matmul(
                        P[:, ab, :], lhsT=FB[:, tt * 128:(tt + 1) * 128], rhs=ABD[:, fc, ab, :],
                        start=True, stop=True), reads=[fk, "ABD"], writes=[pk_])
                k.op("act" if tt % 2 else "dve", (lambda e, P=P, tt=tt, fc=fc: e.activation(
                    out=U[:, tt, :, fc * 128:(fc + 1) * 128], in_=P[:], func=AF.Copy)) if tt % 2 else
                    (lambda e, P=P, tt=tt, fc=fc: e.tensor_copy(out=U[:, tt, :, fc * 128:(fc + 1) * 128], in_=P[:])),
                    reads=[pk_], writes=["U5"])
        for kb in range(8):
            for hf in range(2):
                k.dma("sp", lambda e, kb=kb, hf=hf: e.dma_start(
                    out=CS[:, 0, hf * 16:(hf + 1) * 16, :].rearrange("p t n -> p (t n)"),
                    in_=cosm_d[kb, :, hf * 8192:(hf + 1) * 8192]), writes=["CS5"])
                k.dma("act", lambda e, kb=kb, hf=hf: e.dma_start(
                    out=CS[:, 1, hf * 16:(hf + 1) * 16, :].rearrange("p t n -> p (t n)"),
                    in_=nsin_d[kb, :, hf * 8192:(hf + 1) * 8192]), writes=["CS5"])
            for fc in range(4):
                P = PO[poi % 2]
                pk_ = "PO5_%d" % (poi % 2)
                OB = ob[poi % 2]
                obk = "ob5_%d" % (poi % 2)
                poi += 1
                for tt in range(32):
                    for ab in range(2):
                        k.op("pe", lambda e, P=P, tt=tt, ab=ab, fc=fc: e.matmul(
                            P[:], lhsT=U[:, tt, ab, fc * 128:(fc + 1) * 128], rhs=CS[:, ab, tt, :],
                            start=(tt == 0 and ab == 0), stop=(tt == 31 and ab == 1)),
                            reads=["U5", "CS5"], writes=[pk_])
                k.op("act", lambda e, P=P, OB=OB, fc=fc: e.activation(
                    out=OB[:], in_=P[:], func=AF.Identity, bias=vF[:, 40 + fc:41 + fc]),
                    reads=[pk_, "vF5"], writes=[obk])
                k.dma("sp", lambda e, OB=OB, b=b, fc=fc, kb=kb: e.dma_start(
                    out=MIXT[b, 4 + fc, :, kb * 512:(kb + 1) * 512], in_=OB[:]), reads=[obk], writes=["MIXT"])


def bcast_rows(ap1d, n, parts=128):
    return bass.AP(ap1d.tensor, ap1d.offset, [[0, parts], [1, n]])


def ln_stats(k, src, srck, st, mv, rstd, sfx):
    for hf in range(2):
        k.op("dve", lambda e, hf=hf: e.bn_stats(out=st[:, hf, :], in_=src[:, hf * 512:(hf + 1) * 512]),
             reads=[srck], writes=["bnst" + sfx])
    k.op("dve", lambda e: e.bn_aggr(out=mv[:], in_=st[:].rearrange("p a b -> p (a b)")),
         reads=["bnst" + sfx], writes=["bnmv" + sfx])
    k.op("act", lambda e: e.activation(out=rstd[:], in_=mv[:, 1:2], func=AF.Sqrt, bias=LN_EPS),
         reads=["bnmv" + sfx], writes=["rstd" + sfx])
    k.op("dve", lambda e: e.reciprocal(out=rstd[:], in_=rstd[:]), reads=["rstd" + sfx], writes=["rstd" + sfx])


def phase6_outproj(nc, k, x, ln0, ln1, MOD, w_out, w_router, b_router, MIXT, ident, X1, H2T, GTd, GD=None, sp=None):
    def brow(name, ap1d, n=D, q="sp"):
        t = k.sb(name, [128, n], F32)
        k.dma(q, lambda e: e.dma_start(out=t[:], in_=bcast_rows(ap1d, n)), reads=["MOD"], writes=[name])
        return t
    g0B = brow("g0B", ln0[0])
    b0B = brow("b0B", ln0[1])
    g1B = brow("g1B", ln1[0])
    b1B = brow("b1B", ln1[1])
    brB = brow("brB", b_router[0], NE)
    wobf = k.sb("wobf", [128, 8, D], BF16)
    for kc in range(8):
        k.dma("pool", lambda e, kc=kc: e.dma_start(out=wobf[:, kc, :], in_=w_out[kc * 128:(kc + 1) * 128, :]),
              writes=["wobf"])
    wr = k.sb("wr", [128, 8, NE], F32)
    k.dma("sp", lambda e: e.dma_start(out=wr[:], in_=w_router.ap().rearrange("(c p) n -> p c n", p=128)),
          writes=["wr"])
    Xt = [k.sb("x6_%d" % i, [128, D], F32) for i in range(2)]
    MX = [k.sb("mx6_%d" % i, [128, 8, 128], BF16) for i in range(2)]
    x0 = k.sb("x0_6", [128, D], F32)
    z = k.sb("z6", [128, D], F32)
    x1 = [k.sb("x1_6_%d" % i, [128, D], F32) for i in range(2)]
    h2 = k.sb("h2_6", [128, D], F32)
    h2T = k.sb("h2T6", [128, 8, 128], F32)
    h2Tb = [k.sb("h2Tb6_%d" % i, [128, 8, 128], BF16) for i in range(2)]
    st = k.sb("bnst6", [128, 2, 6], F32)
    mv = k.sb("bnmv6", [128, 2], F32)
    rstd = k.sb("rstd6", [128, 1], F32)
    lg = k.sb("lg6", [128, NE], F32)
    mx8 = k.sb("mx8", [128, 8], F32)
    nmx = k.sb("nmx", [128, 1], F32)
    msk = k.sb("msk6", [128, NE], F32)
    ex = k.sb("ex6", [128, NE], F32)
    ssum = k.sb("ssum6", [128, 1], F32)
    G = k.sb("G6", [128, NE], F32)
    gTs = [k.sb("gTs6_%d" % i, [NE, 128], F32) for i in range(2)]
    PO = k.ps("PO6", [128, D])
    PT = k.ps("PT6", [128, 8, 128])
    PL = k.ps("PL6", [128, NE])
    PG = k.ps("PG6", [NE, 128])
    if sp is not None:
        ustr = k.sb("ustr", [128, 128], F32)
        k.dma("sp", lambda e: e.dma_start(out=ustr[:], in_=sp["ustr_d"].ap()), writes=["ustr"])
        ones6 = k.sb("ones6", [128, 128], F32)
        k.op("pool", lambda e: e.memset(ones6[:], 1.0), writes=["ones6"])
        carry = sp["carry"]
        k.op("pool", lambda e: e.memset(carry[:], 0.0), writes=["carry"])
        rk6 = [k.sb("rk6_%d" % i, [128, NE], F32) for i in range(2)]
        h2b = [k.sb("h2b6_%d" % i, [128, D], BF16) for i in range(2)]
        PR = k.ps("PR6", [128, NE])
        PCn = k.ps("PCn6", [1, NE])
    ti = 0
    for b in range(BPC):
        gt1B = brow("gt1B%d" % b, MOD[b, 2048:3072])
        sc2B = brow("sc2B%d" % b, MOD[b, 4096:5120])
        sh2B = brow("sh2B%d" % b, MOD[b, 3072:4096])
        k.op("dve", lambda e, sc2B=sc2B: e.tensor_scalar(out=sc2B[:], in0=sc2B[:], scalar1=1.0, scalar2=None,
                                                        op0=ALU.add), reads=["sc2B%d" % b], writes=["sc2B%d" % b])
        for t in range(NT6):
            X, M_, X1t, HB, GTS = Xt[ti % 2], MX[ti % 2], x1[ti % 2], h2Tb[ti % 2], gTs[ti % 2]
            xk, mk, x1k, hbk, gtk = ("x6_%d" % (ti % 2), "mx6_%d" % (ti % 2), "x1_6_%d" % (ti % 2),
                                     "h2Tb6_%d" % (ti % 2), "gTs6_%d" % (ti % 2))
            ti += 1
            tok0 = t * 128
            k.dma("sp", lambda e, X=X, b=b, tok0=tok0: e.dma_start(out=X[:], in_=x[b, tok0:tok0 + 128, :]),
                  writes=[xk])
            k.dma("act", lambda e, M_=M_, b=b, tok0=tok0: e.dma_start(
                out=M_[:], in_=MIXT[b, :, :, tok0:tok0 + 128].rearrange("c p n -> p c n")),
                reads=["MIXT"], writes=[mk])
            ln_stats(k, X, xk, st, mv, rstd, "6")
            k.op("dve", lambda e, X=X: e.tensor_scalar(out=x0[:], in0=X[:], scalar1=mv[:, 0:1], scalar2=rstd[:, 0:1],
                                                      op0=ALU.subtract, op1=ALU.mult),
                 reads=[xk, "bnmv6", "rstd6"], writes=["x0_6"])
            k.op("pool", lambda e: e.tensor_tensor(out=x0[:], in0=x0[:], in1=g0B[:], op=ALU.mult),
                 reads=["x0_6", "g0B"], writes=["x0_6"])
            k.op("pool", lambda e: e.tensor_tensor(out=x0[:], in0=x0[:], in1=b0B[:], op=ALU.add),
                 reads=["x0_6", "b0B"], writes=["x0_6"])
            if STOP6 <= 1:
                continue
            for hf in range(2):
                for kc in range(8):
                    k.op("pe", lambda e, M_=M_, hf=hf, kc=kc: e.matmul(
                        PO[:, hf * 512:(hf + 1) * 512], lhsT=M_[:, kc, :], rhs=wobf[:, kc, hf * 512:(hf + 1) * 512],
                        start=(kc == 0), stop=(kc == 7)), reads=[mk, "wobf"], writes=["PO6"])
            k.op("dve", lambda e, gt1B=gt1B: e.tensor_tensor(out=z[:], in0=PO[:], in1=gt1B[:], op=ALU.mult),
                 reads=["PO6", "gt1B%d" % b], writes=["z6"])
            k.op("dve", lambda e: e.scalar_tensor_tensor(out=z[:], in0=x0[:], scalar=ALPHA, in1=z[:],
                                                         op0=ALU.mult, op1=ALU.add),
                 reads=["x0_6", "z6"], writes=["z6"])
            ln_stats(k, z, "z6", st, mv, rstd, "6")
            k.op("dve", lambda e: e.tensor_scalar(out=z[:], in0=z[:], scalar1=mv[:, 0:1], scalar2=rstd[:, 0:1],
                                                  op0=ALU.subtract, op1=ALU.mult),
                 reads=["z6", "bnmv6", "rstd6"], writes=["z6"])
            k.op("pool", lambda e: e.tensor_tensor(out=z[:], in0=z[:], in1=g1B[:], op=ALU.mult),
                 reads=["z6", "g1B"], writes=["z6"])
            k.op("pool", lambda e, X1t=X1t: e.tensor_tensor(out=X1t[:], in0=z[:], in1=b1B[:], op=ALU.add),
                 reads=["z6", "b1B"], writes=[x1k])
            k.dma("sp", lambda e, X1t=X1t, b=b, tok0=tok0: e.dma_start(out=X1[b, tok0:tok0 + 128, :], in_=X1t[:]),
                  reads=[x1k], writes=["X1"])
            if STOP6 <= 2:
                continue
            k.op("pool", lambda e, X1t=X1t, sc2B=sc2B: e.tensor_tensor(out=h2[:], in0=X1t[:], in1=sc2B[:], op=ALU.mult),
                 reads=[x1k, "sc2B%d" % b], writes=["h2_6"])
            k.op("dve", lambda e, sh2B=sh2B: e.tensor_tensor(out=h2[:], in0=h2[:], in1=sh2B[:], op=ALU.add),
                 reads=["h2_6", "sh2B%d" % b], writes=["h2_6"])
            for kc in range(8):
                k.op("pe", lambda e, kc=kc: e.transpose(out=PT[:, kc, :], in_=h2[:, kc * 128:(kc + 1) * 128],
                                                        identity=ident[:]),
                     reads=["h2_6", "ident"], writes=["PT6"])
            if STOP6 <= 2.3:
                continue
            k.op("act", lambda e: e.activation(out=h2T[:], in_=PT[:], func=AF.Copy), reads=["PT6"], writes=["h2T6"])
            if STOP6 <= 2.5:
                continue
            for hb_ in range(2):
                k.op("pool", lambda e, HB=HB, hb_=hb_: e.tensor_copy(out=HB[:, hb_ * 4:(hb_ + 1) * 4, :],
                                                                     in_=h2T[:, hb_ * 4:(hb_ + 1) * 4, :]),
                     reads=["h2T6"], writes=[hbk])
            if STOP6 <= 2.7:
                continue
            k.dma("act", lambda e, HB=HB, b=b, tok0=tok0: e.dma_start(
                out=H2T[b, :, :, tok0:tok0 + 128].rearrange("c p n -> p c n"), in_=HB[:]),
                reads=[hbk], writes=["H2T"])
            if STOP6 <= 3:
                continue
            for kc in range(8):
                k.op("pe", lambda e, kc=kc: e.matmul(PL[:], lhsT=h2T[:, kc, :], rhs=wr[:, kc, :],
                                                     start=(kc == 0), stop=(kc == 7)),
                     reads=["h2T6", "wr"], writes=["PL6"])
            k.op("dve", lambda e: e.tensor_tensor(out=lg[:], in0=PL[:], in1=brB[:], op=ALU.add),
                 reads=["PL6", "brB"], writes=["lg6"])
            if STOP6 <= 4:
                continue
            k.op("dve", lambda e: e.max(out=mx8[:], in_=lg[:]), reads=["lg6"], writes=["mx8"])
            k.op("dve", lambda e: e.tensor_scalar(out=nmx[:], in0=mx8[:, 0:1], scalar1=-1.0, scalar2=None,
                                                  op0=ALU.mult), reads=["mx8"], writes=["nmx"])
            k.op("dve", lambda e: e.tensor_scalar(out=msk[:], in0=lg[:], scalar1=mx8[:, 3:4], scalar2=None,
                                                  op0=ALU.is_ge), reads=["lg6", "mx8"], writes=["msk6"])
            k.op("act", lambda e: e.activation(out=ex[:], in_=lg[:], func=AF.Exp, bias=nmx[:, 0:1]),
                 reads=["lg6", "nmx"], writes=["ex6"])
            k.op("dve", lambda e: e.tensor_tensor(out=ex[:], in0=ex[:], in1=msk[:], op=ALU.mult),
                 reads=["ex6", "msk6"], writes=["ex6"])
            k.op("dve", lambda e: e.reduce_sum(out=ssum[:], in_=ex[:], axis=AX.X), reads=["ex6"], writes=["ssum6"])
            k.op("dve", lambda e: e.reciprocal(out=ssum[:], in_=ssum[:]), reads=["ssum6"], writes=["ssum6"])
            k.op("dve", lambda e: e.tensor_scalar(out=G[:], in0=ex[:], scalar1=ssum[:, 0:1], scalar2=None,
                                                  op0=ALU.mult), reads=["ex6", "ssum6"], writes=["G6"])
            if STOP6 <= 5:
                continue
            if sp is not None:
                tgs = b * SEQ + tok0
                RK, H2B = rk6[ti % 2], h2b[ti % 2]
                rkk, h2k = "rk6_%d" % (ti % 2), "h2b6_%d" % (ti % 2)
                k.op("pe", lambda e: e.matmul(PR[:], lhsT=ustr[:], rhs=msk[:], start=True, stop=False),
                     reads=["ustr", "msk6"], writes=["PR6"])
                k.op("pe", lambda e: e.matmul(PR[:], lhsT=ones6[0:1, :], rhs=carry[0:1, :], start=False, stop=True),
                     reads=["ones6", "carry"], writes=["PR6"])
                k.op("pe", lambda e: e.matmul(PCn[:], lhsT=ones6[:, 0:1], rhs=msk[:], start=True, stop=True),
                     reads=["ones6", "msk6"], writes=["PCn6"])
                k.op("act", lambda e, RK=RK: e.activation(out=RK[:], in_=PR[:], func=AF.Copy),
                     reads=["PR6"], writes=[rkk])
                k.op("dve", lambda e: e.tensor_tensor(out=carry[:], in0=PCn[:], in1=carry[:], op=ALU.add),
                     reads=["PCn6", "carry"], writes=["carry"])
                k.dma("sp", lambda e, RK=RK, tgs=tgs: e.dma_start(out=sp["RANKD"][tgs:tgs + 128, :], in_=RK[:]),
                      reads=[rkk], writes=["RANKD"])
                k.dma("sp", lambda e, tgs=tgs: e.dma_start(out=sp["MSKD"][tgs:tgs + 128, :], in_=msk[:]),
                      reads=["msk6"], writes=["MSKD"])
                k.op("pool", lambda e, H2B=H2B: e.tensor_copy(out=H2B[:], in_=h2[:]), reads=["h2_6"], writes=[h2k])
                k.dma("act", lambda e, H2B=H2B, tgs=tgs: e.dma_start(out=sp["H2D"][tgs:tgs + 128, :], in_=H2B[:]),
                      reads=[h2k], writes=["H2D"])
            if GD is not None:
                tgd = b * SEQ + tok0
                k.dma("sp", lambda e, tgd=tgd: e.dma_start(out=GD[tgd:tgd + 128, :], in_=G[:]),
                      reads=["G6"], writes=["GD"])
            k.op("pe", lambda e: e.transpose(out=PG[:], in_=G[:], identity=ident[:]),
                 reads=["G6", "ident"], writes=["PG6"])
            k.op("act", lambda e, GTS=GTS: e.activation(out=GTS[:], in_=PG[:], func=AF.Copy),
                 reads=["PG6"], writes=[gtk])
            tg0 = b * SEQ + tok0
            k.dma("sp", lambda e, GTS=GTS, tg0=tg0: e.dma_start(out=GTd[:, tg0:tg0 + 128], in_=GTS[:]),
                  reads=[gtk], writes=["GTd"])


def phase7_moe(nc, k, w1, b1, w2, H2T, GD, FD):
    b1GL = k.sb("b1GL", [128, NE, 8, 2], F32)
    for eg in range(NE7):
        k.dma("sp", lambda e, eg=eg: e.dma_start(
            out=b1GL[:, eg, :, :],
            in_=b1[eg, :].rearrange("(m p two) -> p m two", p=128, two=2),
            allow_slow_non_contiguous=True), writes=["b1GL"])
    Gall = k.sb("Gall", [128, BPC * SEQ // 128, NE], F32)
    for t8 in range(8):
        k.dma("sp", lambda e, t8=t8: e.dma_start(
            out=Gall[:, t8 * 8:(t8 + 1) * 8, :],
            in_=GD[t8 * 1024:(t8 + 1) * 1024, :].rearrange("(t p) e -> p t e", p=128)),
            reads=["GD"], writes=["Gall"])
    W1 = [k.sb("W1_%d" % i, [128, 8, 2048], BF16) for i in range(2)]
    W2 = [k.sb("W2_%d" % i, [128, 8, D], BF16) for i in range(2)]
    hT = [k.sb("hT7_%d" % i, [128, 8, 512], BF16) for i in range(2)]
    gb = [k.sb("gb7_%d" % i, [128, 512], F32) for i in range(2)]
    glu = [k.sb("glu7_%d" % i, [128, 512], F32) for i in range(2)]
    sg = [k.sb("sg7_%d" % i, [128, 512], F32) for i in range(2)]
    lin = [k.sb("lin7_%d" % i, [128, 512], F32) for i in range(2)]
    ACTT = [k.sb("ACTT%d" % i, [128, 8, 512], BF16) for i in range(2)]
    yb = [k.sb("yb7_%d" % i, [128, D], F32) for i in range(2)]
    PGL = [k.ps("PGL%d" % i, [128, 512]) for i in range(4)]
    PY = k.ps("PY7", [128, D])
    pi = 0
    bi = 0
    mi = 0
    yi = 0
    for ex in range(NE7):
        W1t, W2t = W1[ex % 2], W2[ex % 2]
        w1k, w2k = "W1_%d" % (ex % 2), "W2_%d" % (ex % 2)
        for kc in range(8):
            k.dma("pool", lambda e, W1t=W1t, ex=ex, kc=kc: e.dma_start(
                out=W1t[:, kc, :], in_=w1[ex, kc * 128:(kc + 1) * 128, :]), writes=[w1k])
        for kc in range(8):
            k.dma("pool", lambda e, W2t=W2t, ex=ex, kc=kc: e.dma_start(
                out=W2t[:, kc, :], in_=w2[ex, kc * 128:(kc + 1) * 128, :]), writes=[w2k])
        for blk in range(NB7):
            b, n0 = divmod(blk * 512, SEQ)
            tg0 = blk * 512
            HT, AT = hT[bi % 2], ACTT[bi % 2]
            hk, gk, ak = "hT7_%d" % (bi % 2), "gb7_%d" % (bi % 2), "ACTT%d" % (bi % 2)
            bi += 1
            k.dma("sp", lambda e, HT=HT, b=b, n0=n0: e.dma_start(
                out=HT[:], in_=H2T[b, :, :, n0:n0 + 512].rearrange("c p n -> p c n")), reads=["H2T"], writes=[hk])
            for m in range(8):
                PGt, PLt = PGL[pi % 4], PGL[(pi + 1) % 4]
                pgk, plk = "PGL%d" % (pi % 4), "PGL%d" % ((pi + 1) % 4)
                pi += 2
                GLU, SG, LIN = glu[mi % 2], sg[mi % 2], lin[mi % 2]
                glk, sgk, lik = "glu7_%d" % (mi % 2), "sg7_%d" % (mi % 2), "lin7_%d" % (mi % 2)
                mi += 1
                for kc in range(8):
                    k.op("pe", lambda e, PGt=PGt, W1t=W1t, HT=HT, kc=kc, m=m: e.matmul(
                        PGt[:], lhsT=W1t[:, kc, 2 * m * 128:2 * (m + 1) * 128:2], rhs=HT[:, kc, :],
                        start=(kc == 0), stop=(kc == 7)), reads=[w1k, hk], writes=[pgk])
                for kc in range(8):
                    k.op("pe", lambda e, PLt=PLt, W1t=W1t, HT=HT, kc=kc, m=m: e.matmul(
                        PLt[:], lhsT=W1t[:, kc, 2 * m * 128 + 1:2 * (m + 1) * 128:2], rhs=HT[:, kc, :],
                        start=(kc == 0), stop=(kc == 7)), reads=[w1k, hk], writes=[plk])
                k.op("dve", lambda e, PGt=PGt, GLU=GLU, ex=ex, m=m: e.tensor_scalar(
                    out=GLU[:], in0=PGt[:], scalar1=b1GL[:, ex, m, 0:1], scalar2=7.0, op0=ALU.add, op1=ALU.min),
                    reads=[pgk, "b1GL"], writes=[glk])
                k.op("act", lambda e, GLU=GLU, SG=SG: e.activation(out=SG[:], in_=GLU[:], func=AF.Sigmoid, scale=1.702),
                     reads=[glk], writes=[sgk])
                k.op("dve", lambda e, PLt=PLt, LIN=LIN, ex=ex, m=m: e.tensor_scalar(
                    out=LIN[:], in0=PLt[:], scalar1=b1GL[:, ex, m, 1:2], scalar2=7.0, op0=ALU.add, op1=ALU.min),
                    reads=[plk, "b1GL"], writes=[lik])
                k.op("dve", lambda e, LIN=LIN: e.tensor_scalar(out=LIN[:], in0=LIN[:], scalar1=-7.0, scalar2=1.0,
                                                              op0=ALU.max, op1=ALU.add), reads=[lik], writes=[lik])
                k.op("dve", lambda e, GLU=GLU, SG=SG: e.tensor_tensor(out=SG[:], in0=GLU[:], in1=SG[:], op=ALU.mult),
                     reads=[glk, sgk], writes=[sgk])
                k.op("dve", lambda e, SG=SG, LIN=LIN, AT=AT, m=m: e.tensor_tensor(out=AT[:, m, :], in0=SG[:],
                                                                              in1=LIN[:], op=ALU.mult),
                     reads=[sgk, lik], writes=[ak])
            for tt in range(4):
                YB = yb[yi % 2]
                ybk = "yb7_%d" % (yi % 2)
                yi += 1
                for hf in range(2):
                    for m in range(8):
                        k.op("pe", lambda e, AT=AT, W2t=W2t, tt=tt, hf=hf, m=m: e.matmul(
                            PY[:, hf * 512:(hf + 1) * 512], lhsT=AT[:, m, tt * 128:(tt + 1) * 128],
                            rhs=W2t[:, m, hf * 512:(hf + 1) * 512], start=(m == 0), stop=(m == 7)),
                            reads=[ak, w2k], writes=["PY7"])
                r0 = tg0 + tt * 128
                k.op("act", lambda e, YB=YB, r0=r0, ex=ex: e.activation(
                    out=YB[:], in_=PY[:], func=AF.Copy, scale=Gall[:, r0 // 128, ex:ex + 1]),
                    reads=["PY7", "Gall"], writes=[ybk])
                if ex == 0:
                    k.dma("pool", lambda e, YB=YB, r0=r0: e.dma_start(out=FD[r0:r0 + 128, :], in_=YB[:]),
                          reads=[ybk], writes=["FD%d" % (r0 // 128)])
                else:
                    k.dma("pool", lambda e, YB=YB, r0=r0: e.dma_start(out=FD[r0:r0 + 128, :], in_=YB[:],
                                                                     accum_op=ALU.add),
                          reads=[ybk], writes=["FD%d" % (r0 // 128)])


def make_sp(nc, k, ustr_d, thr_d, jg_d, kcp_d):
    sp = dict(ustr_d=ustr_d, thr_d=thr_d, jg_d=jg_d, kcp_d=kcp_d)
    sp["RANKD"] = nc.dram_tensor("RANKD", [BPC * SEQ, NE], F32, kind="Internal")
    sp["MSKD"] = nc.dram_tensor("MSKD", [BPC * SEQ, NE], F32, kind="Internal")
    sp["H2D"] = nc.dram_tensor("H2D", [BPC * SEQ, D], BF16, kind="Internal")
    sp["XS"] = nc.dram_tensor("XS", [NSLOT, D], BF16, kind="Internal")
    sp["YS"] = nc.dram_tensor("YS", [NSLOT, D], F32, kind="Internal")
    sp["carry"] = k.sb("carry", [1, NE], F32)
    sp["BEP"] = k.sb("BEP", [128, 128], F32)
    sp["IDX"] = k.sb("IDX", [128, NBLK, 8], I32)
    sp["IDXB"] = k.sb("IDXB", [128, NBLK], I32)
    sp["SLOTI"] = k.sb("SLOTI", [128, BPC * SEQ // 128, 4], I32)
    sp["GK"] = k.sb("GK", [128, BPC * SEQ // 128, 4], F32)
    return sp


def phase6b_blocks(nc, k, sp):
    carry = sp["carry"]
    thr = k.sb("thr6b", [1, NE, 16], F32)
    jg = k.sb("jg6b", [1, NBLK, NE], F32)
    kcp = k.sb("kcp6b", [128, 8], F32)
    k.dma("sp", lambda e: e.dma_start(out=thr[:], in_=sp["thr_d"].ap()), writes=["thr6b"])
    k.dma("sp", lambda e: e.dma_start(out=jg[:], in_=sp["jg_d"].ap()), writes=["jg6b"])
    k.dma("sp", lambda e: e.dma_start(out=kcp[:], in_=sp["kcp_d"].ap()), writes=["kcp6b"])
    cmp_ = k.sb("cmp6b", [1, NE, 16], F32)
    nblk = k.sb("nblk6b", [1, NE], F32)
    bend = k.sb("bend6b", [1, NE], F32)
    one1 = k.sb("one6b", [1, 128], F32)
    k.op("pool", lambda e: e.memset(one1[:], 1.0), writes=["one6b"])
    row = k.sb("row6b", [1, 128], F32)
    cmp2 = k.sb("cmp6b2", [1, NBLK, NE], F32)
    k.op("dve", lambda e: e.tensor_tensor(out=cmp_[:], in0=carry[:].unsqueeze(2).to_broadcast([1, NE, 16]),
                                          in1=thr[:], op=ALU.is_gt), reads=["carry", "thr6b"], writes=["cmp6b"])
    k.op("dve", lambda e: e.reduce_sum(out=nblk[:], in_=cmp_[:], axis=AX.X), reads=["cmp6b"], writes=["nblk6b"])
    k.op("dve", lambda e: e.tensor_tensor_scan(out=bend[:], data0=one1[:, 0:NE], data1=nblk[:], initial=0.0,
                                               op0=ALU.mult, op1=ALU.add),
         reads=["one6b", "nblk6b"], writes=["bend6b"])
    k.op("dve", lambda e: e.tensor_tensor(out=row[:, 96:128], in0=bend[:], in1=nblk[:], op=ALU.subtract),
         reads=["bend6b", "nblk6b"], writes=["row6b"])
    k.op("dve", lambda e: e.tensor_scalar(out=row[:, 96:128], in0=row[:, 96:128], scalar1=512.0, scalar2=None,
                                          op0=ALU.mult), reads=["row6b"], writes=["row6b"])
    k.op("dve", lambda e: e.tensor_tensor(out=cmp2[:], in0=bend[:].unsqueeze(1).to_broadcast([1, NBLK, NE]),
                                          in1=jg[:], op=ALU.is_le), reads=["bend6b", "jg6b"], writes=["cmp6b2"])
    k.op("dve", lambda e: e.reduce_sum(out=row[:, 0:NBLK], in_=cmp2[:], axis=AX.X),
         reads=["cmp6b2", "row6b"], writes=["row6b"])
    k.op("dve", lambda e: e.tensor_scalar(out=row[:, 0:NBLK], in0=row[:, 0:NBLK], scalar1=float(NE - 1),
                                          scalar2=None, op0=ALU.min), reads=["row6b"], writes=["row6b"])
    skp = k.sb("skp6b", [1, NBLK], F32)
    k.op("pool", lambda e: e.memset(skp[:], 0.0), writes=["skp6b"])
    k.op("dve", lambda e: e.tensor_tensor(out=skp[:, 2:NBLK], in0=row[:, 2:NBLK], in1=row[:, 0:NBLK - 2],
                                          op=ALU.is_equal), reads=["row6b", "skp6b"], writes=["skp6b"])
    k.op("dve", lambda e: e.scalar_tensor_tensor(out=row[:, 0:NBLK], in0=skp[:], scalar=64.0, in1=row[:, 0:NBLK],
                                                 op0=ALU.mult, op1=ALU.add),
         reads=["row6b", "skp6b"], writes=["row6b"])
    PBc = k.ps("PBc6b", [128, 128])
    k.op("pe", lambda e: e.matmul(PBc[:], lhsT=one1[:], rhs=row[:], start=True, stop=True),
         reads=["one6b", "row6b"], writes=["PBc6b"])
    BEP = sp["BEP"]
    k.op("act", lambda e: e.activation(out=BEP[:], in_=PBc[:], func=AF.Copy), reads=["PBc6b"], writes=["BEP"])
    idf = k.sb("idf6b", [128, NBLK, 8], F32)
    k.op("dve", lambda e: e.tensor_scalar(out=idf[:], in0=BEP[:, 0:NBLK].unsqueeze(2).to_broadcast([128, NBLK, 8]),
                                          scalar1=1024.0, scalar2=None, op0=ALU.mult),
         reads=["BEP"], writes=["idf6b"])
    k.op("dve", lambda e: e.tensor_tensor(out=idf[:], in0=idf[:], in1=kcp[:].unsqueeze(1).to_broadcast([128, NBLK, 8]),
                                          op=ALU.add), reads=["idf6b", "kcp6b"], writes=["idf6b"])
    k.op("dve", lambda e: e.tensor_copy(out=sp["IDX"][:], in_=idf[:]), reads=["idf6b"], writes=["IDX"])
    k.op("dve", lambda e: e.tensor_copy(out=sp["IDXB"][:], in_=BEP[:, 0:NBLK]), reads=["BEP"], writes=["IDXB"])


def phase6c_dispatch(nc, k, sp):
    BEP = sp["BEP"]
    rk = [k.sb("rk6c_%d" % i, [128, NE], F32) for i in range(2)]
    mk = [k.sb("mk6c_%d" % i, [128, NE], F32) for i in range(2)]
    gg = [k.sb("gg6c_%d" % i, [128, NE], F32) for i in range(2)]
    hx = [k.sb("hx6c_%d" % i, [128, D], BF16) for i in range(2)]
    key = k.sb("key6c", [128, NE], F32)
    t1 = k.sb("t16c", [128, NE], F32)
    mx = k.sb("mx6c", [128, 8], F32)
    sl = k.sb("sl6c", [128, 4], F32)
    CBIG = 65536.0
    for t in range(NTT):
        RK, MK_, GG, HX = rk[t % 2], mk[t % 2], gg[t % 2], hx[t % 2]
        rkk, mkk, ggk, hxk = "rk6c_%d" % (t % 2), "mk6c_%d" % (t % 2), "gg6c_%d" % (t % 2), "hx6c_%d" % (t % 2)
        r0 = t * 128
        k.dma("sp", lambda e, RK=RK, r0=r0: e.dma_start(out=RK[:], in_=sp["RANKD"][r0:r0 + 128, :]),
              reads=["RANKD"], writes=[rkk])
        k.dma("sp", lambda e, MK_=MK_, r0=r0: e.dma_start(out=MK_[:], in_=sp["MSKD"][r0:r0 + 128, :]),
              reads=["MSKD"], writes=[mkk])
        k.dma("act", lambda e, GG=GG, r0=r0: e.dma_start(out=GG[:], in_=sp["GD"][r0:r0 + 128, :]),
              reads=["GD"], writes=[ggk])
        k.dma("act", lambda e, HX=HX, r0=r0: e.dma_start(out=HX[:], in_=sp["H2D"][r0:r0 + 128, :]),
              reads=["H2D"], writes=[hxk])
        k.op("dve", lambda e, RK=RK: e.tensor_tensor(out=key[:], in0=RK[:], in1=BEP[:, 96:128], op=ALU.add),
             reads=[rkk, "BEP"], writes=["key6c"])
        k.op("dve", lambda e: e.tensor_scalar(out=key[:], in0=key[:], scalar1=-1.0, scalar2=CBIG, op0=ALU.mult,
                                              op1=ALU.add), reads=["key6c"], writes=["key6c"])
        k.op("dve", lambda e, MK_=MK_: e.tensor_tensor(out=key[:], in0=key[:], in1=MK_[:], op=ALU.mult),
             reads=["key6c", mkk], writes=["key6c"])
        k.op("dve", lambda e, MK_=MK_: e.scalar_tensor_tensor(out=key[:], in0=MK_[:], scalar=-1.0, in1=key[:],
                                                            op0=ALU.add, op1=ALU.add),
             reads=["key6c", mkk], writes=["key6c"])
        k.op("dve", lambda e: e.max(out=mx[:], in_=key[:]), reads=["key6c"], writes=["mx6c"])
        k.op("dve", lambda e: e.tensor_scalar(out=sl[:], in0=mx[:, 0:4], scalar1=-1.0, scalar2=CBIG, op0=ALU.mult,
                                              op1=ALU.add), reads=["mx6c"], writes=["sl6c"])
        k.op("dve", lambda e, t=t: e.tensor_copy(out=sp["SLOTI"][:, t, :], in_=sl[:]), reads=["sl6c"], writes=["SLOTI"])
        for c4 in range(4):
            k.op("dve", lambda e, c4=c4: e.tensor_scalar(out=t1[:], in0=key[:], scalar1=mx[:, c4:c4 + 1], scalar2=None,
                                                        op0=ALU.is_equal), reads=["key6c", "mx6c"], writes=["t16c"])
            k.op("dve", lambda e, GG=GG: e.tensor_tensor(out=t1[:], in0=t1[:], in1=GG[:], op=ALU.mult),
                 reads=["t16c", ggk], writes=["t16c"])
            k.op("dve", lambda e, t=t, c4=c4: e.reduce_sum(out=sp["GK"][:, t, c4:c4 + 1], in_=t1[:], axis=AX.X),
                 reads=["t16c"], writes=["GK"])
        for c4 in range(4):
            k.dma("pool", lambda e, HX=HX, t=t, c4=c4: e.indirect_dma_start(
                out=sp["XS"][:, :], out_offset=bass.IndirectOffsetOnAxis(ap=sp["SLOTI"][:, t, c4:c4 + 1], axis=0),
                in_=HX[:], in_offset=None), reads=[hxk, "SLOTI"], writes=["XS"])


_BREG = {}


def _breg(e, val):
    key = (id(e), val)
    if key not in _BREG:
        _BREG[key] = e.to_reg(val)
    return _BREG[key]


def phase7_sparse(nc, k, w1, b1, w2, sp, identb):
    XS, YS = sp["XS"], sp["YS"]
    w1r = w1.ap().rearrange("e k n -> (e k) n")
    w2r = w2.ap().rearrange("e k n -> (e k) n")
    ones7 = k.sb("ones7", [1, 512], BF16)
    k.op("pool", lambda e: e.memset(ones7[:], 1.0), writes=["ones7"])
    W1 = [k.sb("W1_%d" % i, [128, 8, 2048], BF16) for i in range(2)]
    W2 = [k.sb("W2_%d" % i, [128, 8, D], BF16) for i in range(2)]
    B1 = [k.sb("B1_%d" % i, [128, 2048], BF16) for i in range(2)]
    xs = [k.sb("xs7_%d" % i, [128, 4, D], BF16) for i in range(2)]
    hT = [k.sb("hT7_%d" % i, [128, 8, 512], BF16) for i in range(2)]
    glu = [k.sb("glu7_%d" % i, [128, 512], F32) for i in range(2)]
    sg = [k.sb("sg7_%d" % i, [128, 512], F32) for i in range(2)]
    lin = [k.sb("lin7_%d" % i, [128, 512], F32) for i in range(2)]
    ACTT = [k.sb("ACTT%d" % i, [128, 8, 512], BF16) for i in range(2)]
    yb = [k.sb("yb7_%d" % i, [128, D], F32) for i in range(2)]
    PGL = [k.ps("PGL%d" % i, [128, 512]) for i in range(4)]
    PY = k.ps("PY7", [128, D])
    PX = [k.ps("PX7_%d" % i, [128, 512], BF16) for i in range(2)]
    pi = 0
    mi = 0
    yi = 0
    xi = 0
    for j in range(NBLK7):
        W1t, W2t, B1t, XSt, HT, AT = W1[j % 2], W2[j % 2], B1[j % 2], xs[j % 2], hT[j % 2], ACTT[j % 2]
        w1k, w2k, b1k, xsk, hk, ak = ("W1_%d" % (j % 2), "W2_%d" % (j % 2), "B1_%d" % (j % 2), "xs7_%d" % (j % 2),
                                      "hT7_%d" % (j % 2), "ACTT%d" % (j % 2))
        for kc in range(8):
            k.dma("pool", lambda e, W1t=W1t, j=j, kc=kc: e.indirect_dma_start(
                out=W1t[:, kc, :], out_offset=None, in_=w1r,
                in_offset=bass.IndirectOffsetOnAxis(ap=sp["IDX"][:, j, kc:kc + 1], axis=0),
                bounds_check=_breg(e, NE * D - 1), oob_is_err=False),
                reads=["IDX"], writes=[w1k])
        for kc in range(8):
            k.dma("pool", lambda e, W2t=W2t, j=j, kc=kc: e.indirect_dma_start(
                out=W2t[:, kc, :], out_offset=None, in_=w2r,
                in_offset=bass.IndirectOffsetOnAxis(ap=sp["IDX"][:, j, kc:kc + 1], axis=0),
                bounds_check=_breg(e, NE * D - 1), oob_is_err=False),
                reads=["IDX"], writes=[w2k])
        k.dma("pool", lambda e, B1t=B1t, j=j: e.indirect_dma_start(
            out=B1t[:], out_offset=None, in_=b1.ap(),
            in_offset=bass.IndirectOffsetOnAxis(ap=sp["IDXB"][:, j:j + 1], axis=0),
            bounds_check=_breg(e, NE - 1), oob_is_err=False),
            reads=["IDXB"], writes=[b1k])
        for tt in range(4):
            k.dma("sp" if tt % 2 == 0 else "act", lambda e, XSt=XSt, j=j, tt=tt: e.dma_start(
                out=XSt[:, tt, :], in_=XS[j * 512 + tt * 128:j * 512 + (tt + 1) * 128, :]),
                reads=["XS"], writes=[xsk])
        for kc in range(8):
            PXt = PX[xi % 2]
            pxk = "PX7_%d" % (xi % 2)
            xi += 1
            for tt in range(4):
                k.op("pe", lambda e, PXt=PXt, XSt=XSt, tt=tt, kc=kc: e.transpose(
                    out=PXt[:, tt * 128:(tt + 1) * 128], in_=XSt[:, tt, kc * 128:(kc + 1) * 128], identity=identb[:]),
                    reads=[xsk, "identb"], writes=[pxk])
            if kc % 2 == 0:
                k.op("act", lambda e, PXt=PXt, HT=HT, kc=kc: e.activation(out=HT[:, kc, :], in_=PXt[:], func=AF.Copy),
                     reads=[pxk], writes=[hk])
            else:
                k.op("dve", lambda e, PXt=PXt, HT=HT, kc=kc: e.tensor_scalar(out=HT[:, kc, :], in0=PXt[:], scalar1=1.0,
                                                                           scalar2=None, op0=ALU.mult),
                     reads=[pxk], writes=[hk])
        for m in range(8):
            PGt, PLt = PGL[pi % 4], PGL[(pi + 1) % 4]
            pgk, plk = "PGL%d" % (pi % 4), "PGL%d" % ((pi + 1) % 4)
            pi += 2
            GLU, SG, LIN = glu[mi % 2], sg[mi % 2], lin[mi % 2]
            glk, sgk, lik = "glu7_%d" % (mi % 2), "sg7_%d" % (mi % 2), "lin7_%d" % (mi % 2)
            mi += 1
            for (Pt, ptk, o) in ((PGt, pgk, 0), (PLt, plk, 1)):
                for kc in range(8):
                    k.op("pe", lambda e, Pt=Pt, W1t=W1t, HT=HT, kc=kc, m=m, o=o: e.matmul(
                        Pt[:], lhsT=W1t[:, kc, 2 * m * 128 + o:2 * (m + 1) * 128:2], rhs=HT[:, kc, :],
                        start=(kc == 0), stop=False), reads=[w1k, hk], writes=[ptk])
                k.op("pe", lambda e, Pt=Pt, B1t=B1t, m=m, o=o: e.matmul(
                    Pt[:], lhsT=B1t[0:1, 2 * m * 128 + o:2 * (m + 1) * 128:2], rhs=ones7[:],
                    start=False, stop=True), reads=[b1k, "ones7"], writes=[ptk])
            k.op("dve", lambda e, PGt=PGt, GLU=GLU: e.tensor_scalar(out=GLU[:], in0=PGt[:], scalar1=7.0, scalar2=None,
                                                                    op0=ALU.min), reads=[pgk], writes=[glk])
            k.op("act", lambda e, GLU=GLU, SG=SG: e.activation(out=SG[:], in_=GLU[:], func=AF.Sigmoid, scale=1.702),
                 reads=[glk], writes=[sgk])
            k.op("dve", lambda e, PLt=PLt, LIN=LIN: e.tensor_scalar(out=LIN[:], in0=PLt[:], scalar1=7.0, scalar2=-7.0,
                                                                    op0=ALU.min, op1=ALU.max),
                 reads=[plk], writes=[lik])
            k.op("dve", lambda e, GLU=GLU, SG=SG: e.tensor_tensor(out=SG[:], in0=GLU[:], in1=SG[:], op=ALU.mult),
                 reads=[glk, sgk], writes=[sgk])
            k.op("dve", lambda e, SG=SG, LIN=LIN, AT=AT, m=m: e.scalar_tensor_tensor(
                out=AT[:, m, :], in0=LIN[:], scalar=1.0, in1=SG[:], op0=ALU.add, op1=ALU.mult),
                reads=[sgk, lik], writes=[ak])
        for tt in range(4):
            YB = yb[yi % 2]
            ybk = "yb7_%d" % (yi % 2)
            yi += 1
            for hf in range(2):
                for m in range(8):
                    k.op("pe", lambda e, AT=AT, W2t=W2t, tt=tt, hf=hf, m=m: e.matmul(
                        PY[:, hf * 512:(hf + 1) * 512], lhsT=AT[:, m, tt * 128:(tt + 1) * 128],
                        rhs=W2t[:, m, hf * 512:(hf + 1) * 512], start=(m == 0), stop=(m == 7)),
                        reads=[ak, w2k], writes=["PY7"])
            k.op("act", lambda e, YB=YB: e.activation(out=YB[:], in_=PY[:], func=AF.Copy), reads=["PY7"], writes=[ybk])
            r0 = j * 512 + tt * 128
            k.dma("sp", lambda e, YB=YB, r0=r0: e.dma_start(out=YS[r0:r0 + 128, :], in_=YB[:]),
                  reads=[ybk], writes=["YS"])


def phase8_final(nc, k, X1, FD, GTd, b2, ln2, MOD, out, sp=None):
    def brow(name, ap1d, n=D):
        t = k.sb(name, [128, n], F32)
        k.dma("sp", lambda e: e.dma_start(out=t[:], in_=bcast_rows(ap1d, n)), reads=["MOD"], writes=[name])
        return t
    g2B = brow("g2B", ln2[0])
    b2B = brow("b2B", ln2[1])
    b2sb = k.sb("b2sb", [NE, D], F32)
    k.dma("sp", lambda e: e.dma_start(out=b2sb[:], in_=b2.ap()), writes=["b2sb"])
    Ft = [k.sb("F8_%d" % i, [128, D], F32) for i in range(2)]
    Xt = [k.sb("X8_%d" % i, [128, D], F32) for i in range(2)]
    gT = [k.sb("gT8_%d" % i, [NE, 128], F32) for i in range(2)]
    z = [k.sb("z8_%d" % i, [128, D], F32) for i in range(2)]
    st = k.sb("bnst8", [128, 2, 6], F32)
    mv = k.sb("bnmv8", [128, 2], F32)
    rstd = k.sb("rstd8", [128, 1], F32)
    PB = k.ps("PB8", [128, D])
    if sp is not None:
        ygs = [k.sb("yg8_%d" % i, [128, D], F32) for i in range(4)]
    ti = 0
    for b in range(BPC):
        gt2B = brow("gt2B%d" % b, MOD[b, 5120:6144])
        for t in range(NT8):
            F_, X_, GT_, Z_ = Ft[ti % 2], Xt[ti % 2], gT[ti % 2], z[ti % 2]
            fk, xk, gk, zk = "F8_%d" % (ti % 2), "X8_%d" % (ti % 2), "gT8_%d" % (ti % 2), "z8_%d" % (ti % 2)
            ti += 1
            tok0 = t * 128
            tg0 = b * SEQ + tok0
            if sp is None:
                k.dma("sp", lambda e, F_=F_, tg0=tg0: e.dma_start(out=F_[:], in_=FD[tg0:tg0 + 128, :]),
                      reads=["FD%d" % (tg0 // 128)], writes=[fk])
            else:
                tl = tg0 // 128
                for c4 in range(4):
                    Yg = ygs[(ti * 4 + c4) % 4]
                    ygk = "yg8_%d" % ((ti * 4 + c4) % 4)
                    k.dma("pool", lambda e, Yg=Yg, tl=tl, c4=c4: e.indirect_dma_start(
                        out=Yg[:], out_offset=None, in_=sp["YS"][:, :],
                        in_offset=bass.IndirectOffsetOnAxis(ap=sp["SLOTI"][:, tl, c4:c4 + 1], axis=0)),
                        reads=["YS", "SLOTI"], writes=[ygk])
                    if c4 == 0:
                        k.op("dve", lambda e, Yg=Yg, F_=F_, tl=tl, c4=c4: e.tensor_scalar(
                            out=F_[:], in0=Yg[:], scalar1=sp["GK"][:, tl, c4:c4 + 1], scalar2=None, op0=ALU.mult),
                            reads=[ygk, "GK"], writes=[fk])
                    else:
                        k.op("dve", lambda e, Yg=Yg, F_=F_, tl=tl, c4=c4: e.scalar_tensor_tensor(
                            out=F_[:], in0=Yg[:], scalar=sp["GK"][:, tl, c4:c4 + 1], in1=F_[:], op0=ALU.mult,
                            op1=ALU.add), reads=[ygk, "GK", fk], writes=[fk])
            k.dma("act", lambda e, X_=X_, b=b, tok0=tok0: e.dma_start(out=X_[:], in_=X1[b, tok0:tok0 + 128, :]),
                  reads=["X1"], writes=[xk])
            k.dma("act", lambda e, GT_=GT_, tg0=tg0: e.dma_start(out=GT_[:], in_=GTd[:, tg0:tg0 + 128]),
                  reads=["GTd"], writes=[gk])
            for hf in range(2):
                k.op("pe", lambda e, GT_=GT_, hf=hf: e.matmul(PB[:, hf * 512:(hf + 1) * 512], lhsT=GT_[:],
                                                             rhs=b2sb[:, hf * 512:(hf + 1) * 512], start=True,
                                                             stop=True), reads=[gk, "b2sb"], writes=["PB8"])
            k.op("dve", lambda e, F_=F_: e.tensor_tensor(out=F_[:], in0=PB[:], in1=F_[:], op=ALU.add),
                 reads=["PB8", fk], writes=[fk])
            k.op("pool", lambda e, F_=F_, gt2B=gt2B: e.tensor_tensor(out=F_[:], in0=F_[:], in1=gt2B[:], op=ALU.mult),
                 reads=[fk, "gt2B%d" % b], writes=[fk])
            k.op("dve", lambda e, F_=F_, X_=X_, Z_=Z_: e.scalar_tensor_tensor(out=Z_[:], in0=X_[:], scalar=ALPHA,
                                                                            in1=F_[:], op0=ALU.mult, op1=ALU.add),
                 reads=[fk, xk], writes=[zk])
            ln_stats(k, Z_, zk, st, mv, rstd, "8")
            k.op("dve", lambda e, Z_=Z_: e.tensor_scalar(out=Z_[:], in0=Z_[:], scalar1=mv[:, 0:1],
                                                        scalar2=rstd[:, 0:1], op0=ALU.subtract, op1=ALU.mult),
                 reads=[zk, "bnmv8", "rstd8"], writes=[zk])
            k.op("pool", lambda e, Z_=Z_: e.tensor_tensor(out=Z_[:], in0=Z_[:], in1=g2B[:], op=ALU.mult),
                 reads=[zk, "g2B"], writes=[zk])
            k.op("pool", lambda e, Z_=Z_: e.tensor_tensor(out=Z_[:], in0=Z_[:], in1=b2B[:], op=ALU.add),
                 reads=[zk, "b2B"], writes=[zk])
            k.dma("sp", lambda e, Z_=Z_, b=b, tok0=tok0: e.dma_start(out=out[b, tok0:tok0 + 128, :], in_=Z_[:]),
                  reads=[zk], writes=["out"])


def moe_consts():
    f32 = np.float32
    ustr_h = np.triu(np.ones((128, 128), f32), 1)
    thr_h = np.ascontiguousarray(np.broadcast_to((512.0 * np.arange(16, dtype=f32))[None, None, :], (1, NE, 16)))
    jg_h = np.ascontiguousarray(np.broadcast_to(np.arange(NBLK, dtype=f32)[None, :, None], (1, NBLK, NE)))
    kcp_h = (np.arange(8, dtype=f32)[None, :] * 128 + np.arange(128, dtype=f32)[:, None]).astype(f32)
    return ustr_h, thr_h, jg_h, kcp_h


_DFT = []


def _dft_consts():
    if not _DFT:
        n = np.arange(SEQ, dtype=np.int64)
        kt = (n[:, None] * n[None, :]) % SEQ
        ang = (2.0 * np.pi / SEQ) * kt.astype(np.float64)
        def lay(m):
            m = m.astype(BF_NP).reshape(32, 128, 8, 512)
            return np.ascontiguousarray(m.transpose(2, 1, 0, 3)).reshape(8, 128, 32 * 512)
        _DFT.append(lay(np.cos(ang) / 64.0))
        _DFT.append(lay(-np.sin(ang) / 64.0))
    return _DFT[0], _DFT[1]


def _run(inputs, dbg=None):
    nc = build_program(dbg)
    x = np.ascontiguousarray(inputs["x"], dtype=np.float32)
    ctx = np.ascontiguousarray(inputs["ctx"], dtype=np.float32)
    in_maps = []
    ident = np.eye(128, dtype=np.float32)
    f32 = np.float32
    vecs_h = np.ascontiguousarray(np.concatenate([
        inputs["w0"][0].reshape(-1), inputs["a0"][0].reshape(-1), inputs["k_k"][0], inputs["k_a"][0],
        inputs["gn_g"][0], inputs["gn_b"][0], inputs["r_k"][0].reshape(-1), inputs["b_fno"][0].reshape(-1),
    ]).astype(f32).reshape(44, 128))
    w2d_h = np.ascontiguousarray(inputs["w2_decay"][0].reshape(128, C).astype(f32))
    a2_h = np.ascontiguousarray(inputs["a2_iclr"][0].reshape(128, C).astype(f32))
    g2_h = np.ascontiguousarray(inputs["g2_gate"][0].astype(f32))
    bones_h = np.kron(np.eye(2, dtype=f32), np.ones((64, 64), f32))
    i2rep_h = np.ascontiguousarray(np.broadcast_to(
        np.concatenate([np.eye(64, dtype=f32)] * 2, 0)[:, None, :], (128, 16, 64))).astype(BF_NP)
    hsel_h = np.kron(np.eye(2, dtype=f32), np.ones((64, 1), f32)).astype(BF_NP)
    jmat_h = np.ascontiguousarray(np.eye(128, dtype=f32)[::-1])
    ustr_h, thr_h, jg_h, kcp_h = moe_consts()
    oh_h = np.zeros((128, TC, 40), f32)
    for tq in range(TC):
        oh_h[tq, tq, 0:8] = 1.0
        oh_h[64 + tq, tq, 32:40] = 1.0
    oh_h = oh_h.astype(BF_NP)
    smask_h = np.zeros((104, 8, 64), f32)
    for gl in range(8):
        smask_h[gl * 2:gl * 2 + 2, gl, :] = 1.0
        smask_h[16 + gl * 2:18 + gl * 2, gl, :] = 1.0
        smask_h[64 + gl, gl, :] = 1.0
        smask_h[96 + gl, gl, :] = 1.0
    smask_h = smask_h.reshape(104, 512)
    cc = np.arange(64)
    ang = 2.0 * np.pi * ((cc[:, None] * cc[None, :]) % 64) / 64.0
    ccbd_h = np.kron(np.eye(2), np.cos(ang) / 8.0).astype(f32)
    scbd_h = np.kron(np.eye(2), np.sin(ang) / 8.0).astype(f32)
    cosm_h, nsin_h = _dft_consts()
    wfno_h = np.ascontiguousarray(inputs["w_fno"][0].astype(f32))
    wout_h = np.ascontiguousarray(inputs["w_out"][0].astype(f32))
    ln1_h = np.ascontiguousarray(np.stack([inputs["ln1_g"][0], inputs["ln1_b"][0]], 0).astype(f32))
    ln2_h = np.ascontiguousarray(np.stack([inputs["ln2_g"][0], inputs["ln2_b"][0]], 0).astype(f32))
    wr_h = np.ascontiguousarray(inputs["w_router"][0].astype(f32))
    br_h = np.ascontiguousarray(inputs["b_router"][0][None, :].astype(f32))
    w1_h = np.ascontiguousarray(inputs["w1"][0].astype(f32))
    b1_h = np.ascontiguousarray(inputs["b1"][0].astype(f32))
    w2_h = np.ascontiguousarray(inputs["w2"][0].astype(f32))
    b2_h = np.ascontiguousarray(inputs["b2"][0].astype(f32))
    for c in range(NCORES):
        b0 = c * BPC
        m = {
            "x": x[b0:b0 + BPC],
            "ctx": ctx[b0:b0 + BPC],
            "cvec": np.ascontiguousarray(np.concatenate(
                [inputs["c"][b0:b0 + BPC], inputs["c_ctx"][None, :]], 0), dtype=np.float32),
            "ln0": np.ascontiguousarray(np.stack([inputs["ln0_g"], inputs["ln0_b"]], 0)),
            "w_ada": np.ascontiguousarray(inputs["w_ada"][0]),
            "b_ada": np.ascontiguousarray(inputs["b_ada"][0][None, :]),
            "w_in": np.ascontiguousarray(inputs["w_in"][0]),
            "mu": np.ascontiguousarray(inputs["mu_shift"][0].reshape(SHW // 128, 128)),
            "ident": ident,
            "ustr": ustr_h, "thr": thr_h, "jg": jg_h, "kcp": kcp_h,
            "bonesb": bones_h.astype(BF_NP), "i2rep": i2rep_h, "hsel": hsel_h, "oh": oh_h, "smask": smask_h,
            "jmat": jmat_h, "ccbd": ccbd_h, "scbd": scbd_h, "cosm": cosm_h, "nsin": nsin_h,
            "wfno": wfno_h, "w_out": wout_h, "ln1": ln1_h, "ln2": ln2_h, "w_router": wr_h, "b_router": br_h,
            "w1": w1_h, "b1": b1_h, "w2": w2_h, "b2": b2_h,
            "vecs": vecs_h, "w2d": w2d_h, "a2": a2_h, "g2": g2_h, "bones": bones_h,
        }
        in_maps.append(m)
    res = run_bass_kernel_spmd(nc, in_maps, core_ids=list(range(NCORES)))
    return res


def kernel(**inputs):
    res = _run(inputs)
    outs = [r["out"] for r in res.results]
    return np.concatenate(outs, axis=0).astype(np.float32)
```

```python
import numpy as np
import ml_dtypes
BF_NP = ml_dtypes.bfloat16
from contextlib import ExitStack, contextmanager
import concourse.bass as bass
import concourse.mybir as mybir
from concourse.bass_utils import run_bass_kernel_spmd

F32 = mybir.dt.float32
BF16 = mybir.dt.bfloat16
I32 = mybir.dt.int32
U32 = mybir.dt.uint32
ALU = mybir.AluOpType
AF = mybir.ActivationFunctionType
AX = mybir.AxisListType

NCORES = 8
BPC = 2
D = 1024
SEQ = 4096
CTX = 256
NV = CTX + SEQ
INW = 2560
SHW = 2048
C = 512
GRID = 64
NE = 32
ALPHA = 2.0 ** 0.25
LN_EPS = 1e-5
GN_EPS = 64e-5
NT6 = SEQ // 128
STOP6 = 99
NT8 = SEQ // 128
NE7 = 32
NB7 = 16
NBLK7 = 96
NTT = BPC * SEQ // 128
NBLK = 96
NSLOT = NBLK * 512
SPARSE = True

ENG = ("pe", "dve", "act", "pool", "sp")
NRING = 8


class Ctx:
    def __init__(self, nc, es):
        self.nc = nc
        self.es = es
        self.q = {e: [] for e in ENG}
        self.cnt = {e: 0 for e in ENG}
        self.sem = {e: es.enter_context(nc.semaphore("s_" + e)) for e in ENG}
        self.seen = {e: {} for e in ENG}
        self.W = {}
        self.R = {}
        self.ring = {}
        self.dn = {}
        for qn in ("sp", "act", "pool"):
            self.ring[qn] = [es.enter_context(nc.semaphore("d_%s%d" % (qn, i))) for i in range(NRING)]
            self.dn[qn] = 0
        self.uid = 0
        self.cur = es

    def sb(self, name, shape, dtype):
        return self.cur.enter_context(self.nc.sbuf_tensor("t_" + name, list(shape), dtype))

    def ps(self, name, shape, dtype=F32):
        return self.cur.enter_context(self.nc.psum_tensor("p_" + name, list(shape), dtype))

    @contextmanager
    def phase(self):
        st = ExitStack()
        prev = self.cur
        self.cur = st
        try:
            yield
        finally:
            self.barrier()
            self.flush()
            st.close()
            self.cur = prev

    def barrier(self):
        toks = []
        for e in ENG:
            if self.cnt[e] > 0:
                toks.append(("e_" + e, self.sem[e], self.cnt[e], e))
        for qn in self.ring:
            n = self.dn[qn]
            for slot in range(NRING):
                if n > slot:
                    rounds = (n - 1 - slot) // NRING + 1
                    toks.append(("d_%s%d" % (qn, slot), self.ring[qn][slot], 16 * rounds, "dma"))
        for eng in ENG:
            waits = []
            for (semkey, sem, val, src) in toks:
                if src == eng:
                    continue
                if self.seen[eng].get(semkey, 0) >= val:
                    continue
                self.seen[eng][semkey] = val
                waits.append((sem, val))

            def emit(e, waits=waits):
                for (s, v) in waits:
                    e.wait_ge(s, v)

            self.q[eng].append(emit)

    def flush(self):
        qs = self.q
        self.q = {e: [] for e in ENG}
        with self.nc.Block() as block:
            @block.tensor
            def _(e):
                for f in qs["pe"]:
                    f(e)

            @block.vector
            def _(e):
                for f in qs["dve"]:
                    f(e)

            @block.scalar
            def _(e):
                for f in qs["act"]:
                    f(e)

            @block.gpsimd
            def _(e):
                for f in qs["pool"]:
                    f(e)

            @block.sync
            def _(e):
                for f in qs["sp"]:
                    f(e)

    def _deps(self, eng, reads, writes):
        toks = []
        for k in reads:
            if k in self.W:
                toks.append(self.W[k])
        for k in writes:
            if k in self.W:
                toks.append(self.W[k])
            toks.extend(self.R.get(k, ()))
        waits = {}
        for (semkey, sem, val, src) in toks:
            if src == eng and eng == "pe":
                continue
            if self.seen[eng].get(semkey, 0) >= val:
                continue
            if waits.get(semkey, (None, 0))[1] < val:
                waits[semkey] = (sem, val)
        for semkey, (sem, val) in waits.items():
            self.seen[eng][semkey] = val
        return list(waits.values())

    def _commit(self, tok, reads, writes):
        for k in reads:
            self.R.setdefault(k, []).append(tok)
        for k in writes:
            self.W[k] = tok
            self.R[k] = []

    def op(self, eng, fn, reads=(), writes=()):
        waits = self._deps(eng, reads, writes)
        self.cnt[eng] += 1
        idx = self.cnt[eng]
        mysem = self.sem[eng]
        tok = ("e_" + eng, mysem, idx, eng)
        self._commit(tok, reads, writes)

        def emit(e, waits=waits, fn=fn, mysem=mysem):
            for (s, v) in waits:
                e.wait_ge(s, v)
            fn(e).then_inc(mysem, 1)

        self.q[eng].append(emit)

    def dma(self, qn, fn, reads=(), writes=()):
        waits = self._deps(qn, reads, writes)
        n = self.dn[qn]
        self.dn[qn] += 1
        slot, rnd = n % NRING, n // NRING
        sem = self.ring[qn][slot]
        semkey = "d_%s%d" % (qn, slot)
        if rnd > 0 and self.seen[qn].get(semkey, 0) < 16 * rnd:
            waits.append((sem, 16 * rnd))
            self.seen[qn][semkey] = 16 * rnd
        tok = (semkey, sem, 16 * (rnd + 1), "dma")
        self._commit(tok, reads, writes)

        def emit(e, waits=waits, fn=fn, sem=sem):
            for (s, v) in waits:
                e.wait_ge(s, v)
            fn(e).then_inc(sem, 16)

        self.q[qn].append(emit)

    def finish(self, final_keys):
        self.barrier()
        self.flush()


def build_program(dbg=None):
    _BREG.clear()
    nc = bass.Bass("TRN2", target_bir_lowering=False)
    es = ExitStack()
    with es:
        k = Ctx(nc, es)
        _build(nc, k, dbg)
    return nc


def dram_in(nc, name, shape, dtype=F32):
    return nc.dram_tensor(name, list(shape), dtype, kind="ExternalInput")


def _build(nc, k, dbg):
    x = dram_in(nc, "x", [BPC, SEQ, D])
    ctx_in = dram_in(nc, "ctx", [BPC, CTX, D])
    cvec = dram_in(nc, "cvec", [3, D])
    ln0 = dram_in(nc, "ln0", [2, D])
    w_ada = dram_in(nc, "w_ada", [D, 6 * D])
    b_ada = dram_in(nc, "b_ada", [1, 6 * D])
    w_in = dram_in(nc, "w_in", [D, INW])
    mu = dram_in(nc, "mu", [SHW // 128, 128])
    ident_d = dram_in(nc, "ident", [128, 128])
    out = nc.dram_tensor("out", [BPC, SEQ, D], F32, kind="ExternalOutput")

    vecs = dram_in(nc, "vecs", [44, 128])
    w2d_d = dram_in(nc, "w2d", [128, C])
    a2_d = dram_in(nc, "a2", [128, C])
    g2_d = dram_in(nc, "g2", [2, 128, C])
    bones_d = dram_in(nc, "bones", [128, 128])
    SC = nc.dram_tensor("SC", [BPC, 9, 4, 128, NV], F32, kind="Internal")
    GT = nc.dram_tensor("GT", [BPC, 2, 4, 128, SEQ], F32, kind="Internal")
    bonesb_d = dram_in(nc, "bonesb", [128, 128], BF16)
    i2rep_d = dram_in(nc, "i2rep", [128, 16, 64], BF16)
    hsel_d = dram_in(nc, "hsel", [128, 2], BF16)
    oh_d = dram_in(nc, "oh", [128, TC, 40], BF16)
    mask_d = dram_in(nc, "smask", [104, 512])
    YSr = nc.dram_tensor("YSr", [2, SEQ, 1024], BF16, kind="Internal")
    jmat_d = dram_in(nc, "jmat", [128, 128])
    ccbd_d = dram_in(nc, "ccbd", [128, 128])
    scbd_d = dram_in(nc, "scbd", [128, 128])
    cosm_d = dram_in(nc, "cosm", [8, 128, 32 * 512], BF16)
    nsin_d = dram_in(nc, "nsin", [8, 128, 32 * 512], BF16)
    wfno_d = dram_in(nc, "wfno", [8, 64, 64])
    w_out = dram_in(nc, "w_out", [D, D])
    ln1 = dram_in(nc, "ln1", [2, D])
    ln2 = dram_in(nc, "ln2", [2, D])
    w_router = dram_in(nc, "w_router", [D, NE])
    b_router = dram_in(nc, "b_router", [1, NE])
    w1 = dram_in(nc, "w1", [NE, D, 2 * D])
    b1 = dram_in(nc, "b1", [NE, 2 * D])
    w2 = dram_in(nc, "w2", [NE, D, D])
    b2 = dram_in(nc, "b2", [NE, D])
    MIXT = nc.dram_tensor("MIXT", [BPC, 8, 128, SEQ], BF16, kind="Internal")
    X1 = nc.dram_tensor("X1", [BPC, SEQ, D], F32, kind="Internal")
    H2T = nc.dram_tensor("H2T", [BPC, 8, 128, SEQ], BF16, kind="Internal")
    GTd = nc.dram_tensor("GTd", [NE, BPC * SEQ], F32, kind="Internal")
    FD = nc.dram_tensor("FD", [BPC * SEQ, D], F32, kind="Internal")
    GD = nc.dram_tensor("GD", [BPC * SEQ, NE], F32, kind="Internal")
    ustr_d = dram_in(nc, "ustr", [128, 128])
    thr_d = dram_in(nc, "thr", [1, NE, 16])
    jg_d = dram_in(nc, "jg", [1, NBLK, NE])
    kcp_d = dram_in(nc, "kcp", [128, 8])
    MOD = nc.dram_tensor("MOD", [3, 6 * D], F32, kind="Internal")
    PFM = nc.dram_tensor("PFM", [BPC, INW, NV], F32, kind="Internal")
    dbg_t = None
    if dbg is not None:
        dbg_t = nc.dram_tensor("dbg", list(dbg[1]), F32, kind="ExternalOutput")

    ident = k.sb("ident", [128, 128], F32)
    k.dma("sp", lambda e: e.dma_start(out=ident[:], in_=ident_d.ap()), writes=["ident"])
    identb = k.sb("identb", [128, 128], BF16)
    k.dma("pool", lambda e: e.dma_start(out=identb[:], in_=ident_d.ap()), writes=["identb"])

    with k.phase():
        phase0_mod(nc, k, cvec, w_ada, b_ada, MOD)
    if dbg is not None and dbg[0] == "mod":
        t = k.sb("dbgt", [3, 6 * D], F32)
        k.dma("sp", lambda e: e.dma_start(out=t[:], in_=MOD.ap()), reads=["MOD"], writes=["dbgt"])
        k.dma("sp", lambda e: e.dma_start(out=dbg_t.ap(), in_=t[:]), reads=["dbgt"], writes=["dbg"])
        k.finish(["dbg"])
        return
    with k.phase():
        phase1_proj(nc, k, x, ctx_in, ln0, MOD, w_in, mu, PFM, ident)
    if dbg is not None and dbg[0] == "sc":
        with k.phase():
            phase2_prepare(nc, k, PFM, vecs, w2d_d, a2_d, g2_d, bones_d, SC, GT)
        b_, q_, hp_, n0 = dbg[2]
        t = k.sb("dbgt", [128, 512], F32)
        srcap = SC[b_, q_, hp_, :, n0:n0 + 512] if q_ < 9 else GT[b_, q_ - 9, hp_, :, n0:n0 + 512]
        k.dma("sp", lambda e: e.dma_start(out=t[:], in_=srcap), reads=["SC", "GT"], writes=["dbgt"])
        k.dma("sp", lambda e: e.dma_start(out=dbg_t.ap(), in_=t[:]), reads=["dbgt"], writes=["dbg"])
        k.finish(["dbg"])
        return
    if dbg is not None and dbg[0] == "scan":
        with k.phase():
            phase2_prepare(nc, k, PFM, vecs, w2d_d, a2_d, g2_d, bones_d, SC, GT)
        with k.phase():
            phase3_scan(nc, k, SC, YSr, ident, identb, oh_d, mask_d)
        hh_, s0 = dbg[2]
        t = k.sb("dbgt", [128, 1024], F32)
        k.dma("pool", lambda e: e.dma_start(out=t[:], in_=YSr[hh_, s0:s0 + 128, :]), reads=["YSr"], writes=["dbgt"])
        k.dma("sp", lambda e: e.dma_start(out=dbg_t.ap(), in_=t[:]), reads=["dbgt"], writes=["dbg"])
        k.finish(["dbg"])
        return
    if dbg is not None and dbg[0] == "pfm":
        b_, r0, n0 = dbg[2]
        t = k.sb("dbgt", [128, 512], F32)
        k.dma("sp", lambda e: e.dma_start(out=t[:], in_=PFM[b_, r0:r0 + 128, n0:n0 + 512]), reads=["PFM"], writes=["dbgt"])
        k.dma("sp", lambda e: e.dma_start(out=dbg_t.ap(), in_=t[:]), reads=["dbgt"], writes=["dbg"])
        k.finish(["dbg"])
        return
    with k.phase():
        phase2_prepare(nc, k, PFM, vecs, w2d_d, a2_d, g2_d, bones_d, SC, GT)
    with k.phase():
        phase3_scan(nc, k, SC, YSr, ident, identb, oh_d, mask_d)
    with k.phase():
        phase4_rwkv_out(nc, k, SC, GT, YSr, vecs, bones_d, ident, jmat_d, MIXT)
    if not (dbg is not None and len(dbg) > 3 and dbg[3] == "skip5"):
        with k.phase():
            phase5_fft(nc, k, PFM, wfno_d, vecs, ccbd_d, scbd_d, cosm_d, nsin_d, MIXT)
    if dbg is not None and dbg[0] == "mixt":
        b_, c_, n0 = dbg[2]
        t = k.sb("dbgtb", [128, 512], BF16)
        t2 = k.sb("dbgt", [128, 512], F32)
        k.dma("sp", lambda e: e.dma_start(out=t[:], in_=MIXT[b_, c_, :, n0:n0 + 512]), reads=["MIXT"], writes=["dbgtb"])
        k.op("dve", lambda e: e.tensor_copy(out=t2[:], in_=t[:]), reads=["dbgtb"], writes=["dbgt"])
        k.dma("sp", lambda e: e.dma_start(out=dbg_t.ap(), in_=t2[:]), reads=["dbgt"], writes=["dbg"])
        k.finish(["dbg"])
        return
    sp = make_sp(nc, k, ustr_d, thr_d, jg_d, kcp_d)
    sp["GD"] = GD
    with k.phase():
        phase6_outproj(nc, k, x, ln0, ln1, MOD, w_out, w_router, b_router, MIXT, ident, X1, H2T, GTd, GD,
                       sp if SPARSE else None)
    if dbg is not None and dbg[0] == "x1":
        b_, n0 = dbg[2]
        t2 = k.sb("dbgt", [128, 1024], F32)
        k.dma("sp", lambda e: e.dma_start(out=t2[:], in_=X1[b_, n0:n0 + 128, :]), reads=["X1"], writes=["dbgt"])
        k.dma("sp", lambda e: e.dma_start(out=dbg_t.ap(), in_=t2[:]), reads=["dbgt"], writes=["dbg"])
        k.finish(["dbg"])
        return
    if SPARSE:
        with k.phase():
            phase6b_blocks(nc, k, sp)
        with k.phase():
            phase6c_dispatch(nc, k, sp)
        with k.phase():
            phase7_sparse(nc, k, w1, b1, w2, sp, identb)
        with k.phase():
            phase8_final(nc, k, X1, FD, GTd, b2, ln2, MOD, out, sp)
    else:
        with k.phase():
            phase7_moe(nc, k, w1, b1, w2, H2T, GD, FD)
        with k.phase():
            phase8_final(nc, k, X1, FD, GTd, b2, ln2, MOD, out)
    k.finish(["out"])


def phase0_mod(nc, k, cvec, w_ada, b_ada, MOD):
    cT = k.sb("cT", [128, 8, 3], F32)
    for r in range(3):
        k.dma("sp", lambda e, r=r: e.dma_start(
            out=cT[:, :, r], in_=cvec[r].rearrange("(kc p) -> p kc", p=128),
            allow_slow_non_contiguous=True), writes=["cT"])
    sT = k.sb("sT", [128, 8, 3], F32)
    k.op("act", lambda e: e.activation(out=sT[:], in_=cT[:], func=AF.Silu), reads=["cT"], writes=["sT"])
    bada = k.sb("bada", [3, 6 * D], F32)
    for r in range(3):
        k.dma("sp", lambda e, r=r: e.dma_start(out=bada[r:r + 1, :], in_=b_ada.ap()), writes=["bada"])
    modsb = k.sb("modsb", [3, 6 * D], F32)
    wblk = [k.sb("wadab%d" % i, [128, 8, 512], F32) for i in range(2)]
    pm = k.ps("pmod", [3, 512])
    for cb in range(12):
        wb = wblk[cb % 2]
        wn = "wadab%d" % (cb % 2)
        k.dma("sp", lambda e, cb=cb, wb=wb: e.dma_start(
            out=wb[:], in_=w_ada[:, cb * 512:(cb + 1) * 512].rearrange("(kc p) n -> p kc n", p=128)),
            writes=[wn])
        for kc in range(8):
            k.op("pe", lambda e, kc=kc, wb=wb: e.matmul(pm[:], lhsT=sT[:, kc, :], rhs=wb[:, kc, :],
                                                       start=(kc == 0), stop=(kc == 7)),
                 reads=["sT", wn], writes=["pmod"])
        k.op("dve", lambda e, cb=cb: e.tensor_tensor(out=modsb[:, cb * 512:(cb + 1) * 512], in0=pm[:],
                                                    in1=bada[:, cb * 512:(cb + 1) * 512], op=ALU.add),
             reads=["pmod", "bada"], writes=["modsb"])
    k.dma("sp", lambda e: e.dma_start(out=MOD.ap(), in_=modsb[:]), reads=["modsb"], writes=["MOD"])


def load_fm_vec(k, name, src_ap_1d, ncols, q="sp"):
    t = k.sb(name, [128, ncols], F32)
    k.dma(q, lambda e: e.dma_start(out=t[:], in_=src_ap_1d.rearrange("(j p) -> p j", p=128),
                                   allow_slow_non_contiguous=True), reads=["MOD"], writes=[name])
    return t


def phase1_proj(nc, k, x, ctx_in, ln0, MOD, w_in, mu, PFM, ident):
    g0F = load_fm_vec(k, "g0F", ln0[0], 8)
    b0F = load_fm_vec(k, "b0F", ln0[1], 8)
    modF = [load_fm_vec(k, "modF%d" % r, MOD[r], 48) for r in range(3)]
    S1 = k.sb("S1", [128, 3, 8], F32)
    B1 = k.sb("B1", [128, 3, 8], F32)
    for r in range(3):
        k.op("dve", lambda e, r=r: e.scalar_tensor_tensor(out=S1[:, r, :], in0=modF[r][:, 8:16], scalar=1.0,
                                                         in1=g0F[:], op0=ALU.add, op1=ALU.mult),
             reads=["modF%d" % r, "g0F"], writes=["S1"])
        k.op("dve", lambda e, r=r: e.scalar_tensor_tensor(out=B1[:, r, :], in0=modF[r][:, 8:16], scalar=1.0,
                                                         in1=b0F[:], op0=ALU.add, op1=ALU.mult),
             reads=["modF%d" % r, "b0F"], writes=["B1"])
        k.op("dve", lambda e, r=r: e.tensor_tensor(out=B1[:, r, :], in0=B1[:, r, :], in1=modF[r][:, 0:8],
                                                  op=ALU.add),
             reads=["modF%d" % r, "B1"], writes=["B1"])
    muF = k.sb("muF", [128, 16], F32)
    k.dma("sp", lambda e: e.dma_start(out=muF[:], in_=mu.ap().rearrange("o p -> p o"),
                                      allow_slow_non_contiguous=True), writes=["muF"])
    omm = k.sb("omm", [128, 16], F32)
    mu025 = k.sb("mu025", [128, 16], F32)
    mu05 = k.sb("mu05", [128, 16], F32)
    k.op("dve", lambda e: e.tensor_scalar(out=omm[:], in0=muF[:], scalar1=-1.0, scalar2=1.0,
                                          op0=ALU.mult, op1=ALU.add), reads=["muF"], writes=["omm"])
    k.op("dve", lambda e: e.tensor_scalar(out=mu025[:], in0=muF[:], scalar1=0.25, scalar2=None,
                                          op0=ALU.mult), reads=["muF"], writes=["mu025"])
    k.op("dve", lambda e: e.tensor_scalar(out=mu05[:], in0=muF[:], scalar1=0.5, scalar2=None,
                                          op0=ALU.mult), reads=["muF"], writes=["mu05"])
    wbf = k.sb("winbf", [128, 8, INW], BF16)
    for kc in range(8):
        for hf in range(2):
            k.dma("pool", lambda e, kc=kc, hf=hf: e.dma_start(
                out=wbf[:, kc, hf * 1280:(hf + 1) * 1280],
                in_=w_in[kc * 128:(kc + 1) * 128, hf * 1280:(hf + 1) * 1280]), writes=["winbf"])
    hT = k.sb("hT", [128, 8, SEQ], BF16)
    xt = [k.sb("xt%d" % i, [128, D], F32) for i in range(2)]
    xn = [k.sb("xn%d" % i, [128, D], F32) for i in range(2)]
    st = k.sb("bnst", [128, 2, 6], F32)
    mv = k.sb("bnmv", [128, 2], F32)
    rstd = k.sb("rstd", [128, 1], F32)
    pT = [k.ps("pT%d" % i, [128, 8, 128]) for i in range(2)]
    pmm = [k.ps("pmm%d" % i, [128, 512]) for i in range(2)]
    pch = [k.sb("pch%d" % i, [128, SEQ], F32) for i in range(2)]
    acc = k.sb("shacc", [128, SEQ], F32)
    mch = k.sb("mch", [128, SEQ], F32)
    ti = 0
    ci = 0
    mi = 0
    for b in range(BPC):
        for seg in range(2):
            ntok = CTX if seg == 0 else SEQ
            r = 2 if seg == 0 else b
            off = 0 if seg == 0 else CTX
            src = ctx_in[b] if seg == 0 else x[b]
            for t in range(ntok // 128):
                X, XN, PT = xt[ti % 2], xn[ti % 2], pT[ti % 2]
                xk, xnk, ptk = "xt%d" % (ti % 2), "xn%d" % (ti % 2), "pT%d" % (ti % 2)
                ti += 1
                k.dma("sp", lambda e, X=X, t=t, src=src: e.dma_start(out=X[:], in_=src[t * 128:(t + 1) * 128, :]),
                      writes=[xk])
                for hf in range(2):
                    k.op("dve", lambda e, X=X, hf=hf: e.bn_stats(out=st[:, hf, :], in_=X[:, hf * 512:(hf + 1) * 512]),
                         reads=[xk], writes=["bnst"])
                k.op("dve", lambda e: e.bn_aggr(out=mv[:], in_=st[:].rearrange("p a b -> p (a b)")),
                     reads=["bnst"], writes=["bnmv"])
                k.op("act", lambda e: e.activation(out=rstd[:], in_=mv[:, 1:2], func=AF.Sqrt, bias=LN_EPS),
                     reads=["bnmv"], writes=["rstd"])
                k.op("dve", lambda e: e.reciprocal(out=rstd[:], in_=rstd[:]), reads=["rstd"], writes=["rstd"])
                k.op("dve", lambda e, X=X, XN=XN: e.tensor_scalar(out=XN[:], in0=X[:], scalar1=mv[:, 0:1],
                                                                 scalar2=rstd[:, 0:1], op0=ALU.subtract,
                                                                 op1=ALU.mult),
                     reads=[xk, "bnmv", "rstd"], writes=[xnk])
                for kc in range(8):
                    k.op("pe", lambda e, XN=XN, PT=PT, kc=kc: e.transpose(out=PT[:, kc, :],
                                                                         in_=XN[:, kc * 128:(kc + 1) * 128],
                                                                         identity=ident[:]),
                         reads=[xnk, "ident"], writes=[ptk])
                for kc in range(8):
                    if kc % 2 == 0:
                        k.op("act", lambda e, PT=PT, kc=kc, t=t, r=r: e.activation(
                            out=hT[:, kc, t * 128:(t + 1) * 128], in_=PT[:, kc, :], func=AF.Identity,
                            scale=S1[:, r, kc:kc + 1], bias=B1[:, r, kc:kc + 1]),
                            reads=[ptk, "S1", "B1"], writes=["hT"])
                    else:
                        k.op("dve", lambda e, PT=PT, kc=kc, t=t, r=r: e.tensor_scalar(
                            out=hT[:, kc, t * 128:(t + 1) * 128], in0=PT[:, kc, :],
                            scalar1=S1[:, r, kc:kc + 1], scalar2=B1[:, r, kc:kc + 1],
                            op0=ALU.mult, op1=ALU.add),
                            reads=[ptk, "S1", "B1"], writes=["hT"])
            noc = 16 if seg == 0 else 20
            tbw = min(512, ntok)
            for oc in range(noc):
                PCH = pch[ci % 2]
                pk = "pch%d" % (ci % 2)
                ci += 1
                for tb in range(ntok // tbw):
                    PM = pmm[mi % 2]
                    pmk = "pmm%d" % (mi % 2)
                    mi += 1
                    for kc in range(8):
                        k.op("pe", lambda e, PM=PM, kc=kc, oc=oc, tb=tb, tbw=tbw: e.matmul(
                            PM[:, 0:tbw], lhsT=wbf[:, kc, oc * 128:(oc + 1) * 128],
                            rhs=hT[:, kc, tb * tbw:(tb + 1) * tbw], start=(kc == 0), stop=(kc == 7)),
                            reads=["winbf", "hT"], writes=[pmk])
                    k.op("act", lambda e, PM=PM, PCH=PCH, tb=tb, tbw=tbw: e.activation(
                        out=PCH[:, tb * tbw:(tb + 1) * tbw], in_=PM[:, 0:tbw], func=AF.Copy),
                        reads=[pmk], writes=[pk])
                dst = PFM[b, oc * 128:(oc + 1) * 128, off:off + ntok]
                if oc >= 16:
                    k.dma("sp", lambda e, PCH=PCH, dst=dst, ntok=ntok: e.dma_start(out=dst, in_=PCH[:, 0:ntok]),
                          reads=[pk], writes=["PFM"])
                    continue
                if seg == 1:
                    s3 = PCH[:].rearrange("p (r c) -> p r c", c=GRID)
                    a3 = acc[:].rearrange("p (r c) -> p r c", c=GRID)
                    k.op("pool", lambda e, a3=a3: e.memset(a3[:, 0:1, :], 0.0), writes=["shacc"])
                    k.op("act", lambda e, a3=a3, s3=s3: e.activation(out=a3[:, 1:, :], in_=s3[:, 0:GRID - 1, :],
                                                                     func=AF.Copy),
                         reads=[pk], writes=["shacc"])
                    k.op("pool", lambda e, a3=a3, s3=s3: e.tensor_tensor(out=a3[:, 0:GRID - 1, :],
                                                                        in0=a3[:, 0:GRID - 1, :],
                                                                        in1=s3[:, 1:, :], op=ALU.add),
                         reads=[pk, "shacc"], writes=["shacc"])
                    k.op("dve", lambda e, a3=a3, s3=s3: e.tensor_tensor(out=a3[:, :, 1:], in0=a3[:, :, 1:],
                                                                       in1=s3[:, :, 0:GRID - 1], op=ALU.add),
                         reads=[pk, "shacc"], writes=["shacc"])
                    k.op("dve", lambda e, a3=a3, s3=s3: e.tensor_tensor(out=a3[:, :, 0:GRID - 1],
                                                                       in0=a3[:, :, 0:GRID - 1],
                                                                       in1=s3[:, :, 1:], op=ALU.add),
                         reads=[pk, "shacc"], writes=["shacc"])
                    msc = mu025
                    mk = "mu025"
                else:
                    k.op("pool", lambda e: e.memset(acc[:, 0:1], 0.0), writes=["shacc"])
                    k.op("act", lambda e, PCH=PCH: e.activation(out=acc[:, 1:CTX], in_=PCH[:, 0:CTX - 1], func=AF.Copy),
                         reads=[pk], writes=["shacc"])
                    k.op("dve", lambda e, PCH=PCH: e.tensor_tensor(out=acc[:, 0:CTX - 1], in0=acc[:, 0:CTX - 1],
                                                                  in1=PCH[:, 1:CTX], op=ALU.add),
                         reads=[pk, "shacc"], writes=["shacc"])
                    msc = mu05
                    mk = "mu05"
                k.op("dve", lambda e, msc=msc, oc=oc, ntok=ntok: e.tensor_scalar(
                    out=mch[:, 0:ntok], in0=acc[:, 0:ntok], scalar1=msc[:, oc:oc + 1], scalar2=None, op0=ALU.mult),
                    reads=["shacc", mk], writes=["mch"])
                k.op("dve", lambda e, PCH=PCH, oc=oc, ntok=ntok: e.scalar_tensor_tensor(
                    out=mch[:, 0:ntok], in0=PCH[:, 0:ntok], scalar=omm[:, oc:oc + 1], in1=mch[:, 0:ntok],
                    op0=ALU.mult, op1=ALU.add),
                    reads=[pk, "omm", "mch"], writes=["mch"])
                k.dma("sp", lambda e, dst=dst, ntok=ntok: e.dma_start(out=dst, in_=mch[:, 0:ntok]),
                      reads=["mch"], writes=["PFM"])


Q_KKNEG, Q_R, Q_V = 0, 1, 2
Q_W, Q_BB, Q_KD = 3, 4, 5


def phase2_prepare(nc, k, PFM, vecs, w2d_d, a2_d, g2_d, bones_d, SC, GT):
    vF = load_fm_vec(k, "vF", vecs.ap().rearrange("a n -> (a n)"), 44)
    W2D = k.sb("W2D", [128, C], F32)
    A2 = k.sb("A2", [128, C], F32)
    G2 = [k.sb("G2_%d" % d, [128, C], F32) for d in range(2)]
    bones = k.sb("bones", [128, 128], F32)
    k.dma("sp", lambda e: e.dma_start(out=W2D[:], in_=w2d_d.ap()), writes=["W2D"])
    k.dma("sp", lambda e: e.dma_start(out=A2[:], in_=a2_d.ap()), writes=["A2"])
    for d in range(2):
        k.dma("sp", lambda e, d=d: e.dma_start(out=G2[d][:], in_=g2_d[d]), writes=["G2_%d" % d])
    k.dma("sp", lambda e: e.dma_start(out=bones[:], in_=bones_d.ap()), writes=["bones"])
    omka = k.sb("omka", [128, 4], F32)
    k.op("dve", lambda e: e.tensor_scalar(out=omka[:], in0=vF[:, 20:24], scalar1=-1.0, scalar2=1.0,
                                          op0=ALU.mult, op1=ALU.add), reads=["vF"], writes=["omka"])
    TB = 512
    mt = [k.sb("mt%d" % i, [128, 16, TB], F32) for i in range(2)]
    ot = [k.sb("ot%d" % i, [128, 11, TB], F32) for i in range(2)]
    twd = k.sb("twd", [128, TB], F32)
    sgd = k.sb("sgd", [128, 2, TB], F32)
    tmp = k.sb("p2tmp", [128, TB], F32)
    tmp2 = k.sb("p2tmp2", [128, TB], F32)
    aic = k.sb("aic", [128, TB], F32)
    pp = [k.ps("pp%d" % i, [128, TB]) for i in range(4)]
    ppi = 0
    bi = 0
    oi = 0
    for b in range(BPC):
        blocks = [(0, CTX)] + [(CTX + i * TB, TB) for i in range(SEQ // TB)]
        for (n0, tb) in blocks:
            M = mt[bi % 2]
            mk = "mt%d" % (bi % 2)
            bi += 1
            for half in range(2):
                k.dma("sp", lambda e, M=M, half=half, n0=n0, tb=tb, b=b: e.dma_start(
                    out=M[:, half * 8:(half + 1) * 8, 0:tb],
                    in_=PFM[b, half * 1024:(half + 1) * 1024, n0:n0 + tb].rearrange("(c p) n -> p c n", p=128)),
                    reads=["PFM"], writes=[mk])
            k.op("act", lambda e, M=M, tb=tb: e.activation(out=twd[:, 0:tb], in_=M[:, 12, 0:tb], func=AF.Tanh),
                 reads=[mk], writes=["twd"])
            k.op("act", lambda e, M=M, tb=tb: e.activation(out=sgd[:, :, 0:tb], in_=M[:, 14:16, 0:tb],
                                                          func=AF.Sigmoid), reads=[mk], writes=["sgd"])
            for hp in range(4):
                O = ot[oi % 2]
                ok = "ot%d" % (oi % 2)
                oi += 1
                kt = M[:, 4 + hp, 0:tb]
                k.op("dve", lambda e, O=O, kt=kt, hp=hp, tb=tb: e.tensor_scalar(
                    out=O[:, 9, 0:tb], in0=kt, scalar1=vF[:, 16 + hp:17 + hp], scalar2=None, op0=ALU.mult),
                    reads=[mk, "vF"], writes=[ok])
                k.op("dve", lambda e, O=O, tb=tb: e.tensor_tensor(out=tmp[:, 0:tb], in0=O[:, 9, 0:tb],
                                                                 in1=O[:, 9, 0:tb], op=ALU.mult),
                     reads=[ok], writes=["p2tmp"])
                P = pp[ppi % 4]
                pk_ = "pp%d" % (ppi % 4)
                ppi += 1
                k.op("pe", lambda e, P=P, tb=tb: e.matmul(P[:, 0:tb], lhsT=bones[:], rhs=tmp[:, 0:tb],
                                                         start=True, stop=True),
                     reads=["bones", "p2tmp"], writes=[pk_])
                k.op("act", lambda e, P=P, tb=tb: e.activation(out=tmp2[:, 0:tb], in_=P[:, 0:tb], func=AF.Sqrt),
                     reads=[pk_], writes=["p2tmp2"])
                k.op("dve", lambda e, tb=tb: e.tensor_scalar(out=tmp2[:, 0:tb], in0=tmp2[:, 0:tb], scalar1=1e-12,
                                                            scalar2=None, op0=ALU.max),
                     reads=["p2tmp2"], writes=["p2tmp2"])
                k.op("dve", lambda e, tb=tb: e.reciprocal(out=tmp2[:, 0:tb], in_=tmp2[:, 0:tb]),
                     reads=["p2tmp2"], writes=["p2tmp2"])
                k.op("dve", lambda e, O=O, tb=tb: e.tensor_tensor(out=O[:, 9, 0:tb], in0=O[:, 9, 0:tb],
                                                                 in1=tmp2[:, 0:tb], op=ALU.mult),
                     reads=[ok, "p2tmp2"], writes=[ok])
                k.op("pool", lambda e, O=O, tb=tb: e.tensor_scalar(out=O[:, Q_KKNEG, 0:tb], in0=O[:, 9, 0:tb],
                                                                  scalar1=-1.0, scalar2=None, op0=ALU.mult),
                     reads=[ok], writes=[ok])
                for d in range(2):
                    P = pp[ppi % 4]
                    pk_ = "pp%d" % (ppi % 4)
                    ppi += 1
                    k.op("pe", lambda e, P=P, d=d, hp=hp, tb=tb: e.matmul(
                        P[:, 0:tb], lhsT=W2D[d * 64:(d + 1) * 64, hp * 128:(hp + 1) * 128],
                        rhs=twd[d * 64:(d + 1) * 64, 0:tb], start=True, stop=True),
                        reads=["W2D", "twd"], writes=[pk_])
                    k.op("act", lambda e, P=P, d=d, hp=hp, tb=tb: e.activation(
                        out=tmp[:, 0:tb], in_=P[:, 0:tb], func=AF.Sigmoid,
                        bias=vF[:, d * 4 + hp:d * 4 + hp + 1]), reads=[pk_, "vF"], writes=["p2tmp"])
                    k.op("act", lambda e, O=O, d=d, tb=tb: e.activation(
                        out=O[:, Q_W + 3 * d, 0:tb], in_=tmp[:, 0:tb], func=AF.Exp, scale=-0.6065306597126334),
                        reads=["p2tmp"], writes=[ok])
                    P = pp[ppi % 4]
                    pk_ = "pp%d" % (ppi % 4)
                    ppi += 1
                    k.op("pe", lambda e, P=P, d=d, hp=hp, tb=tb, M=M: e.matmul(
                        P[:, 0:tb], lhsT=A2[d * 64:(d + 1) * 64, hp * 128:(hp + 1) * 128],
                        rhs=M[d * 64:(d + 1) * 64, 13, 0:tb], start=True, stop=True),
                        reads=["A2", mk], writes=[pk_])
                    k.op("act", lambda e, P=P, d=d, hp=hp, tb=tb: e.activation(
                        out=aic[:, 0:tb], in_=P[:, 0:tb], func=AF.Sigmoid,
                        bias=vF[:, 8 + d * 4 + hp:8 + d * 4 + hp + 1]), reads=[pk_, "vF"], writes=["aic"])
                    k.op("dve", lambda e, O=O, d=d, tb=tb: e.tensor_tensor(
                        out=O[:, Q_BB + 3 * d, 0:tb], in0=O[:, 9, 0:tb], in1=aic[:, 0:tb], op=ALU.mult),
                        reads=[ok, "aic"], writes=[ok])
                    k.op("dve", lambda e, hp=hp, tb=tb: e.tensor_scalar(
                        out=tmp2[:, 0:tb], in0=aic[:, 0:tb], scalar1=vF[:, 20 + hp:21 + hp],
                        scalar2=omka[:, hp:hp + 1], op0=ALU.mult, op1=ALU.add),
                        reads=["aic", "vF", "omka"], writes=["p2tmp2"])
                    k.op("dve", lambda e, O=O, d=d, tb=tb, kt=kt: e.tensor_tensor(
                        out=O[:, Q_KD + 3 * d, 0:tb], in0=kt, in1=tmp2[:, 0:tb], op=ALU.mult),
                        reads=[mk, "p2tmp2"], writes=[ok])
                    if n0 >= CTX:
                        P = pp[ppi % 4]
                        pk_ = "pp%d" % (ppi % 4)
                        ppi += 1
                        k.op("pe", lambda e, P=P, d=d, hp=hp, tb=tb: e.matmul(
                            P[:, 0:tb], lhsT=G2[d][:, hp * 128:(hp + 1) * 128], rhs=sgd[:, d, 0:tb],
                            start=True, stop=True), reads=["G2_%d" % d, "sgd"], writes=[pk_])
                        k.op("act", lambda e, P=P, O=O, d=d, tb=tb: e.activation(
                            out=O[:, 9 + d, 0:tb] if False else O[:, 10, 0:tb], in_=P[:, 0:tb], func=AF.Copy),
                            reads=[pk_], writes=[ok])
                        k.dma("act", lambda e, O=O, b=b, d=d, hp=hp, n0=n0, tb=tb: e.dma_start(
                            out=GT[b, d, hp, :, n0 - CTX:n0 - CTX + tb], in_=O[:, 10, 0:tb]),
                            reads=[ok], writes=["GT"])
                k.dma("sp", lambda e, M=M, b=b, hp=hp, n0=n0, tb=tb: e.dma_start(
                    out=SC[b, Q_R, hp, :, n0:n0 + tb], in_=M[:, hp, 0:tb]), reads=[mk], writes=["SC"])
                k.dma("sp", lambda e, M=M, b=b, hp=hp, n0=n0, tb=tb: e.dma_start(
                    out=SC[b, Q_V, hp, :, n0:n0 + tb], in_=M[:, 8 + hp, 0:tb]), reads=[mk], writes=["SC"])
                for q in (Q_KKNEG, 3, 4, 5, 6, 7, 8):
                    k.dma("sp", lambda e, O=O, b=b, hp=hp, n0=n0, tb=tb, q=q: e.dma_start(
                        out=SC[b, q, hp, :, n0:n0 + tb], in_=O[:, q, 0:tb]), reads=[ok], writes=["SC"])


TC = 64
NSTEP = NV
NSL = 16


def phase3_scan(nc, k, SC, YSr, ident, identb, oh_d, mask_d):
    OH = k.sb("s_oh", [128, TC, 40], BF16)
    MK = k.sb("s_mask", [104, 512], F32)
    k.dma("sp", lambda e: e.dma_start(out=OH[:], in_=oh_d.ap()), writes=["s_oh"])
    k.dma("sp", lambda e: e.dma_start(out=MK[:], in_=mask_d.ap()), writes=["s_mask"])
    zer = k.sb("s_zero", [128, 512], BF16)
    k.op("pool", lambda e: e.memset(zer[:], 0.0), writes=["s_zero"])
    CO = [k.sb("CO%d" % i, [128, 6, 16, TC], F32) for i in range(2)]
    LA = [k.sb("LA%d" % i, [128, TC, 2, 32], BF16) for i in range(2)]
    CS = [k.sb("CSt%d" % i, [128, TC, 2, 104], BF16) for i in range(2)]
    VT = [k.sb("VT%d" % i, [TC, 2, 8, 2, 64], BF16) for i in range(2)]
    VT2 = [k.sb("VT2_%d" % i, [128, 2, 8, 64], BF16) for i in range(2)]
    for i in range(2):
        k.op("pool", lambda e, i=i: e.memset(LA[i][:], 0.0), writes=["LA%d" % i])
        k.op("pool", lambda e, i=i: e.memset(CS[i][:], 0.0), writes=["CSt%d" % i])
    Tbf = [k.sb("Tbf%d" % h, [128, 512], BF16) for h in range(2)]
    CT = [k.sb("CT%d" % h, [104, 128], BF16) for h in range(2)]
    RR = [[k.sb("RR%d_%d" % (h, p), [104, NSL, 512], BF16) for p in range(2)] for h in range(2)]
    PT = [k.ps("PT%d" % h, [128, 512]) for h in range(2)]
    PA = [k.ps("PA%d" % h, [128, 512]) for h in range(2)]
    PC = [k.ps("PC%d" % h, [104, 128], BF16) for h in range(2)]
    PVT = k.ps("PVT", [TC, 4, 128])
    for h in range(2):
        k.op("pe", lambda e, h=h: e.matmul(PT[h][:], lhsT=zer[:, 0:128], rhs=zer[:], start=True, stop=True),
             reads=["s_zero"], writes=["PT%d" % h])
        k.op("pe", lambda e, h=h: e.matmul(PA[h][:], lhsT=zer[:, 0:128], rhs=zer[:], start=True, stop=True),
             reads=["s_zero"], writes=["PA%d" % h])

    def nat_lo(c, d):
        if d == 0:
            return c * TC
        if c < CTX // TC:
            return CTX - TC * (c + 1)
        return CTX + SEQ + CTX - TC * (c + 1)

    def prep_chunk(c, nsteps=TC):
        COt, LAt, CSt, VTt = CO[c % 2], LA[c % 2], CS[c % 2], VT[c % 2]
        ck, lk, sk, vk = "CO%d" % (c % 2), "LA%d" % (c % 2), "CSt%d" % (c % 2), "VT%d" % (c % 2)
        last = (nsteps == 1)
        for q in range(6):
            if last and q != 1:
                continue
            for d in range(2):
                nlo = nat_lo(c, d) if not last else (NV if d == 0 else CTX - 1)
                if last:
                    lo, hi, dst0 = (NV - 1, NV, 0) if d == 0 else (CTX, CTX + 1, 0)
                elif q == 1:
                    if d == 0:
                        lo, hi, dst0 = max(nlo - 1, 0), nlo + TC - 1, (1 if nlo == 0 else 0)
                    else:
                        lo, hi, dst0 = nlo + 1, min(nlo + TC + 1, NV), 0
                else:
                    lo, hi, dst0 = nlo, nlo + TC, 0
                for b in range(BPC):
                    srcq = q if q < 3 else q + 3 * d
                    k.dma("sp", lambda e, COt=COt, q=q, d=d, b=b, srcq=srcq, lo=lo, hi=hi, dst0=dst0: e.dma_start(
                        out=COt[:, q, d * 8 + b * 4:d * 8 + b * 4 + 4, dst0:dst0 + hi - lo],
                        in_=SC[b, srcq, :, :, lo:hi].rearrange("g p t -> p g t"),
                        allow_slow_non_contiguous=True),
                        reads=["SC"], writes=[ck])
        for h in range(2):
            for hh in range(2):
                ps = slice(hh * 64, (hh + 1) * 64)
                if not last:
                    k.op("pool", lambda e, h=h, hh=hh, ps=ps, COt=COt, LAt=LAt: e.tensor_copy(
                        out=LAt[ps, :, h, hh:16:2], in_=COt[ps, 0, h * 8:(h + 1) * 8, :].rearrange("p g t -> p t g")),
                        reads=[ck], writes=[lk])
                k.op("pool", lambda e, h=h, hh=hh, ps=ps, COt=COt, LAt=LAt: e.tensor_copy(
                    out=LAt[ps, :, h, 16 + hh:32:2], in_=COt[ps, 1, h * 8:(h + 1) * 8, :].rearrange("p g t -> p t g")),
                    reads=[ck], writes=[lk])
                if last:
                    continue
                k.op("pool", lambda e, h=h, hh=hh, ps=ps, COt=COt, CSt=CSt: e.tensor_copy(
                    out=CSt[ps, :, h, hh:16:2], in_=COt[ps, 4, h * 8:(h + 1) * 8, :].rearrange("p g t -> p t g")),
                    reads=[ck], writes=[sk])
                k.op("pool", lambda e, h=h, hh=hh, ps=ps, COt=COt, CSt=CSt: e.tensor_copy(
                    out=CSt[ps, :, h, 64 + 32 * hh:72 + 32 * hh],
                    in_=COt[ps, 5, h * 8:(h + 1) * 8, :].rearrange("p g t -> p t g")),
                    reads=[ck], writes=[sk])
        if last:
            return
        for h in range(2):
            for half in range(2):
                for gg in range(4):
                    g = h * 8 + half * 4 + gg
                    k.op("pe", lambda e, COt=COt, g=g, gg=gg: e.transpose(out=PVT[:, gg, :], in_=COt[:, 2, g, :],
                                                                          identity=ident[:]),
                         reads=[ck, "ident"], writes=["PVT"])
                k.op("act", lambda e, VTt=VTt, h=h, half=half: e.activation(
                    out=VTt[:, h, half * 4:(half + 1) * 4, :, :].rearrange("t g a i -> t g (a i)"), in_=PVT[:],
                    func=AF.Copy), reads=["PVT"], writes=[vk])
        V2t = VT2[c % 2]
        for hh in range(2):
            k.dma("act", lambda e, VTt=VTt, V2t=V2t, hh=hh: e.dma_start(
                out=V2t[hh * 64:(hh + 1) * 64, :, :, :], in_=VTt[:, :, :, hh, :]),
                reads=[vk], writes=["VT2_%d" % (c % 2)])

    def w_ap(COt, h, tau):
        base = COt[:]
        pstep = base.ap[0][0]
        off = base.offset + 3 * 16 * TC + h * 8 * TC + tau
        return bass.AP(base.tensor, off, [[pstep, 128], [TC, 8], [0, 64]])

    nchunk = NSTEP // TC
    prep_chunk(0)
    for c in range(nchunk + 1):
        last = (c == nchunk)
        if c + 1 <= nchunk:
            prep_chunk(c + 1, TC if c + 1 < nchunk else 1)
        COt, LAt, CSt, VTt = CO[c % 2], LA[c % 2], CS[c % 2], VT[c % 2]
        ck, lk, sk, vk = "CO%d" % (c % 2), "LA%d" % (c % 2), "CSt%d" % (c % 2), "VT%d" % (c % 2)
        V2t, v2k = VT2[c % 2], "VT2_%d" % (c % 2)
        for j in range(1 if last else TC):
            s_ = c * TC + j
            ls = s_ - 1 - CTX
            par = (ls // NSL) % 2 if ls >= 0 else 0
            slot = ls % NSL if ls >= 0 else 0
            taus = [0, 0] if last else [j, TC - 1 - j]
            Rs = [RR[h][par] for h in range(2)]
            rks = ["RR%d_%d" % (h, par) for h in range(2)]
            for h in range(2):
                k.op("act", lambda e, h=h: e.activation(out=Tbf[h][:], in_=PT[h][:], func=AF.Copy),
                     reads=["PT%d" % h], writes=["Tbf%d" % h])
            for h in range(2):
                tau = taus[h]
                if not last:
                    k.op("pe", lambda e, h=h, tau=tau, V2t=V2t: e.matmul(
                        PA[h][64:104, :], lhsT=OH[:, tau, :], rhs=V2t[:, h, :, :], start=True, stop=True),
                        reads=["s_oh", v2k], writes=["PA%d" % h])
                    k.op("pe", lambda e, h=h, tau=tau, CSt=CSt: e.transpose(out=PC[h][:], in_=CSt[:, tau, h, :],
                                                                           identity=identb[:]),
                         reads=[sk, "identb"], writes=["PC%d" % h])
                k.op("pe", lambda e, h=h, tau=tau, LAt=LAt: e.matmul(PA[h][0:32, :], lhsT=LAt[:, tau, h, :],
                                                                    rhs=Tbf[h][:], start=True, stop=True),
                     reads=[lk, "Tbf%d" % h], writes=["PA%d" % h])
            if not last:
                for h in range(2):
                    k.op("act", lambda e, h=h: e.activation(out=CT[h][:], in_=PC[h][:], func=AF.Copy),
                         reads=["PC%d" % h], writes=["CT%d" % h])
                for h in range(2):
                    tau = taus[h]
                    k.op("dve", lambda e, h=h, tau=tau, COt=COt: e.tensor_tensor(
                        out=PT[h][:].rearrange("p (g i) -> p g i", g=8), in0=PT[h][:].rearrange("p (g i) -> p g i", g=8),
                        in1=w_ap(COt, h, tau), op=ALU.mult), reads=["PT%d" % h, ck], writes=["PT%d" % h])
            for h in range(2):
                k.op("dve", lambda e, h=h, R=Rs[h], slot=slot: e.tensor_tensor(out=R[:, slot, :], in0=PA[h][0:104, :],
                                                                              in1=MK[:], op=ALU.mult),
                     reads=["PA%d" % h, "s_mask"], writes=[rks[h]])
            if not last:
                for h in range(2):
                    k.op("pe", lambda e, h=h, R=Rs[h], slot=slot: e.matmul(PT[h][:], lhsT=CT[h][:], rhs=R[:, slot, :],
                                                                          start=False, stop=True),
                         reads=["CT%d" % h, rks[h], "PT%d" % h], writes=["PT%d" % h])
            if ls >= 0 and slot == NSL - 1:
                for h in range(2):
                    for gl in range(8):
                        g = h * 8 + gl
                        k.dma("sp", lambda e, R=Rs[h], gl=gl, g=g, ls=ls: e.dma_start(
                            out=YSr[:, ls - NSL + 1:ls + 1, g * 64:(g + 1) * 64],
                            in_=R[16 + gl * 2:18 + gl * 2, :, gl * 64:(gl + 1) * 64]),
                            reads=[rks[h]], writes=["YSr"])


def phase4_rwkv_out(nc, k, SC, GT, YSr, vecs, bones_d, ident, jmat_d, MIXT):
    vF = load_fm_vec(k, "vF4", vecs.ap().rearrange("a n -> (a n)"), 44)
    bones = k.sb("bones4", [128, 128], F32)
    jmat = k.sb("jmat", [128, 128], F32)
    k.dma("sp", lambda e: e.dma_start(out=bones[:], in_=bones_d.ap()), writes=["bones4"])
    k.dma("sp", lambda e: e.dma_start(out=jmat[:], in_=jmat_d.ap()), writes=["jmat"])
    TB = 512
    Yt = [k.sb("Yt%d" % i, [128, 2, 64], BF16) for i in range(4)]
    identb4 = k.sb("identb4", [128, 128], BF16)
    jmatb = k.sb("jmatb", [128, 128], BF16)
    k.op("pool", lambda e: e.tensor_copy(out=identb4[:], in_=ident[:]), reads=["ident"], writes=["identb4"])
    k.op("pool", lambda e: e.tensor_copy(out=jmatb[:], in_=jmat[:]), reads=["jmat"], writes=["jmatb"])
    rt = k.sb("r4", [128, TB], F32)
    vt = k.sb("v4", [128, TB], F32)
    kdt = [k.sb("kd4_%d" % i, [128, TB], F32) for i in range(2)]
    gt = [k.sb("g4_%d" % i, [128, TB], F32) for i in range(2)]
    yF = k.sb("yF", [128, TB], F32)
    ym = k.sb("ym", [128, TB], F32)
    sq = k.sb("sq4", [128, TB], F32)
    rs = k.sb("rs4", [128, TB], F32)
    t1 = k.sb("t14", [128, TB], F32)
    acc = k.sb("acc4", [128, TB], F32)
    rwbf = [k.sb("rwbf%d" % i, [128, TB], BF16) for i in range(2)]
    PSY = k.ps("PSY", [128, TB])
    PM = k.ps("PM4", [128, TB])
    PV_ = k.ps("PV4", [128, TB])
    PB = k.ps("PB4", [128, TB])
    yi = 0
    oi = 0
    for b in range(BPC):
        for hp in range(4):
            for blk in range(SEQ // TB):
                n0 = blk * TB
                k.dma("sp", lambda e, b=b, hp=hp, n0=n0: e.dma_start(
                    out=rt[:], in_=SC[b, Q_R, hp, :, CTX + n0:CTX + n0 + TB]), reads=["SC"], writes=["r4"])
                k.dma("sp", lambda e, b=b, hp=hp, n0=n0: e.dma_start(
                    out=vt[:], in_=SC[b, Q_V, hp, :, CTX + n0:CTX + n0 + TB]), reads=["SC"], writes=["v4"])
                for d in range(2):
                    g = d * 8 + b * 4 + hp
                    KD, GTt = kdt[d], gt[d]
                    k.dma("sp", lambda e, b=b, hp=hp, n0=n0, d=d, KD=KD: e.dma_start(
                        out=KD[:], in_=SC[b, Q_KD + 3 * d, hp, :, CTX + n0:CTX + n0 + TB]),
                        reads=["SC"], writes=["kd4_%d" % d])
                    k.dma("sp", lambda e, b=b, hp=hp, n0=n0, d=d, GTt=GTt: e.dma_start(
                        out=GTt[:], in_=GT[b, d, hp, :, n0:n0 + TB]), reads=["GT"], writes=["g4_%d" % d])
                    for sub in range(4):
                        nn = n0 + sub * 128
                        ls0 = nn if d == 0 else SEQ - 128 - nn
                        Y = Yt[yi % 4]
                        yk = "Yt%d" % (yi % 4)
                        yi += 1
                        for hh in range(2):
                            k.dma("act", lambda e, Y=Y, hh=hh, ls0=ls0, g=g: e.dma_start(
                                out=Y[:, hh, :], in_=YSr[hh, ls0:ls0 + 128, g * 64:(g + 1) * 64]),
                                reads=["YSr"], writes=[yk])
                        rm, rmk = (identb4, "identb4") if d == 0 else (jmatb, "jmatb")
                        k.op("pe", lambda e, Y=Y, sub=sub, rm=rm: e.matmul(
                            PSY[:, sub * 128:(sub + 1) * 128], lhsT=Y[:].rearrange("p h i -> p (h i)"),
                            rhs=rm[:], start=True, stop=True), reads=[yk, rmk], writes=["PSY"])
                    k.op("act", lambda e: e.activation(out=yF[:], in_=PSY[:], func=AF.Copy),
                         reads=["PSY"], writes=["yF"])
                    k.op("pe", lambda e: e.matmul(PM[:], lhsT=bones[:], rhs=yF[:], start=True, stop=True),
                         reads=["bones4", "yF"], writes=["PM4"])
                    k.op("dve", lambda e: e.scalar_tensor_tensor(out=ym[:], in0=PM[:], scalar=-1.0 / 64, in1=yF[:],
                                                                 op0=ALU.mult, op1=ALU.add),
                         reads=["PM4", "yF"], writes=["ym"])
                    k.op("pool", lambda e: e.tensor_tensor(out=sq[:], in0=ym[:], in1=ym[:], op=ALU.mult),
                         reads=["ym"], writes=["sq4"])
                    k.op("pe", lambda e: e.matmul(PV_[:], lhsT=bones[:], rhs=sq[:], start=True, stop=True),
                         reads=["bones4", "sq4"], writes=["PV4"])
                    k.op("act", lambda e: e.activation(out=rs[:], in_=PV_[:], func=AF.Sqrt, scale=1.0 / 64,
                                                       bias=GN_EPS), reads=["PV4"], writes=["rs4"])
                    k.op("dve", lambda e: e.reciprocal(out=rs[:], in_=rs[:]), reads=["rs4"], writes=["rs4"])
                    k.op("dve", lambda e: e.tensor_tensor(out=ym[:], in0=ym[:], in1=rs[:], op=ALU.mult),
                         reads=["ym", "rs4"], writes=["ym"])
                    k.op("dve", lambda e, hp=hp: e.tensor_scalar(out=ym[:], in0=ym[:], scalar1=vF[:, 24 + hp:25 + hp],
                                                                scalar2=vF[:, 28 + hp:29 + hp], op0=ALU.mult,
                                                                op1=ALU.add), reads=["ym", "vF4"], writes=["ym"])
                    k.op("pool", lambda e, KD=KD: e.tensor_tensor(out=t1[:], in0=rt[:], in1=KD[:], op=ALU.mult),
                         reads=["r4", "kd4_%d" % d], writes=["t14"])
                    k.op("pool", lambda e, d=d, hp=hp: e.tensor_scalar(
                        out=t1[:], in0=t1[:], scalar1=vF[:, 32 + d * 4 + hp:33 + d * 4 + hp], scalar2=None,
                        op0=ALU.mult), reads=["t14", "vF4"], writes=["t14"])
                    k.op("pe", lambda e: e.matmul(PB[:], lhsT=bones[:], rhs=t1[:], start=True, stop=True),
                         reads=["bones4", "t14"], writes=["PB4"])
                    k.op("dve", lambda e: e.tensor_tensor(out=t1[:], in0=PB[:], in1=vt[:], op=ALU.mult),
                         reads=["PB4", "v4", "t14"], writes=["t14"])
                    k.op("dve", lambda e: e.tensor_tensor(out=ym[:], in0=ym[:], in1=t1[:], op=ALU.add),
                         reads=["ym", "t14"], writes=["ym"])
                    if d == 0:
                        k.op("dve", lambda e, GTt=GTt: e.tensor_tensor(out=acc[:], in0=ym[:], in1=GTt[:], op=ALU.mult),
                             reads=["ym", "g4_0"], writes=["acc4"])
                    else:
                        k.op("dve", lambda e, GTt=GTt: e.tensor_tensor(out=ym[:], in0=ym[:], in1=GTt[:], op=ALU.mult),
                             reads=["ym", "g4_1"], writes=["ym"])
                        RW = rwbf[oi % 2]
                        rk_ = "rwbf%d" % (oi % 2)
                        oi += 1
                        k.op("dve", lambda e, RW=RW: e.tensor_tensor(out=RW[:], in0=acc[:], in1=ym[:], op=ALU.add),
                             reads=["acc4", "ym"], writes=[rk_])
                        k.dma("sp", lambda e, RW=RW, b=b, hp=hp, n0=n0: e.dma_start(
                            out=MIXT[b, hp, :, n0:n0 + TB], in_=RW[:]), reads=[rk_], writes=["MIXT"])


def phase5_fft(nc, k, PFM, wfno_d, vecs, ccbd_d, scbd_d, cosm_d, nsin_d, MIXT):
    vF = load_fm_vec(k, "vF5", vecs.ap().rearrange("a n -> (a n)"), 44)
    ccbd = k.sb("ccbd", [128, 128], F32)
    scbd = k.sb("scbd", [128, 128], F32)
    k.dma("sp", lambda e: e.dma_start(out=ccbd[:], in_=ccbd_d.ap()), writes=["ccbd"])
    k.dma("sp", lambda e: e.dma_start(out=scbd[:], in_=scbd_d.ap()), writes=["scbd"])
    wst = k.sb("wst", [128, 4, 128], F32)
    k.op("pool", lambda e: e.memset(wst[:], 0.0), writes=["wst"])
    for fc in range(4):
        for g2 in range(2):
            k.dma("sp", lambda e, fc=fc, g2=g2: e.dma_start(
                out=wst[g2 * 64:(g2 + 1) * 64, fc, g2 * 64:(g2 + 1) * 64], in_=wfno_d[fc * 2 + g2]),
                writes=["wst"])
    ABD = k.sb("ABD", [128, 4, 2, 128], BF16)
    PA = k.ps("PA5", [128, 2, 128])
    for fc in range(4):
        k.op("pe", lambda e, fc=fc: e.matmul(PA[:, 0, :], lhsT=ccbd[:], rhs=wst[:, fc, :], start=True, stop=True),
             reads=["ccbd", "wst"], writes=["PA5"])
        k.op("pe", lambda e, fc=fc: e.matmul(PA[:, 1, :], lhsT=scbd[:], rhs=wst[:, fc, :], start=True, stop=True),
             reads=["scbd", "wst"], writes=["PA5"])
        k.op("act", lambda e, fc=fc: e.activation(out=ABD[:, fc, :, :], in_=PA[:], func=AF.Copy),
             reads=["PA5"], writes=["ABD"])
    U = k.sb("U5", [128, 32, 2, 512], BF16)
    fbf = [k.sb("fbf%d" % i, [128, SEQ], BF16) for i in range(2)]
    CS = k.sb("CS5", [128, 2, 32, 512], BF16)
    PU = [k.ps("PU5_%d" % i, [128, 2, 128]) for i in range(2)]
    PO = [k.ps("PO5_%d" % i, [128, 512]) for i in range(2)]
    ob = [k.sb("ob5_%d" % i, [128, 512], BF16) for i in range(2)]
    pui = 0
    poi = 0
    for b in range(BPC):
        for fc in range(4):
            FB = fbf[fc % 2]
            fk = "fbf%d" % (fc % 2)
            for hf in range(2):
                k.dma("pool", lambda e, FB=FB, b=b, fc=fc, hf=hf: e.dma_start(
                    out=FB[:, hf * 2048:(hf + 1) * 2048],
                    in_=PFM[b, SHW + fc * 128:SHW + (fc + 1) * 128, CTX + hf * 2048:CTX + (hf + 1) * 2048]),
                    reads=["PFM"], writes=[fk])
            for tt in range(32):
                P = PU[pui % 2]
                pk_ = "PU5_%d" % (pui % 2)
                pui += 1
                for ab in range(2):
                    k.op("pe", lambda e, P=P, FB=FB, tt=tt, fc=fc, ab=ab: e.matmul(
                        P[:, ab, :], lhsT=FB[:, tt * 128:(tt + 1) * 128], rhs=ABD[:, fc, ab, :],
                        start=True, stop=True), reads=[fk, "ABD"], writes=[pk_])
                k.op("act" if tt % 2 else "dve", (lambda e, P=P, tt=tt, fc=fc: e.activation(
                    out=U[:, tt, :, fc * 128:(fc + 1) * 128], in_=P[:], func=AF.Copy)) if tt % 2 else
                    (lambda e, P=P, tt=tt, fc=fc: e.tensor_copy(out=U[:, tt, :, fc * 128:(fc + 1) * 128], in_=P[:])),
                    reads=[pk_], writes=["U5"])
        for kb in range(8):
            for hf in range(2):
                k.dma("sp", lambda e, kb=kb, hf=hf: e.dma_start(
                    out=CS[:, 0, hf * 16:(hf + 1) * 16, :].rearrange("p t n -> p (t n)"),
                    in_=cosm_d[kb, :, hf * 8192:(hf + 1) * 8192]), writes=["CS5"])
                k.dma("act", lambda e, kb=kb, hf=hf: e.dma_start(
                    out=CS[:, 1, hf * 16:(hf + 1) * 16, :].rearrange("p t n -> p (t n)"),
                    in_=nsin_d[kb, :, hf * 8192:(hf + 1) * 8192]), writes=["CS5"])
            for fc in range(4):
                P = PO[poi % 2]
                pk_ = "PO5_%d" % (poi % 2)
                OB = ob[poi % 2]
                obk = "ob5_%d" % (poi % 2)
                poi += 1
                for tt in range(32):
                    for ab in range(2):
                        k.op("pe", lambda e, P=P, tt=tt, ab=ab, fc=fc: e.matmul(
                            P[:], lhsT=U[:, tt, ab, fc * 128:(fc + 1) * 128], rhs=CS[:, ab, tt, :],
                            start=(tt == 0 and ab == 0), stop=(tt == 31 and ab == 1)),
                            reads=["U5", "CS5"], writes=[pk_])
                k.op("act", lambda e, P=P, OB=OB, fc=fc: e.activation(
                    out=OB[:], in_=P[:], func=AF.Identity, bias=vF[:, 40 + fc:41 + fc]),
                    reads=[pk_, "vF5"], writes=[obk])
                k.dma("sp", lambda e, OB=OB, b=b, fc=fc, kb=kb: e.dma_start(
                    out=MIXT[b, 4 + fc, :, kb * 512:(kb + 1) * 512], in_=OB[:]), reads=[obk], writes=["MIXT"])


def bcast_rows(ap1d, n, parts=128):
    return bass.AP(ap1d.tensor, ap1d.offset, [[0, parts], [1, n]])


def ln_stats(k, src, srck, st, mv, rstd, sfx):
    for hf in range(2):
        k.op("dve", lambda e, hf=hf: e.bn_stats(out=st[:, hf, :], in_=src[:, hf * 512:(hf + 1) * 512]),
             reads=[srck], writes=["bnst" + sfx])
    k.op("dve", lambda e: e.bn_aggr(out=mv[:], in_=st[:].rearrange("p a b -> p (a b)")),
         reads=["bnst" + sfx], writes=["bnmv" + sfx])
    k.op("act", lambda e: e.activation(out=rstd[:], in_=mv[:, 1:2], func=AF.Sqrt, bias=LN_EPS),
         reads=["bnmv" + sfx], writes=["rstd" + sfx])
    k.op("dve", lambda e: e.reciprocal(out=rstd[:], in_=rstd[:]), reads=["rstd" + sfx], writes=["rstd" + sfx])


def phase6_outproj(nc, k, x, ln0, ln1, MOD, w_out, w_router, b_router, MIXT, ident, X1, H2T, GTd, GD=None, sp=None):
    def brow(name, ap1d, n=D, q="sp"):
        t = k.sb(name, [128, n], F32)
        k.dma(q, lambda e: e.dma_start(out=t[:], in_=bcast_rows(ap1d, n)), reads=["MOD"], writes=[name])
        return t
    g0B = brow("g0B", ln0[0])
    b0B = brow("b0B", ln0[1])
    g1B = brow("g1B", ln1[0])
    b1B = brow("b1B", ln1[1])
    brB = brow("brB", b_router[0], NE)
    wobf = k.sb("wobf", [128, 8, D], BF16)
    for kc in range(8):
        k.dma("pool", lambda e, kc=kc: e.dma_start(out=wobf[:, kc, :], in_=w_out[kc * 128:(kc + 1) * 128, :]),
              writes=["wobf"])
    wr = k.sb("wr", [128, 8, NE], F32)
    k.dma("sp", lambda e: e.dma_start(out=wr[:], in_=w_router.ap().rearrange("(c p) n -> p c n", p=128)),
          writes=["wr"])
    Xt = [k.sb("x6_%d" % i, [128, D], F32) for i in range(2)]
    MX = [k.sb("mx6_%d" % i, [128, 8, 128], BF16) for i in range(2)]
    x0 = k.sb("x0_6", [128, D], F32)
    z = k.sb("z6", [128, D], F32)
    x1 = [k.sb("x1_6_%d" % i, [128, D], F32) for i in range(2)]
    h2 = k.sb("h2_6", [128, D], F32)
    h2T = k.sb("h2T6", [128, 8, 128], F32)
    h2Tb = [k.sb("h2Tb6_%d" % i, [128, 8, 128], BF16) for i in range(2)]
    st = k.sb("bnst6", [128, 2, 6], F32)
    mv = k.sb("bnmv6", [128, 2], F32)
    rstd = k.sb("rstd6", [128, 1], F32)
    lg = k.sb("lg6", [128, NE], F32)
    mx8 = k.sb("mx8", [128, 8], F32)
    nmx = k.sb("nmx", [128, 1], F32)
    msk = k.sb("msk6", [128, NE], F32)
    ex = k.sb("ex6", [128, NE], F32)
    ssum = k.sb("ssum6", [128, 1], F32)
    G = k.sb("G6", [128, NE], F32)
    gTs = [k.sb("gTs6_%d" % i, [NE, 128], F32) for i in range(2)]
    PO = k.ps("PO6", [128, D])
    PT = k.ps("PT6", [128, 8, 128])
    PL = k.ps("PL6", [128, NE])
    PG = k.ps("PG6", [NE, 128])
    if sp is not None:
        ustr = k.sb("ustr", [128, 128], F32)
        k.dma("sp", lambda e: e.dma_start(out=ustr[:], in_=sp["ustr_d"].ap()), writes=["ustr"])
        ones6 = k.sb("ones6", [128, 128], F32)
        k.op("pool", lambda e: e.memset(ones6[:], 1.0), writes=["ones6"])
        carry = sp["carry"]
        k.op("pool", lambda e: e.memset(carry[:], 0.0), writes=["carry"])
        rk6 = [k.sb("rk6_%d" % i, [128, NE], F32) for i in range(2)]
        h2b = [k.sb("h2b6_%d" % i, [128, D], BF16) for i in range(2)]
        PR = k.ps("PR6", [128, NE])
        PCn = k.ps("PCn6", [1, NE])
    ti = 0
    for b in range(BPC):
        gt1B = brow("gt1B%d" % b, MOD[b, 2048:3072])
        sc2B = brow("sc2B%d" % b, MOD[b, 4096:5120])
        sh2B = brow("sh2B%d" % b, MOD[b, 3072:4096])
        k.op("dve", lambda e, sc2B=sc2B: e.tensor_scalar(out=sc2B[:], in0=sc2B[:], scalar1=1.0, scalar2=None,
                                                        op0=ALU.add), reads=["sc2B%d" % b], writes=["sc2B%d" % b])
        for t in range(NT6):
            X, M_, X1t, HB, GTS = Xt[ti % 2], MX[ti % 2], x1[ti % 2], h2Tb[ti % 2], gTs[ti % 2]
            xk, mk, x1k, hbk, gtk = ("x6_%d" % (ti % 2), "mx6_%d" % (ti % 2), "x1_6_%d" % (ti % 2),
                                     "h2Tb6_%d" % (ti % 2), "gTs6_%d" % (ti % 2))
            ti += 1
            tok0 = t * 128
            k.dma("sp", lambda e, X=X, b=b, tok0=tok0: e.dma_start(out=X[:], in_=x[b, tok0:tok0 + 128, :]),
                  writes=[xk])
            k.dma("act", lambda e, M_=M_, b=b, tok0=tok0: e.dma_start(
                out=M_[:], in_=MIXT[b, :, :, tok0:tok0 + 128].rearrange("c p n -> p c n")),
                reads=["MIXT"], writes=[mk])
            ln_stats(k, X, xk, st, mv, rstd, "6")
            k.op("dve", lambda e, X=X: e.tensor_scalar(out=x0[:], in0=X[:], scalar1=mv[:, 0:1], scalar2=rstd[:, 0:1],
                                                      op0=ALU.subtract, op1=ALU.mult),
                 reads=[xk, "bnmv6", "rstd6"], writes=["x0_6"])
            k.op("pool", lambda e: e.tensor_tensor(out=x0[:], in0=x0[:], in1=g0B[:], op=ALU.mult),
                 reads=["x0_6", "g0B"], writes=["x0_6"])
            k.op("pool", lambda e: e.tensor_tensor(out=x0[:], in0=x0[:], in1=b0B[:], op=ALU.add),
                 reads=["x0_6", "b0B"], writes=["x0_6"])
            if STOP6 <= 1:
                continue
            for hf in range(2):
                for kc in range(8):
                    k.op("pe", lambda e, M_=M_, hf=hf, kc=kc: e.matmul(
                        PO[:, hf * 512:(hf + 1) * 512], lhsT=M_[:, kc, :], rhs=wobf[:, kc, hf * 512:(hf + 1) * 512],
                        start=(kc == 0), stop=(kc == 7)), reads=[mk, "wobf"], writes=["PO6"])
            k.op("dve", lambda e, gt1B=gt1B: e.tensor_tensor(out=z[:], in0=PO[:], in1=gt1B[:], op=ALU.mult),
                 reads=["PO6", "gt1B%d" % b], writes=["z6"])
            k.op("dve", lambda e: e.scalar_tensor_tensor(out=z[:], in0=x0[:], scalar=ALPHA, in1=z[:],
                                                         op0=ALU.mult, op1=ALU.add),
                 reads=["x0_6", "z6"], writes=["z6"])
            ln_stats(k, z, "z6", st, mv, rstd, "6")
            k.op("dve", lambda e: e.tensor_scalar(out=z[:], in0=z[:], scalar1=mv[:, 0:1], scalar2=rstd[:, 0:1],
                                                  op0=ALU.subtract, op1=ALU.mult),
                 reads=["z6", "bnmv6", "rstd6"], writes=["z6"])
            k.op("pool", lambda e: e.tensor_tensor(out=z[:], in0=z[:], in1=g1B[:], op=ALU.mult),
                 reads=["z6", "g1B"], writes=["z6"])
            k.op("pool", lambda e, X1t=X1t: e.tensor_tensor(out=X1t[:], in0=z[:], in1=b1B[:], op=ALU.add),
                 reads=["z6", "b1B"], writes=[x1k])
            k.dma("sp", lambda e, X1t=X1t, b=b, tok0=tok0: e.dma_start(out=X1[b, tok0:tok0 + 128, :], in_=X1t[:]),
                  reads=[x1k], writes=["X1"])
            if STOP6 <= 2:
                continue
            k.op("pool", lambda e, X1t=X1t, sc2B=sc2B: e.tensor_tensor(out=h2[:], in0=X1t[:], in1=sc2B[:], op=ALU.mult),
                 reads=[x1k, "sc2B%d" % b], writes=["h2_6"])
            k.op("dve", lambda e, sh2B=sh2B: e.tensor_tensor(out=h2[:], in0=h2[:], in1=sh2B[:], op=ALU.add),
                 reads=["h2_6", "sh2B%d" % b], writes=["h2_6"])
            for kc in range(8):
                k.op("pe", lambda e, kc=kc: e.transpose(out=PT[:, kc, :], in_=h2[:, kc * 128:(kc + 1) * 128],
                                                        identity=ident[:]),
                     reads=["h2_6", "ident"], writes=["PT6"])
            if STOP6 <= 2.3:
                continue
            k.op("act", lambda e: e.activation(out=h2T[:], in_=PT[:], func=AF.Copy), reads=["PT6"], writes=["h2T6"])
            if STOP6 <= 2.5:
                continue
            for hb_ in range(2):
                k.op("pool", lambda e, HB=HB, hb_=hb_: e.tensor_copy(out=HB[:, hb_ * 4:(hb_ + 1) * 4, :],
                                                                     in_=h2T[:, hb_ * 4:(hb_ + 1) * 4, :]),
                     reads=["h2T6"], writes=[hbk])
            if STOP6 <= 2.7:
                continue
            k.dma("act", lambda e, HB=HB, b=b, tok0=tok0: e.dma_start(
                out=H2T[b, :, :, tok0:tok0 + 128].rearrange("c p n -> p c n"), in_=HB[:]),
                reads=[hbk], writes=["H2T"])
            if STOP6 <= 3:
                continue
            for kc in range(8):
                k.op("pe", lambda e, kc=kc: e.matmul(PL[:], lhsT=h2T[:, kc, :], rhs=wr[:, kc, :],
                                                     start=(kc == 0), stop=(kc == 7)),
                     reads=["h2T6", "wr"], writes=["PL6"])
            k.op("dve", lambda e: e.tensor_tensor(out=lg[:], in0=PL[:], in1=brB[:], op=ALU.add),
                 reads=["PL6", "brB"], writes=["lg6"])
            if STOP6 <= 4:
                continue
            k.op("dve", lambda e: e.max(out=mx8[:], in_=lg[:]), reads=["lg6"], writes=["mx8"])
            k.op("dve", lambda e: e.tensor_scalar(out=nmx[:], in0=mx8[:, 0:1], scalar1=-1.0, scalar2=None,
                                                  op0=ALU.mult), reads=["mx8"], writes=["nmx"])
            k.op("dve", lambda e: e.tensor_scalar(out=msk[:], in0=lg[:], scalar1=mx8[:, 3:4], scalar2=None,
                                                  op0=ALU.is_ge), reads=["lg6", "mx8"], writes=["msk6"])
            k.op("act", lambda e: e.activation(out=ex[:], in_=lg[:], func=AF.Exp, bias=nmx[:, 0:1]),
                 reads=["lg6", "nmx"], writes=["ex6"])
            k.op("dve", lambda e: e.tensor_tensor(out=ex[:], in0=ex[:], in1=msk[:], op=ALU.mult),
                 reads=["ex6", "msk6"], writes=["ex6"])
            k.op("dve", lambda e: e.reduce_sum(out=ssum[:], in_=ex[:], axis=AX.X), reads=["ex6"], writes=["ssum6"])
            k.op("dve", lambda e: e.reciprocal(out=ssum[:], in_=ssum[:]), reads=["ssum6"], writes=["ssum6"])
            k.op("dve", lambda e: e.tensor_scalar(out=G[:], in0=ex[:], scalar1=ssum[:, 0:1], scalar2=None,
                                                  op0=ALU.mult), reads=["ex6", "ssum6"], writes=["G6"])
            if STOP6 <= 5:
                continue
            if sp is not None:
                tgs = b * SEQ + tok0
                RK, H2B = rk6[ti % 2], h2b[ti % 2]
                rkk, h2k = "rk6_%d" % (ti % 2), "h2b6_%d" % (ti % 2)
                k.op("pe", lambda e: e.matmul(PR[:], lhsT=ustr[:], rhs=msk[:], start=True, stop=False),
                     reads=["ustr", "msk6"], writes=["PR6"])
                k.op("pe", lambda e: e.matmul(PR[:], lhsT=ones6[0:1, :], rhs=carry[0:1, :], start=False, stop=True),
                     reads=["ones6", "carry"], writes=["PR6"])
                k.op("pe", lambda e: e.matmul(PCn[:], lhsT=ones6[:, 0:1], rhs=msk[:], start=True, stop=True),
                     reads=["ones6", "msk6"], writes=["PCn6"])
                k.op("act", lambda e, RK=RK: e.activation(out=RK[:], in_=PR[:], func=AF.Copy),
                     reads=["PR6"], writes=[rkk])
                k.op("dve", lambda e: e.tensor_tensor(out=carry[:], in0=PCn[:], in1=carry[:], op=ALU.add),
                     reads=["PCn6", "carry"], writes=["carry"])
                k.dma("sp", lambda e, RK=RK, tgs=tgs: e.dma_start(out=sp["RANKD"][tgs:tgs + 128, :], in_=RK[:]),
                      reads=[rkk], writes=["RANKD"])
                k.dma("sp", lambda e, tgs=tgs: e.dma_start(out=sp["MSKD"][tgs:tgs + 128, :], in_=msk[:]),
                      reads=["msk6"], writes=["MSKD"])
                k.op("pool", lambda e, H2B=H2B: e.tensor_copy(out=H2B[:], in_=h2[:]), reads=["h2_6"], writes=[h2k])
                k.dma("act", lambda e, H2B=H2B, tgs=tgs: e.dma_start(out=sp["H2D"][tgs:tgs + 128, :], in_=H2B[:]),
                      reads=[h2k], writes=["H2D"])
            if GD is not None:
                tgd = b * SEQ + tok0
                k.dma("sp", lambda e, tgd=tgd: e.dma_start(out=GD[tgd:tgd + 128, :], in_=G[:]),
                      reads=["G6"], writes=["GD"])
            k.op("pe", lambda e: e.transpose(out=PG[:], in_=G[:], identity=ident[:]),
                 reads=["G6", "ident"], writes=["PG6"])
            k.op("act", lambda e, GTS=GTS: e.activation(out=GTS[:], in_=PG[:], func=AF.Copy),
                 reads=["PG6"], writes=[gtk])
            tg0 = b * SEQ + tok0
            k.dma("sp", lambda e, GTS=GTS, tg0=tg0: e.dma_start(out=GTd[:, tg0:tg0 + 128], in_=GTS[:]),
                  reads=[gtk], writes=["GTd"])


def phase7_moe(nc, k, w1, b1, w2, H2T, GD, FD):
    b1GL = k.sb("b1GL", [128, NE, 8, 2], F32)
    for eg in range(NE7):
        k.dma("sp", lambda e, eg=eg: e.dma_start(
            out=b1GL[:, eg, :, :],
            in_=b1[eg, :].rearrange("(m p two) -> p m two", p=128, two=2),
            allow_slow_non_contiguous=True), writes=["b1GL"])
    Gall = k.sb("Gall", [128, BPC * SEQ // 128, NE], F32)
    for t8 in range(8):
        k.dma("sp", lambda e, t8=t8: e.dma_start(
            out=Gall[:, t8 * 8:(t8 + 1) * 8, :],
            in_=GD[t8 * 1024:(t8 + 1) * 1024, :].rearrange("(t p) e -> p t e", p=128)),
            reads=["GD"], writes=["Gall"])
    W1 = [k.sb("W1_%d" % i, [128, 8, 2048], BF16) for i in range(2)]
    W2 = [k.sb("W2_%d" % i, [128, 8, D], BF16) for i in range(2)]
    hT = [k.sb("hT7_%d" % i, [128, 8, 512], BF16) for i in range(2)]
    gb = [k.sb("gb7_%d" % i, [128, 512], F32) for i in range(2)]
    glu = [k.sb("glu7_%d" % i, [128, 512], F32) for i in range(2)]
    sg = [k.sb("sg7_%d" % i, [128, 512], F32) for i in range(2)]
    lin = [k.sb("lin7_%d" % i, [128, 512], F32) for i in range(2)]
    ACTT = [k.sb("ACTT%d" % i, [128, 8, 512], BF16) for i in range(2)]
    yb = [k.sb("yb7_%d" % i, [128, D], F32) for i in range(2)]
    PGL = [k.ps("PGL%d" % i, [128, 512]) for i in range(4)]
    PY = k.ps("PY7", [128, D])
    pi = 0
    bi = 0
    mi = 0
    yi = 0
    for ex in range(NE7):
        W1t, W2t = W1[ex % 2], W2[ex % 2]
        w1k, w2k = "W1_%d" % (ex % 2), "W2_%d" % (ex % 2)
        for kc in range(8):
            k.dma("pool", lambda e, W1t=W1t, ex=ex, kc=kc: e.dma_start(
                out=W1t[:, kc, :], in_=w1[ex, kc * 128:(kc + 1) * 128, :]), writes=[w1k])
        for kc in range(8):
            k.dma("pool", lambda e, W2t=W2t, ex=ex, kc=kc: e.dma_start(
                out=W2t[:, kc, :], in_=w2[ex, kc * 128:(kc + 1) * 128, :]), writes=[w2k])
        for blk in range(NB7):
            b, n0 = divmod(blk * 512, SEQ)
            tg0 = blk * 512
            HT, AT = hT[bi % 2], ACTT[bi % 2]
            hk, gk, ak = "hT7_%d" % (bi % 2), "gb7_%d" % (bi % 2), "ACTT%d" % (bi % 2)
            bi += 1
            k.dma("sp", lambda e, HT=HT, b=b, n0=n0: e.dma_start(
                out=HT[:], in_=H2T[b, :, :, n0:n0 + 512].rearrange("c p n -> p c n")), reads=["H2T"], writes=[hk])
            for m in range(8):
                PGt, PLt = PGL[pi % 4], PGL[(pi + 1) % 4]
                pgk, plk = "PGL%d" % (pi % 4), "PGL%d" % ((pi + 1) % 4)
                pi += 2
                GLU, SG, LIN = glu[mi % 2], sg[mi % 2], lin[mi % 2]
                glk, sgk, lik = "glu7_%d" % (mi % 2), "sg7_%d" % (mi % 2), "lin7_%d" % (mi % 2)
                mi += 1
                for kc in range(8):
                    k.op("pe", lambda e, PGt=PGt, W1t=W1t, HT=HT, kc=kc, m=m: e.matmul(
                        PGt[:], lhsT=W1t[:, kc, 2 * m * 128:2 * (m + 1) * 128:2], rhs=HT[:, kc, :],
                        start=(kc == 0), stop=(kc == 7)), reads=[w1k, hk], writes=[pgk])
                for kc in range(8):
                    k.op("pe", lambda e, PLt=PLt, W1t=W1t, HT=HT, kc=kc, m=m: e.matmul(
                        PLt[:], lhsT=W1t[:, kc, 2 * m * 128 + 1:2 * (m + 1) * 128:2], rhs=HT[:, kc, :],
                        start=(kc == 0), stop=(kc == 7)), reads=[w1k, hk], writes=[plk])
                k.op("dve", lambda e, PGt=PGt, GLU=GLU, ex=ex, m=m: e.tensor_scalar(
                    out=GLU[:], in0=PGt[:], scalar1=b1GL[:, ex, m, 0:1], scalar2=7.0, op0=ALU.add, op1=ALU.min),
                    reads=[pgk, "b1GL"], writes=[glk])
                k.op("act", lambda e, GLU=GLU, SG=SG: e.activation(out=SG[:], in_=GLU[:], func=AF.Sigmoid, scale=1.702),
                     reads=[glk], writes=[sgk])
                k.op("dve", lambda e, PLt=PLt, LIN=LIN, ex=ex, m=m: e.tensor_scalar(
                    out=LIN[:], in0=PLt[:], scalar1=b1GL[:, ex, m, 1:2], scalar2=7.0, op0=ALU.add, op1=ALU.min),
                    reads=[plk, "b1GL"], writes=[lik])
                k.op("dve", lambda e, LIN=LIN: e.tensor_scalar(out=LIN[:], in0=LIN[:], scalar1=-7.0, scalar2=1.0,
                                                              op0=ALU.max, op1=ALU.add), reads=[lik], writes=[lik])
                k.op("dve", lambda e, GLU=GLU, SG=SG: e.tensor_tensor(out=SG[:], in0=GLU[:], in1=SG[:], op=ALU.mult),
                     reads=[glk, sgk], writes=[sgk])
                k.op("dve", lambda e, SG=SG, LIN=LIN, AT=AT, m=m: e.tensor_tensor(out=AT[:, m, :], in0=SG[:],
                                                                              in1=LIN[:], op=ALU.mult),
                     reads=[sgk, lik], writes=[ak])
            for tt in range(4):
                YB = yb[yi % 2]
                ybk = "yb7_%d" % (yi % 2)
                yi += 1
                for hf in range(2):
                    for m in range(8):
                        k.op("pe", lambda e, AT=AT, W2t=W2t, tt=tt, hf=hf, m=m: e.matmul(
                            PY[:, hf * 512:(hf + 1) * 512], lhsT=AT[:, m, tt * 128:(tt + 1) * 128],
                            rhs=W2t[:, m, hf * 512:(hf + 1) * 512], start=(m == 0), stop=(m == 7)),
                            reads=[ak, w2k], writes=["PY7"])
                r0 = tg0 + tt * 128
                k.op("act", lambda e, YB=YB, r0=r0, ex=ex: e.activation(
                    out=YB[:], in_=PY[:], func=AF.Copy, scale=Gall[:, r0 // 128, ex:ex + 1]),
                    reads=["PY7", "Gall"], writes=[ybk])
                if ex == 0:
                    k.dma("pool", lambda e, YB=YB, r0=r0: e.dma_start(out=FD[r0:r0 + 128, :], in_=YB[:]),
                          reads=[ybk], writes=["FD%d" % (r0 // 128)])
                else:
                    k.dma("pool", lambda e, YB=YB, r0=r0: e.dma_start(out=FD[r0:r0 + 128, :], in_=YB[:],
                                                                     accum_op=ALU.add),
                          reads=[ybk], writes=["FD%d" % (r0 // 128)])


def make_sp(nc, k, ustr_d, thr_d, jg_d, kcp_d):
    sp = dict(ustr_d=ustr_d, thr_d=thr_d, jg_d=jg_d, kcp_d=kcp_d)
    sp["RANKD"] = nc.dram_tensor("RANKD", [BPC * SEQ, NE], F32, kind="Internal")
    sp["MSKD"] = nc.dram_tensor("MSKD", [BPC * SEQ, NE], F32, kind="Internal")
    sp["H2D"] = nc.dram_tensor("H2D", [BPC * SEQ, D], BF16, kind="Internal")
    sp["XS"] = nc.dram_tensor("XS", [NSLOT, D], BF16, kind="Internal")
    sp["YS"] = nc.dram_tensor("YS", [NSLOT, D], F32, kind="Internal")
    sp["carry"] = k.sb("carry", [1, NE], F32)
    sp["BEP"] = k.sb("BEP", [128, 128], F32)
    sp["IDX"] = k.sb("IDX", [128, NBLK, 8], I32)
    sp["IDXB"] = k.sb("IDXB", [128, NBLK], I32)
    sp["SLOTI"] = k.sb("SLOTI", [128, BPC * SEQ // 128, 4], I32)
    sp["GK"] = k.sb("GK", [128, BPC * SEQ // 128, 4], F32)
    return sp


def phase6b_blocks(nc, k, sp):
    carry = sp["carry"]
    thr = k.sb("thr6b", [1, NE, 16], F32)
    jg = k.sb("jg6b", [1, NBLK, NE], F32)
    kcp = k.sb("kcp6b", [128, 8], F32)
    k.dma("sp", lambda e: e.dma_start(out=thr[:], in_=sp["thr_d"].ap()), writes=["thr6b"])
    k.dma("sp", lambda e: e.dma_start(out=jg[:], in_=sp["jg_d"].ap()), writes=["jg6b"])
    k.dma("sp", lambda e: e.dma_start(out=kcp[:], in_=sp["kcp_d"].ap()), writes=["kcp6b"])
    cmp_ = k.sb("cmp6b", [1, NE, 16], F32)
    nblk = k.sb("nblk6b", [1, NE], F32)
    bend = k.sb("bend6b", [1, NE], F32)
    one1 = k.sb("one6b", [1, 128], F32)
    k.op("pool", lambda e: e.memset(one1[:], 1.0), writes=["one6b"])
    row = k.sb("row6b", [1, 128], F32)
    cmp2 = k.sb("cmp6b2", [1, NBLK, NE], F32)
    k.op("dve", lambda e: e.tensor_tensor(out=cmp_[:], in0=carry[:].unsqueeze(2).to_broadcast([1, NE, 16]),
                                          in1=thr[:], op=ALU.is_gt), reads=["carry", "thr6b"], writes=["cmp6b"])
    k.op("dve", lambda e: e.reduce_sum(out=nblk[:], in_=cmp_[:], axis=AX.X), reads=["cmp6b"], writes=["nblk6b"])
    k.op("dve", lambda e: e.tensor_tensor_scan(out=bend[:], data0=one1[:, 0:NE], data1=nblk[:], initial=0.0,
                                               op0=ALU.mult, op1=ALU.add),
         reads=["one6b", "nblk6b"], writes=["bend6b"])
    k.op("dve", lambda e: e.tensor_tensor(out=row[:, 96:128], in0=bend[:], in1=nblk[:], op=ALU.subtract),
         reads=["bend6b", "nblk6b"], writes=["row6b"])
    k.op("dve", lambda e: e.tensor_scalar(out=row[:, 96:128], in0=row[:, 96:128], scalar1=512.0, scalar2=None,
                                          op0=ALU.mult), reads=["row6b"], writes=["row6b"])
    k.op("dve", lambda e: e.tensor_tensor(out=cmp2[:], in0=bend[:].unsqueeze(1).to_broadcast([1, NBLK, NE]),
                                          in1=jg[:], op=ALU.is_le), reads=["bend6b", "jg6b"], writes=["cmp6b2"])
    k.op("dve", lambda e: e.reduce_sum(out=row[:, 0:NBLK], in_=cmp2[:], axis=AX.X),
         reads=["cmp6b2", "row6b"], writes=["row6b"])
    k.op("dve", lambda e: e.tensor_scalar(out=row[:, 0:NBLK], in0=row[:, 0:NBLK], scalar1=float(NE - 1),
                                          scalar2=None, op0=ALU.min), reads=["row6b"], writes=["row6b"])
    skp = k.sb("skp6b", [1, NBLK], F32)
    k.op("pool", lambda e: e.memset(skp[:], 0.0), writes=["skp6b"])
    k.op("dve", lambda e: e.tensor_tensor(out=skp[:, 2:NBLK], in0=row[:, 2:NBLK], in1=row[:, 0:NBLK - 2],
                                          op=ALU.is_equal), reads=["row6b", "skp6b"], writes=["skp6b"])
    k.op("dve", lambda e: e.scalar_tensor_tensor(out=row[:, 0:NBLK], in0=skp[:], scalar=64.0, in1=row[:, 0:NBLK],
                                                 op0=ALU.mult, op1=ALU.add),
         reads=["row6b", "skp6b"], writes=["row6b"])
    PBc = k.ps("PBc6b", [128, 128])
    k.op("pe", lambda e: e.matmul(PBc[:], lhsT=one1[:], rhs=row[:], start=True, stop=True),
         reads=["one6b", "row6b"], writes=["PBc6b"])
    BEP = sp["BEP"]
    k.op("act", lambda e: e.activation(out=BEP[:], in_=PBc[:], func=AF.Copy), reads=["PBc6b"], writes=["BEP"])
    idf = k.sb("idf6b", [128, NBLK, 8], F32)
    k.op("dve", lambda e: e.tensor_scalar(out=idf[:], in0=BEP[:, 0:NBLK].unsqueeze(2).to_broadcast([128, NBLK, 8]),
                                          scalar1=1024.0, scalar2=None, op0=ALU.mult),
         reads=["BEP"], writes=["idf6b"])
    k.op("dve", lambda e: e.tensor_tensor(out=idf[:], in0=idf[:], in1=kcp[:].unsqueeze(1).to_broadcast([128, NBLK, 8]),
                                          op=ALU.add), reads=["idf6b", "kcp6b"], writes=["idf6b"])
    k.op("dve", lambda e: e.tensor_copy(out=sp["IDX"][:], in_=idf[:]), reads=["idf6b"], writes=["IDX"])
    k.op("dve", lambda e: e.tensor_copy(out=sp["IDXB"][:], in_=BEP[:, 0:NBLK]), reads=["BEP"], writes=["IDXB"])


def phase6c_dispatch(nc, k, sp):
    BEP = sp["BEP"]
    rk = [k.sb("rk6c_%d" % i, [128, NE], F32) for i in range(2)]
    mk = [k.sb("mk6c_%d" % i, [128, NE], F32) for i in range(2)]
    gg = [k.sb("gg6c_%d" % i, [128, NE], F32) for i in range(2)]
    hx = [k.sb("hx6c_%d" % i, [128, D], BF16) for i in range(2)]
    key = k.sb("key6c", [128, NE], F32)
    t1 = k.sb("t16c", [128, NE], F32)
    mx = k.sb("mx6c", [128, 8], F32)
    sl = k.sb("sl6c", [128, 4], F32)
    CBIG = 65536.0
    for t in range(NTT):
        RK, MK_, GG, HX = rk[t % 2], mk[t % 2], gg[t % 2], hx[t % 2]
        rkk, mkk, ggk, hxk = "rk6c_%d" % (t % 2), "mk6c_%d" % (t % 2), "gg6c_%d" % (t % 2), "hx6c_%d" % (t % 2)
        r0 = t * 128
        k.dma("sp", lambda e, RK=RK, r0=r0: e.dma_start(out=RK[:], in_=sp["RANKD"][r0:r0 + 128, :]),
              reads=["RANKD"], writes=[rkk])
        k.dma("sp", lambda e, MK_=MK_, r0=r0: e.dma_start(out=MK_[:], in_=sp["MSKD"][r0:r0 + 128, :]),
              reads=["MSKD"], writes=[mkk])
        k.dma("act", lambda e, GG=GG, r0=r0: e.dma_start(out=GG[:], in_=sp["GD"][r0:r0 + 128, :]),
              reads=["GD"], writes=[ggk])
        k.dma("act", lambda e, HX=HX, r0=r0: e.dma_start(out=HX[:], in_=sp["H2D"][r0:r0 + 128, :]),
              reads=["H2D"], writes=[hxk])
        k.op("dve", lambda e, RK=RK: e.tensor_tensor(out=key[:], in0=RK[:], in1=BEP[:, 96:128], op=ALU.add),
             reads=[rkk, "BEP"], writes=["key6c"])
        k.op("dve", lambda e: e.tensor_scalar(out=key[:], in0=key[:], scalar1=-1.0, scalar2=CBIG, op0=ALU.mult,
                                              op1=ALU.add), reads=["key6c"], writes=["key6c"])
        k.op("dve", lambda e, MK_=MK_: e.tensor_tensor(out=key[:], in0=key[:], in1=MK_[:], op=ALU.mult),
             reads=["key6c", mkk], writes=["key6c"])
        k.op("dve", lambda e, MK_=MK_: e.scalar_tensor_tensor(out=key[:], in0=MK_[:], scalar=-1.0, in1=key[:],
                                                            op0=ALU.add, op1=ALU.add),
             reads=["key6c", mkk], writes=["key6c"])
        k.op("dve", lambda e: e.max(out=mx[:], in_=key[:]), reads=["key6c"], writes=["mx6c"])
        k.op("dve", lambda e: e.tensor_scalar(out=sl[:], in0=mx[:, 0:4], scalar1=-1.0, scalar2=CBIG, op0=ALU.mult,
                                              op1=ALU.add), reads=["mx6c"], writes=["sl6c"])
        k.op("dve", lambda e, t=t: e.tensor_copy(out=sp["SLOTI"][:, t, :], in_=sl[:]), reads=["sl6c"], writes=["SLOTI"])
        for c4 in range(4):
            k.op("dve", lambda e, c4=c4: e.tensor_scalar(out=t1[:], in0=key[:], scalar1=mx[:, c4:c4 + 1], scalar2=None,
                                                        op0=ALU.is_equal), reads=["key6c", "mx6c"], writes=["t16c"])
            k.op("dve", lambda e, GG=GG: e.tensor_tensor(out=t1[:], in0=t1[:], in1=GG[:], op=ALU.mult),
                 reads=["t16c", ggk], writes=["t16c"])
            k.op("dve", lambda e, t=t, c4=c4: e.reduce_sum(out=sp["GK"][:, t, c4:c4 + 1], in_=t1[:], axis=AX.X),
                 reads=["t16c"], writes=["GK"])
        for c4 in range(4):
            k.dma("pool", lambda e, HX=HX, t=t, c4=c4: e.indirect_dma_start(
                out=sp["XS"][:, :], out_offset=bass.IndirectOffsetOnAxis(ap=sp["SLOTI"][:, t, c4:c4 + 1], axis=0),
                in_=HX[:], in_offset=None), reads=[hxk, "SLOTI"], writes=["XS"])


_BREG = {}


def _breg(e, val):
    key = (id(e), val)
    if key not in _BREG:
        _BREG[key] = e.to_reg(val)
    return _BREG[key]


def phase7_sparse(nc, k, w1, b1, w2, sp, identb):
    XS, YS = sp["XS"], sp["YS"]
    w1r = w1.ap().rearrange("e k n -> (e k) n")
    w2r = w2.ap().rearrange("e k n -> (e k) n")
    ones7 = k.sb("ones7", [1, 512], BF16)
    k.op("pool", lambda e: e.memset(ones7[:], 1.0), writes=["ones7"])
    W1 = [k.sb("W1_%d" % i, [128, 8, 2048], BF16) for i in range(2)]
    W2 = [k.sb("W2_%d" % i, [128, 8, D], BF16) for i in range(2)]
    B1 = [k.sb("B1_%d" % i, [128, 2048], BF16) for i in range(2)]
    xs = [k.sb("xs7_%d" % i, [128, 4, D], BF16) for i in range(2)]
    hT = [k.sb("hT7_%d" % i, [128, 8, 512], BF16) for i in range(2)]
    glu = [k.sb("glu7_%d" % i, [128, 512], F32) for i in range(2)]
    sg = [k.sb("sg7_%d" % i, [128, 512], F32) for i in range(2)]
    lin = [k.sb("lin7_%d" % i, [128, 512], F32) for i in range(2)]
    ACTT = [k.sb("ACTT%d" % i, [128, 8, 512], BF16) for i in range(2)]
    yb = [k.sb("yb7_%d" % i, [128, D], F32) for i in range(2)]
    PGL = [k.ps("PGL%d" % i, [128, 512]) for i in range(4)]
    PY = k.ps("PY7", [128, D])
    PX = [k.ps("PX7_%d" % i, [128, 512], BF16) for i in range(2)]
    pi = 0
    mi = 0
    yi = 0
    xi = 0
    for j in range(NBLK7):
        W1t, W2t, B1t, XSt, HT, AT = W1[j % 2], W2[j % 2], B1[j % 2], xs[j % 2], hT[j % 2], ACTT[j % 2]
        w1k, w2k, b1k, xsk, hk, ak = ("W1_%d" % (j % 2), "W2_%d" % (j % 2), "B1_%d" % (j % 2), "xs7_%d" % (j % 2),
                                      "hT7_%d" % (j % 2), "ACTT%d" % (j % 2))
        for kc in range(8):
            k.dma("pool", lambda e, W1t=W1t, j=j, kc=kc: e.indirect_dma_start(
                out=W1t[:, kc, :], out_offset=None, in_=w1r,
                in_offset=bass.IndirectOffsetOnAxis(ap=sp["IDX"][:, j, kc:kc + 1], axis=0),
                bounds_check=_breg(e, NE * D - 1), oob_is_err=False),
                reads=["IDX"], writes=[w1k])
        for kc in range(8):
            k.dma("pool", lambda e, W2t=W2t, j=j, kc=kc: e.indirect_dma_start(
                out=W2t[:, kc, :], out_offset=None, in_=w2r,
                in_offset=bass.IndirectOffsetOnAxis(ap=sp["IDX"][:, j, kc:kc + 1], axis=0),
                bounds_check=_breg(e, NE * D - 1), oob_is_err=False),
                reads=["IDX"], writes=[w2k])
        k.dma("pool", lambda e, B1t=B1t, j=j: e.indirect_dma_start(
            out=B1t[:], out_offset=None, in_=b1.ap(),
            in_offset=bass.IndirectOffsetOnAxis(ap=sp["IDXB"][:, j:j + 1], axis=0),
            bounds_check=_breg(e, NE - 1), oob_is_err=False),
            reads=["IDXB"], writes=[b1k])
        for tt in range(4):
            k.dma("sp" if tt % 2 == 0 else "act", lambda e, XSt=XSt, j=j, tt=tt: e.dma_start(
                out=XSt[:, tt, :], in_=XS[j * 512 + tt * 128:j * 512 + (tt + 1) * 128, :]),
                reads=["XS"], writes=[xsk])
        for kc in range(8):
            PXt = PX[xi % 2]
            pxk = "PX7_%d" % (xi % 2)
            xi += 1
            for tt in range(4):
                k.op("pe", lambda e, PXt=PXt, XSt=XSt, tt=tt, kc=kc: e.transpose(
                    out=PXt[:, tt * 128:(tt + 1) * 128], in_=XSt[:, tt, kc * 128:(kc + 1) * 128], identity=identb[:]),
                    reads=[xsk, "identb"], writes=[pxk])
            if kc % 2 == 0:
                k.op("act", lambda e, PXt=PXt, HT=HT, kc=kc: e.activation(out=HT[:, kc, :], in_=PXt[:], func=AF.Copy),
                     reads=[pxk], writes=[hk])
            else:
                k.op("dve", lambda e, PXt=PXt, HT=HT, kc=kc: e.tensor_scalar(out=HT[:, kc, :], in0=PXt[:], scalar1=1.0,
                                                                           scalar2=None, op0=ALU.mult),
                     reads=[pxk], writes=[hk])
        for m in range(8):
            PGt, PLt = PGL[pi % 4], PGL[(pi + 1) % 4]
            pgk, plk = "PGL%d" % (pi % 4), "PGL%d" % ((pi + 1) % 4)
            pi += 2
            GLU, SG, LIN = glu[mi % 2], sg[mi % 2], lin[mi % 2]
            glk, sgk, lik = "glu7_%d" % (mi % 2), "sg7_%d" % (mi % 2), "lin7_%d" % (mi % 2)
            mi += 1
            for (Pt, ptk, o) in ((PGt, pgk, 0), (PLt, plk, 1)):
                for kc in range(8):
                    k.op("pe", lambda e, Pt=Pt, W1t=W1t, HT=HT, kc=kc, m=m, o=o: e.matmul(
                        Pt[:], lhsT=W1t[:, kc, 2 * m * 128 + o:2 * (m + 1) * 128:2], rhs=HT[:, kc, :],
                        start=(kc == 0), stop=False), reads=[w1k, hk], writes=[ptk])
                k.op("pe", lambda e, Pt=Pt, B1t=B1t, m=m, o=o: e.matmul(
                    Pt[:], lhsT=B1t[0:1, 2 * m * 128 + o:2 * (m + 1) * 128:2], rhs=ones7[:],
                    start=False, stop=True), reads=[b1k, "ones7"], writes=[ptk])
            k.op("dve", lambda e, PGt=PGt, GLU=GLU: e.tensor_scalar(out=GLU[:], in0=PGt[:], scalar1=7.0, scalar2=None,
                                                                    op0=ALU.min), reads=[pgk], writes=[glk])
            k.op("act", lambda e, GLU=GLU, SG=SG: e.activation(out=SG[:], in_=GLU[:], func=AF.Sigmoid, scale=1.702),
                 reads=[glk], writes=[sgk])
            k.op("dve", lambda e, PLt=PLt, LIN=LIN: e.tensor_scalar(out=LIN[:], in0=PLt[:], scalar1=7.0, scalar2=-7.0,
                                                                    op0=ALU.min, op1=ALU.max),
                 reads=[plk], writes=[lik])
            k.op("dve", lambda e, GLU=GLU, SG=SG: e.tensor_tensor(out=SG[:], in0=GLU[:], in1=SG[:], op=ALU.mult),
                 reads=[glk, sgk], writes=[sgk])
            k.op("dve", lambda e, SG=SG, LIN=LIN, AT=AT, m=m: e.scalar_tensor_tensor(
                out=AT[:, m, :], in0=LIN[:], scalar=1.0, in1=SG[:], op0=ALU.add, op1=ALU.mult),
                reads=[sgk, lik], writes=[ak])
        for tt in range(4):
            YB = yb[yi % 2]
            ybk = "yb7_%d" % (yi % 2)
            yi += 1
            for hf in range(2):
                for m in range(8):
                    k.op("pe", lambda e, AT=AT, W2t=W2t, tt=tt, hf=hf, m=m: e.matmul(
                        PY[:, hf * 512:(hf + 1) * 512], lhsT=AT[:, m, tt * 128:(tt + 1) * 128],
                        rhs=W2t[:, m, hf * 512:(hf + 1) * 512], start=(m == 0), stop=(m == 7)),
                        reads=[ak, w2k], writes=["PY7"])
            k.op("act", lambda e, YB=YB: e.activation(out=YB[:], in_=PY[:], func=AF.Copy), reads=["PY7"], writes=[ybk])
            r0 = j * 512 + tt * 128
            k.dma("sp", lambda e, YB=YB, r0=r0: e.dma_start(out=YS[r0:r0 + 128, :], in_=YB[:]),
                  reads=[ybk], writes=["YS"])


def phase8_final(nc, k, X1, FD, GTd, b2, ln2, MOD, out, sp=None):
    def brow(name, ap1d, n=D):
        t = k.sb(name, [128, n], F32)
        k.dma("sp", lambda e: e.dma_start(out=t[:], in_=bcast_rows(ap1d, n)), reads=["MOD"], writes=[name])
        return t
    g2B = brow("g2B", ln2[0])
    b2B = brow("b2B", ln2[1])
    b2sb = k.sb("b2sb", [NE, D], F32)
    k.dma("sp", lambda e: e.dma_start(out=b2sb[:], in_=b2.ap()), writes=["b2sb"])
    Ft = [k.sb("F8_%d" % i, [128, D], F32) for i in range(2)]
    Xt = [k.sb("X8_%d" % i, [128, D], F32) for i in range(2)]
    gT = [k.sb("gT8_%d" % i, [NE, 128], F32) for i in range(2)]
    z = [k.sb("z8_%d" % i, [128, D], F32) for i in range(2)]
    st = k.sb("bnst8", [128, 2, 6], F32)
    mv = k.sb("bnmv8", [128, 2], F32)
    rstd = k.sb("rstd8", [128, 1], F32)
    PB = k.ps("PB8", [128, D])
    if sp is not None:
        ygs = [k.sb("yg8_%d" % i, [128, D], F32) for i in range(4)]
    ti = 0
    for b in range(BPC):
        gt2B = brow("gt2B%d" % b, MOD[b, 5120:6144])
        for t in range(NT8):
            F_, X_, GT_, Z_ = Ft[ti % 2], Xt[ti % 2], gT[ti % 2], z[ti % 2]
            fk, xk, gk, zk = "F8_%d" % (ti % 2), "X8_%d" % (ti % 2), "gT8_%d" % (ti % 2), "z8_%d" % (ti % 2)
            ti += 1
            tok0 = t * 128
            tg0 = b * SEQ + tok0
            if sp is None:
                k.dma("sp", lambda e, F_=F_, tg0=tg0: e.dma_start(out=F_[:], in_=FD[tg0:tg0 + 128, :]),
                      reads=["FD%d" % (tg0 // 128)], writes=[fk])
            else:
                tl = tg0 // 128
                for c4 in range(4):
                    Yg = ygs[(ti * 4 + c4) % 4]
                    ygk = "yg8_%d" % ((ti * 4 + c4) % 4)
                    k.dma("pool", lambda e, Yg=Yg, tl=tl, c4=c4: e.indirect_dma_start(
                        out=Yg[:], out_offset=None, in_=sp["YS"][:, :],
                        in_offset=bass.IndirectOffsetOnAxis(ap=sp["SLOTI"][:, tl, c4:c4 + 1], axis=0)),
                        reads=["YS", "SLOTI"], writes=[ygk])
                    if c4 == 0:
                        k.op("dve", lambda e, Yg=Yg, F_=F_, tl=tl, c4=c4: e.tensor_scalar(
                            out=F_[:], in0=Yg[:], scalar1=sp["GK"][:, tl, c4:c4 + 1], scalar2=None, op0=ALU.mult),
                            reads=[ygk, "GK"], writes=[fk])
                    else:
                        k.op("dve", lambda e, Yg=Yg, F_=F_, tl=tl, c4=c4: e.scalar_tensor_tensor(
                            out=F_[:], in0=Yg[:], scalar=sp["GK"][:, tl, c4:c4 + 1], in1=F_[:], op0=ALU.mult,
                            op1=ALU.add), reads=[ygk, "GK", fk], writes=[fk])
            k.dma("act", lambda e, X_=X_, b=b, tok0=tok0: e.dma_start(out=X_[:], in_=X1[b, tok0:tok0 + 128, :]),
                  reads=["X1"], writes=[xk])
            k.dma("act", lambda e, GT_=GT_, tg0=tg0: e.dma_start(out=GT_[:], in_=GTd[:, tg0:tg0 + 128]),
                  reads=["GTd"], writes=[gk])
            for hf in range(2):
                k.op("pe", lambda e, GT_=GT_, hf=hf: e.matmul(PB[:, hf * 512:(hf + 1) * 512], lhsT=GT_[:],
                                                             rhs=b2sb[:, hf * 512:(hf + 1) * 512], start=True,
                                                             stop=True), reads=[gk, "b2sb"], writes=["PB8"])
            k.op("dve", lambda e, F_=F_: e.tensor_tensor(out=F_[:], in0=PB[:], in1=F_[:], op=ALU.add),
                 reads=["PB8", fk], writes=[fk])
            k.op("pool", lambda e, F_=F_, gt2B=gt2B: e.tensor_tensor(out=F_[:], in0=F_[:], in1=gt2B[:], op=ALU.mult),
                 reads=[fk, "gt2B%d" % b], writes=[fk])
            k.op("dve", lambda e, F_=F_, X_=X_, Z_=Z_: e.scalar_tensor_tensor(out=Z_[:], in0=X_[:], scalar=ALPHA,
                                                                            in1=F_[:], op0=ALU.mult, op1=ALU.add),
                 reads=[fk, xk], writes=[zk])
            ln_stats(k, Z_, zk, st, mv, rstd, "8")
            k.op("dve", lambda e, Z_=Z_: e.tensor_scalar(out=Z_[:], in0=Z_[:], scalar1=mv[:, 0:1],
                                                        scalar2=rstd[:, 0:1], op0=ALU.subtract, op1=ALU.mult),
                 reads=[zk, "bnmv8", "rstd8"], writes=[zk])
            k.op("pool", lambda e, Z_=Z_: e.tensor_tensor(out=Z_[:], in0=Z_[:], in1=g2B[:], op=ALU.mult),
                 reads=[zk, "g2B"], writes=[zk])
            k.op("pool", lambda e, Z_=Z_: e.tensor_tensor(out=Z_[:], in0=Z_[:], in1=b2B[:], op=ALU.add),
                 reads=[zk, "b2B"], writes=[zk])
            k.dma("sp", lambda e, Z_=Z_, b=b, tok0=tok0: e.dma_start(out=out[b, tok0:tok0 + 128, :], in_=Z_[:]),
                  reads=[zk], writes=["out"])


def moe_consts():
    f32 = np.float32
    ustr_h = np.triu(np.ones((128, 128), f32), 1)
    thr_h = np.ascontiguousarray(np.broadcast_to((512.0 * np.arange(16, dtype=f32))[None, None, :], (1, NE, 16)))
    jg_h = np.ascontiguousarray(np.broadcast_to(np.arange(NBLK, dtype=f32)[None, :, None], (1, NBLK, NE)))
    kcp_h = (np.arange(8, dtype=f32)[None, :] * 128 + np.arange(128, dtype=f32)[:, None]).astype(f32)
    return ustr_h, thr_h, jg_h, kcp_h


_DFT = []


def _dft_consts():
    if not _DFT:
        n = np.arange(SEQ, dtype=np.int64)
        kt = (n[:, None] * n[None, :]) % SEQ
        ang = (2.0 * np.pi / SEQ) * kt.astype(np.float64)
        def lay(m):
            m = m.astype(BF_NP).reshape(32, 128, 8, 512)
            return np.ascontiguousarray(m.transpose(2, 1, 0, 3)).reshape(8, 128, 32 * 512)
        _DFT.append(lay(np.cos(ang) / 64.0))
        _DFT.append(lay(-np.sin(ang) / 64.0))
    return _DFT[0], _DFT[1]


def _run(inputs, dbg=None):
    nc = build_program(dbg)
    x = np.ascontiguousarray(inputs["x"], dtype=np.float32)
    ctx = np.ascontiguousarray(inputs["ctx"], dtype=np.float32)
    in_maps = []
    ident = np.eye(128, dtype=np.float32)
    f32 = np.float32
    vecs_h = np.ascontiguousarray(np.concatenate([
        inputs["w0"][0].reshape(-1), inputs["a0"][0].reshape(-1), inputs["k_k"][0], inputs["k_a"][0],
        inputs["gn_g"][0], inputs["gn_b"][0], inputs["r_k"][0].reshape(-1), inputs["b_fno"][0].reshape(-1),
    ]).astype(f32).reshape(44, 128))
    w2d_h = np.ascontiguousarray(inputs["w2_decay"][0].reshape(128, C).astype(f32))
    a2_h = np.ascontiguousarray(inputs["a2_iclr"][0].reshape(128, C).astype(f32))
    g2_h = np.ascontiguousarray(inputs["g2_gate"][0].astype(f32))
    bones_h = np.kron(np.eye(2, dtype=f32), np.ones((64, 64), f32))
    i2rep_h = np.ascontiguousarray(np.broadcast_to(
        np.concatenate([np.eye(64, dtype=f32)] * 2, 0)[:, None, :], (128, 16, 64))).astype(BF_NP)
    hsel_h = np.kron(np.eye(2, dtype=f32), np.ones((64, 1), f32)).astype(BF_NP)
    jmat_h = np.ascontiguousarray(np.eye(128, dtype=f32)[::-1])
    ustr_h, thr_h, jg_h, kcp_h = moe_consts()
    oh_h = np.zeros((128, TC, 40), f32)
    for tq in range(TC):
        oh_h[tq, tq, 0:8] = 1.0
        oh_h[64 + tq, tq, 32:40] = 1.0
    oh_h = oh_h.astype(BF_NP)
    smask_h = np.zeros((104, 8, 64), f32)
    for gl in range(8):
        smask_h[gl * 2:gl * 2 + 2, gl, :] = 1.0
        smask_h[16 + gl * 2:18 + gl * 2, gl, :] = 1.0
        smask_h[64 + gl, gl, :] = 1.0
        smask_h[96 + gl, gl, :] = 1.0
    smask_h = smask_h.reshape(104, 512)
    cc = np.arange(64)
    ang = 2.0 * np.pi * ((cc[:, None] * cc[None, :]) % 64) / 64.0
    ccbd_h = np.kron(np.eye(2), np.cos(ang) / 8.0).astype(f32)
    scbd_h = np.kron(np.eye(2), np.sin(ang) / 8.0).astype(f32)
    cosm_h, nsin_h = _dft_consts()
    wfno_h = np.ascontiguousarray(inputs["w_fno"][0].astype(f32))
    wout_h = np.ascontiguousarray(inputs["w_out"][0].astype(f32))
    ln1_h = np.ascontiguousarray(np.stack([inputs["ln1_g"][0], inputs["ln1_b"][0]], 0).astype(f32))
    ln2_h = np.ascontiguousarray(np.stack([inputs["ln2_g"][0], inputs["ln2_b"][0]], 0).astype(f32))
    wr_h = np.ascontiguousarray(inputs["w_router"][0].astype(f32))
    br_h = np.ascontiguousarray(inputs["b_router"][0][None, :].astype(f32))
    w1_h = np.ascontiguousarray(inputs["w1"][0].astype(f32))
    b1_h = np.ascontiguousarray(inputs["b1"][0].astype(f32))
    w2_h = np.ascontiguousarray(inputs["w2"][0].astype(f32))
    b2_h = np.ascontiguousarray(inputs["b2"][0].astype(f32))
    for c in range(NCORES):
        b0 = c * BPC
        m = {
            "x": x[b0:b0 + BPC],
            "ctx": ctx[b0:b0 + BPC],
            "cvec": np.ascontiguousarray(np.concatenate(
                [inputs["c"][b0:b0 + BPC], inputs["c_ctx"][None, :]], 0), dtype=np.float32),
            "ln0": np.ascontiguousarray(np.stack([inputs["ln0_g"], inputs["ln0_b"]], 0)),
            "w_ada": np.ascontiguousarray(inputs["w_ada"][0]),
            "b_ada": np.ascontiguousarray(inputs["b_ada"][0][None, :]),
            "w_in": np.ascontiguousarray(inputs["w_in"][0]),
            "mu": np.ascontiguousarray(inputs["mu_shift"][0].reshape(SHW // 128, 128)),
            "ident": ident,
            "ustr": ustr_h, "thr": thr_h, "jg": jg_h, "kcp": kcp_h,
            "bonesb": bones_h.astype(BF_NP), "i2rep": i2rep_h, "hsel": hsel_h, "oh": oh_h, "smask": smask_h,
            "jmat": jmat_h, "ccbd": ccbd_h, "scbd": scbd_h, "cosm": cosm_h, "nsin": nsin_h,
            "wfno": wfno_h, "w_out": wout_h, "ln1": ln1_h, "ln2": ln2_h, "w_router": wr_h, "b_router": br_h,
            "w1": w1_h, "b1": b1_h, "w2": w2_h, "b2": b2_h,
            "vecs": vecs_h, "w2d": w2d_h, "a2": a2_h, "g2": g2_h, "bones": bones_h,
        }
        in_maps.append(m)
    res = run_bass_kernel_spmd(nc, in_maps, core_ids=list(range(NCORES)))
    return res


def kernel(**inputs):
    res = _run(inputs)
    outs = [r["out"] for r in res.results]
    return np.concatenate(outs, axis=0).astype(np.float32)
```
